# Optimizing a Trainium2 kernel written in Bass

```python
import math
import jax
import jax.numpy as jnp
from jax import lax
import numpy as np

D_MODEL = 2048
BATCH = 4
SEQ = 2048
DEPTH = 1

GRID_W = 64
CTX_LEN = 256
HEAD_DIM = 64
D_HY = 1024
D_NA = D_MODEL - D_HY
NA_HEADS = D_NA // HEAD_DIM
OFF_Q = 3 * D_HY
OFF_K = OFF_Q + D_NA
OFF_V = OFF_K + D_NA
D_PROJ = OFF_V + D_NA
SHORT_CONV = 3
HY_ORDER = 2
FILTER_EMB = 33
FILTER_ORDER = 64
DECAY_TARGET = 1e-2
FAST_DECAY_PCT = 0.3
SLOW_DECAY_PCT = 1.5
NA_KH = 8
NA_KW = 16
NA_QB = 16
NA_KB = 2 * NA_KW
PEER_HEADS = 8
PEER_NKEYS = 128
PEER_EXPERTS = PEER_NKEYS * PEER_NKEYS
PEER_DKEY = 256
PEER_TOPK = 16
PEER_CHUNK = 128
LN_EPS = 1e-5
ALPHA = (2.0 * DEPTH) ** 0.25
BETA = (8.0 * DEPTH) ** -0.25
NEG_INF = -1e30

kernel_name = 'hymba_hyena_natten_peer_dit_block'


def layer_norm(x, g, b):
    xf = x.astype(jnp.float32)
    xc = xf - jnp.mean(xf, -1, keepdims=True)
    var = jnp.mean(xc * xc, -1, keepdims=True)
    return (xc * lax.rsqrt(var + LN_EPS) * g + b).astype(x.dtype)


def rms_norm(x, g):
    xf = x.astype(jnp.float32)
    return (xf * lax.rsqrt(jnp.mean(xf * xf, -1, keepdims=True) + LN_EPS) * g).astype(x.dtype)


def ada_params(cond, w_ada, b_ada):
    m = jax.nn.silu(cond) @ w_ada + b_ada
    return m.reshape(cond.shape[0], 6, 1, D_MODEL)


def modulate(x, shift, scale):
    return x * (1.0 + scale) + shift


def short_conv(z, w, b):
    L = z.shape[1]
    zp = jnp.pad(z, ((0, 0), (1, 1), (0, 0)))
    return zp[:, :L] * w[0] + zp[:, 1:L + 1] * w[1] + zp[:, 2:L + 2] * w[2] + b


def hyena_filters(L, w1, b1, w2, b2, w3, b3, w4, freq):
    t = jnp.linspace(0.0, 1.0, L, dtype=jnp.float32)[:, None]
    bands = (FILTER_EMB - 1) // 2
    w = 2.0 * math.pi * jnp.arange(L, dtype=jnp.float32)[:, None] / L
    f = jnp.linspace(1e-4, bands - 1, bands, dtype=jnp.float32)[None, :]
    z = jnp.concatenate([t, jnp.cos(f * w), -jnp.sin(f * w)], axis=-1)
    h = jnp.sin(freq[0] * (z @ w1 + b1))
    h = jnp.sin(freq[1] * (h @ w2 + b2))
    h = jnp.sin(freq[2] * (h @ w3 + b3))
    h = (h @ w4).astype(jnp.float32).reshape(L, HY_ORDER, 2, D_HY)
    deltas = jnp.abs(jnp.linspace(math.log(DECAY_TARGET) / SLOW_DECAY_PCT,
                                  math.log(DECAY_TARGET) / FAST_DECAY_PCT, D_HY, dtype=jnp.float32))
    h = h * jnp.exp(-t * deltas)[:, None, None, :]
    h = h / jnp.sum(jnp.abs(h), axis=(0, 2), keepdims=True)
    fwd, bwd = h[:, :, 0], h[:, :, 1]
    k_circ = jnp.concatenate([fwd, jnp.zeros_like(fwd[:1]), bwd[:0:-1]], axis=0)
    return jnp.fft.rfft(k_circ, axis=0)


def hyena(z, kf, skip, conv_w, conv_b):
    L = z.shape[1]
    z = short_conv(z, conv_w, conv_b)
    v, x1, x2 = jnp.split(z, 3, axis=-1)

    def long_conv(u, o):
        uf = jnp.fft.rfft(u.astype(jnp.float32), n=2 * L, axis=1)
        y = jnp.fft.irfft(uf * kf[:, o], n=2 * L, axis=1)[:, :L]
        return y.astype(u.dtype) + u * skip[o]

    y = x1 * long_conv(v, 0)
    return x2 * long_conv(y, 1)


def neighbourhood_attention(q, k, v, k_ctx, v_ctx, rpb):
    B, L, H, DH = q.shape
    rows = L // GRID_W
    kh = min(NA_KH, rows)
    ncb = GRID_W // NA_QB
    nk = kh * NA_KB
    r = jnp.arange(rows)
    row_start = jnp.clip(r - kh // 2, 0, rows - kh)
    key_rows = row_start[:, None] + jnp.arange(kh)
    j = jnp.arange(ncb)
    col_start = jnp.clip(j * NA_QB - NA_KW // 2, 0, GRID_W - NA_KB)
    key_cols = col_start[:, None] + jnp.arange(NA_KB)
    tok = (key_rows[:, None, :, None] * GRID_W + key_cols[None, :, None, :]).reshape(rows, ncb, nk)
    k_blk = k[:, tok]
    v_blk = v[:, tok]
    q_blk = q.reshape(B, rows, ncb, NA_QB, H, DH)
    q_col = j[:, None] * NA_QB + jnp.arange(NA_QB)
    win_start = jnp.clip(q_col - NA_KW // 2, 0, GRID_W - NA_KW)
    rel = key_cols[:, None, :] - win_start[:, :, None]
    col_ok = (rel >= 0) & (rel < NA_KW)
    mask = jnp.broadcast_to(col_ok[:, :, None, :], (ncb, NA_QB, kh, NA_KB)).reshape(ncb, NA_QB, nk)
    d_row = key_rows - r[:, None]
    d_col = jnp.clip(key_cols[:, None, :] - q_col[:, :, None] + NA_KW - 1, 0, 2 * NA_KW - 2)
    bias = rpb[:, d_row + NA_KH - 1][..., d_col]
    bias = bias.transpose(0, 1, 3, 4, 2, 5).reshape(H, rows, ncb, NA_QB, nk)
    scale = HEAD_DIM ** -0.5
    s_lat = jnp.einsum('brjqhd,brjkhd->bhrjqk', q_blk, k_blk).astype(jnp.float32) * scale + bias
    s_lat = jnp.where(mask, s_lat, NEG_INF)
    s_ctx = jnp.einsum('brjqhd,bchd->bhrjqc', q_blk, k_ctx).astype(jnp.float32) * scale
    p = jax.nn.softmax(jnp.concatenate([s_lat, s_ctx], axis=-1), axis=-1).astype(v.dtype)
    o = (jnp.einsum('bhrjqk,brjkhd->brjqhd', p[..., :nk], v_blk)
         + jnp.einsum('bhrjqc,bchd->brjqhd', p[..., nk:], v_ctx))
    return o.reshape(B, L, H * DH)


def context_attention(q, k, v):
    B, C, H, DH = q.shape
    s = jnp.einsum('bqhd,bkhd->bhqk', q, k).astype(jnp.float32) * HEAD_DIM ** -0.5
    p = jax.nn.softmax(s, axis=-1).astype(v.dtype)
    return jnp.einsum('bhqk,bkhd->bqhd', p, v).reshape(B, C, H * DH)


def merge_groups(hy_out, na_out, g_hy, g_na, w_out):
    return jnp.concatenate([rms_norm(hy_out, g_hy), rms_norm(na_out, g_na)], axis=-1) @ w_out


def peer(u, w_q, sub_keys, expert_u, expert_v):
    B, L, D = u.shape
    q = (u @ w_q).reshape(B, L, PEER_HEADS, 2, PEER_DKEY // 2)
    s = jnp.einsum('blhpd,hpnd->blhpn', q, sub_keys).astype(jnp.float32)
    s_a, i_a = lax.top_k(s[..., 0, :], PEER_TOPK)
    s_b, i_b = lax.top_k(s[..., 1, :], PEER_TOPK)
    n_cand = PEER_TOPK * PEER_TOPK
    cand_s = (s_a[..., :, None] + s_b[..., None, :]).reshape(B, L, PEER_HEADS, n_cand)
    cand_i = (i_a[..., :, None] * PEER_NKEYS + i_b[..., None, :]).reshape(B, L, PEER_HEADS, n_cand)
    top_s, pos = lax.top_k(cand_s, PEER_TOPK)
    idx = jnp.take_along_axis(cand_i, pos, axis=-1)
    g = jax.nn.softmax(top_s, axis=-1).astype(u.dtype)
    n_chunks = (B * L) // PEER_CHUNK
    sel = PEER_HEADS * PEER_TOPK
    u_c = u.reshape(n_chunks, PEER_CHUNK, D)
    idx_c = idx.reshape(n_chunks, PEER_CHUNK, sel)
    g_c = g.reshape(n_chunks, PEER_CHUNK, sel)

    def chunk(args):
        ut, it, gt = args
        act = jax.nn.gelu(jnp.einsum('td,tkd->tk', ut, expert_u[it]), approximate=False)
        return jnp.einsum('tk,tkd->td', gt * act, expert_v[it])

    return lax.map(chunk, (u_c, idx_c, g_c)).reshape(B, L, D)


def setup_inputs(seed: int = 0) -> dict:
    key = jax.random.key(seed)
    ks = jax.random.split(key, 32)

    def nrm(k, shape, s):
        return jax.random.normal(k, shape, jnp.float32) * s

    col_scale = jnp.concatenate([jnp.full((D_HY,), BETA, jnp.float32),
                                 jnp.ones((OFF_V - D_HY,), jnp.float32),
                                 jnp.full((D_NA,), BETA, jnp.float32)])
    return {
        'x': nrm(ks[0], (BATCH, SEQ, D_MODEL), 1.0),
        'c': nrm(ks[1], (BATCH, D_MODEL), 1.0),
        'ctx': nrm(ks[2], (BATCH, CTX_LEN, D_MODEL), 1.0),
        'c_ctx': nrm(ks[3], (D_MODEL,), 1.0),
        'w_ada': nrm(ks[4], (DEPTH, D_MODEL, 6 * D_MODEL), D_MODEL ** -0.5),
        'b_ada': nrm(ks[5], (DEPTH, 6 * D_MODEL), 0.01),
        'w_in': nrm(ks[6], (DEPTH, D_MODEL, D_PROJ), D_MODEL ** -0.5) * col_scale,
        'conv_w': nrm(ks[7], (DEPTH, SHORT_CONV, 3 * D_HY), SHORT_CONV ** -0.5),
        'conv_b': nrm(ks[8], (DEPTH, 3 * D_HY), 0.01),
        'filt_w1': nrm(ks[9], (DEPTH, FILTER_EMB, FILTER_ORDER), FILTER_EMB ** -0.5),
        'filt_b1': nrm(ks[10], (DEPTH, FILTER_ORDER), 0.1),
        'filt_w2': nrm(ks[11], (DEPTH, FILTER_ORDER, FILTER_ORDER), FILTER_ORDER ** -0.5),
        'filt_b2': nrm(ks[12], (DEPTH, FILTER_ORDER), 0.1),
        'filt_w3': nrm(ks[13], (DEPTH, FILTER_ORDER, FILTER_ORDER), FILTER_ORDER ** -0.5),
        'filt_b3': nrm(ks[14], (DEPTH, FILTER_ORDER), 0.1),
        'filt_w4': nrm(ks[15], (DEPTH, FILTER_ORDER, HY_ORDER * 2 * D_HY), FILTER_ORDER ** -0.5),
        'filt_freq': 1.0 + nrm(ks[16], (DEPTH, 3, FILTER_ORDER), 0.01),
        'hy_skip': nrm(ks[17], (DEPTH, HY_ORDER, D_HY), 0.5),
        'na_rpb': nrm(ks[18], (DEPTH, NA_HEADS, 2 * NA_KH - 1, 2 * NA_KW - 1), 0.02),
        'hy_norm_g': 1.0 + nrm(ks[19], (DEPTH, D_HY), 0.01),
        'na_norm_g': 1.0 + nrm(ks[20], (DEPTH, D_NA), 0.01),
        'w_out': nrm(ks[21], (DEPTH, D_MODEL, D_MODEL), D_MODEL ** -0.5 * BETA),
        'ln1_g': 1.0 + nrm(ks[22], (DEPTH, D_MODEL), 0.01),
        'ln1_b': nrm(ks[23], (DEPTH, D_MODEL), 0.01),
        'peer_wq': nrm(ks[24], (DEPTH, D_MODEL, PEER_HEADS * PEER_DKEY), D_MODEL ** -0.5),
        'peer_keys': nrm(ks[25], (DEPTH, PEER_HEADS, 2, PEER_NKEYS, PEER_DKEY // 2), (PEER_DKEY // 2) ** -0.5),
        'peer_u': nrm(ks[26], (DEPTH, PEER_EXPERTS, D_MODEL), D_MODEL ** -0.5 * BETA),
        'peer_v': nrm(ks[27], (DEPTH, PEER_EXPERTS, D_MODEL), BETA),
        'ln2_g': 1.0 + nrm(ks[28], (DEPTH, D_MODEL), 0.01),
        'ln2_b': nrm(ks[29], (DEPTH, D_MODEL), 0.01),
    }


def reference(x, c, ctx, c_ctx, w_ada, b_ada, w_in, conv_w, conv_b, filt_w1, filt_b1, filt_w2, filt_b2,
              filt_w3, filt_b3, filt_w4, filt_freq, hy_skip, na_rpb, hy_norm_g, na_norm_g, w_out,
              ln1_g, ln1_b, peer_wq, peer_keys, peer_u, peer_v, ln2_g, ln2_b):
    B, L, _ = x.shape
    C = ctx.shape[1]
    for l in range(DEPTH):
        m = ada_params(c, w_ada[l], b_ada[l])
        mc = ada_params(c_ctx[None], w_ada[l], b_ada[l])
        filt = (filt_w1[l], filt_b1[l], filt_w2[l], filt_b2[l], filt_w3[l], filt_b3[l], filt_w4[l], filt_freq[l])

        u = modulate(x, m[:, 0], m[:, 1])
        uc = modulate(ctx, mc[:, 0], mc[:, 1])
        proj = u @ w_in[l]
        kv_c = uc @ w_in[l][:, OFF_K:]
        k_c = kv_c[..., :D_NA].reshape(B, C, NA_HEADS, HEAD_DIM)
        v_c = kv_c[..., D_NA:].reshape(B, C, NA_HEADS, HEAD_DIM)
        hy_out = hyena(proj[..., :OFF_Q], hyena_filters(L, *filt), hy_skip[l], conv_w[l], conv_b[l])
        q = proj[..., OFF_Q:OFF_K].reshape(B, L, NA_HEADS, HEAD_DIM)
        k = proj[..., OFF_K:OFF_V].reshape(B, L, NA_HEADS, HEAD_DIM)
        v = proj[..., OFF_V:].reshape(B, L, NA_HEADS, HEAD_DIM)
        na_out = neighbourhood_attention(q, k, v, k_c, v_c, na_rpb[l])
        y = merge_groups(hy_out, na_out, hy_norm_g[l], na_norm_g[l], w_out[l])
        x_new = layer_norm(ALPHA * x + m[:, 2] * y, ln1_g[l], ln1_b[l])

        f = peer(modulate(x_new, m[:, 3], m[:, 4]), peer_wq[l], peer_keys[l], peer_u[l], peer_v[l])
        x_new = layer_norm(ALPHA * x_new + m[:, 5] * f, ln2_g[l], ln2_b[l])

        if l < DEPTH - 1:
            proj_c = uc @ w_in[l][:, :OFF_K]
            hy_c = hyena(proj_c[..., :OFF_Q], hyena_filters(C, *filt), hy_skip[l], conv_w[l], conv_b[l])
            q_c = proj_c[..., OFF_Q:].reshape(B, C, NA_HEADS, HEAD_DIM)
            att_c = context_attention(q_c, k_c, v_c)
            y_c = merge_groups(hy_c, att_c, hy_norm_g[l], na_norm_g[l], w_out[l])
            ctx = layer_norm(ALPHA * ctx + mc[:, 2] * y_c, ln1_g[l], ln1_b[l])
            f_c = peer(modulate(ctx, mc[:, 3], mc[:, 4]), peer_wq[l], peer_keys[l], peer_u[l], peer_v[l])
            ctx = layer_norm(ALPHA * ctx + mc[:, 5] * f_c, ln2_g[l], ln2_b[l])
        x = x_new
    return x
```

```python
import numpy as np
import ml_dtypes
from contextlib import ExitStack
import concourse.bass as bass
import concourse.mybir as mybir
from concourse.bass_utils import run_bass_kernel_spmd

F32 = mybir.dt.float32
BF16 = mybir.dt.bfloat16
U32 = mybir.dt.uint32
I32 = mybir.dt.int32
AF = mybir.ActivationFunctionType
ALU = mybir.AluOpType
AX = mybir.AxisListType

D = 2048
L = 2048
LH = 1024
CTX = 256
NTOK = 2560
DHY = 1024
NEG = -30000.0
ALPHA = 2.0 ** 0.25
LN_EPS = 1e-5
NFFT = 4096
STRICT_SAME_ENGINE = True


class KB:
    NDS = 12

    def __init__(self, nc, es):
        self.nc = nc
        self.E = {'pe': nc.tensor, 'act': nc.scalar, 'dve': nc.vector, 'pool': nc.gpsimd, 'sp': nc.sync}
        self.sem = {e: es.enter_context(nc.semaphore('sem_' + e)) for e in ('pe', 'act', 'dve', 'pool')}
        self.cnt = dict.fromkeys(self.sem, 0)
        self.dsem = {q: [es.enter_context(nc.semaphore('dq_%s_%d' % (q, i))) for i in range(self.NDS)]
                     for q in ('sp', 'pool')}
        self.dcnt = {q: [0] * self.NDS for q in ('sp', 'pool')}
        self.dnext = {'sp': 0, 'pool': 0}
        self.seen = {e: {} for e in self.E}
        self.lastw = {}
        self.readers = {}
        self.ninst = 0

    def _wait(self, e, tok):
        sem, val = tok
        if e in self.sem and sem is self.sem[e] and (e == 'pe' or not STRICT_SAME_ENGINE):
            return
        if self.seen[e].get(id(sem), 0) >= val:
            return
        self.E[e].wait_ge(sem, val)
        self.seen[e][id(sem)] = val

    def _deps(self, e, r, w):
        for key in r:
            t = self.lastw.get(key)
            if t:
                self._wait(e, t)
        for key in w:
            t = self.lastw.get(key)
            if t:
                self._wait(e, t)
            for t in self.readers.get(key, {}).values():
                self._wait(e, t)

    def _record(self, tok, r, w):
        for key in r:
            d = self.readers.setdefault(key, {})
            old = d.get(id(tok[0]))
            if old is None or old[1] < tok[1]:
                d[id(tok[0])] = tok
        for key in w:
            self.lastw[key] = tok
            self.readers[key] = {}

    def op(self, e, fn, r=(), w=()):
        self._deps(e, r, w)
        inst = fn(self.E[e])
        self.cnt[e] += 1
        inst.then_inc(self.sem[e], 1)
        self._record((self.sem[e], self.cnt[e]), r, w)
        self.ninst += 1

    def dma(self, q, fn, r=(), w=()):
        slot = self.dnext[q] % self.NDS
        self.dnext[q] += 1
        sem = self.dsem[q][slot]
        if self.dcnt[q][slot] > 0:
            self._wait(q, (sem, 16 * self.dcnt[q][slot]))
        self._deps(q, r, w)
        inst = fn(self.E[q])
        self.dcnt[q][slot] += 1
        inst.then_inc(sem, 16)
        self._record((sem, 16 * self.dcnt[q][slot]), r, w)
        self.ninst += 1

    def barrier(self):
        toks = [(self.sem[e], self.cnt[e]) for e in self.sem if self.cnt[e] > 0]
        for q in self.dsem:
            for i, sem in enumerate(self.dsem[q]):
                if self.dcnt[q][i] > 0:
                    toks.append((sem, 16 * self.dcnt[q][i]))
        for e in self.E:
            for tok in toks:
                self._wait(e, tok)

    def finish(self, keys):
        for key in keys:
            t = self.lastw.get(key)
            if t:
                self._wait('sp', t)


class T:
    pass


def keys(name, n):
    return [(name, i) for i in range(n)]


def stage_ada(kb, t):
    nc = kb.nc
    with ExitStack() as es:
        sb = lambda name, shape, dt: es.enter_context(nc.sbuf_tensor(name, shape, dt))
        cT = sb('cT', [128, 2, 16], F32)
        scT = sb('scT', [128, 16, 2], F32)
        wbuf = [sb('wada%d' % i, [128, 16, 512], F32) for i in range(2)]
        bada = sb('bada', [2, 12288], F32)
        mall = sb('mall', [2, 12288], F32)
        kb.dma('sp', lambda e: e.dma_start(out=cT[:], in_=t.c2.ap().rearrange("n (k p) -> p n k", p=128)), w=['cT'])
        kb.op('act', lambda e: e.activation(out=scT[:].rearrange("p k n -> p n k"), in_=cT[:], func=AF.Silu),
              r=['cT'], w=['scT'])
        kb.dma('sp', lambda e: e.dma_start(out=bada[:], in_=bass.AP(t.b_ada, 0, [[0, 2], [1, 12288]])), w=['bada'])
        wv = t.w_ada.ap().rearrange("(k p) n -> p k n", p=128)
        for j in range(24):
            wb = wbuf[j % 2]
            kb.dma('sp', lambda e: e.dma_start(out=wb[:], in_=wv[:, :, j * 512:(j + 1) * 512]), w=[('wada', j % 2)])
            ps = t.ps[j % 2]
            for k in range(16):
                kb.op('pe', lambda e: e.matmul(ps[0:2, :], lhsT=scT[:, k, :], rhs=wb[:, k, :],
                                               start=(k == 0), stop=(k == 15)),
                      r=['scT', ('wada', j % 2)], w=[('ps', j % 2)])
            kb.op('dve', lambda e: e.tensor_tensor(out=mall[:, j * 512:(j + 1) * 512], in0=ps[0:2, :],
                                                   in1=bada[:, j * 512:(j + 1) * 512], op=ALU.add),
                  r=[('ps', j % 2), 'bada'], w=['mall'])
        kb.dma('sp', lambda e: e.dma_start(out=t.m_scr.ap(), in_=mall[:]), r=['mall'], w=['m_scr'])


def stage_uT(kb, t, uT):
    nc = kb.nc
    UK = keys('uT', 16)
    with ExitStack() as es:
        sb = lambda name, shape, dt: es.enter_context(nc.sbuf_tensor(name, shape, dt))
        mfm = sb('mfm', [128, 2, 6, 16], F32)
        sc1p = sb('sc1p', [128, 2, 16], F32)
        xg = [sb('xg%d' % i, [128, 4, 2048], F32) for i in range(2)]
        kb.dma('sp', lambda e: e.dma_start(out=mfm[:], in_=bass.AP(t.m_scr, 0, [[1, 128], [12288, 2], [2048, 6], [128, 16]])),
               r=['m_scr'], w=['mfm'])
        kb.op('dve', lambda e: e.tensor_scalar(out=sc1p[:], in0=mfm[:, :, 1, :], scalar1=1.0, scalar2=None, op0=ALU.add),
              r=['mfm'], w=['sc1p'])
        xv = t.xr.ap().rearrange("(g t p) d -> g p t d", t=4, p=128)
        pi = 0
        for g in range(5):
            xb = xg[g % 2]
            nt = 4 if g < 4 else 2
            if g < 4:
                kb.dma('sp', lambda e: e.dma_start(out=xb[:], in_=xv[g]), w=[('xg', g % 2)])
                n = 0
                col0 = 256 + g * 512
            else:
                kb.dma('sp', lambda e: e.dma_start(out=xb[:, 0:2, :], in_=t.ctxb.ap().rearrange("(t p) d -> p t d", p=128)),
                       w=[('xg', g % 2)])
                n = 1
                col0 = 2304
            for k in range(16):
                p = pi % 4
                pi += 1
                ps = t.ps[p]
                for tt in range(nt):
                    kb.op('pe', lambda e: e.transpose(ps[:, tt * 128:(tt + 1) * 128], xb[:, tt, k * 128:(k + 1) * 128], t.ident[:]),
                          r=[('xg', g % 2), 'ident'], w=[('ps', p)])
                kb.op('act', lambda e: e.activation(out=uT[:, k, col0:col0 + nt * 128], in_=ps[:, 0:nt * 128], func=AF.Identity,
                                                    scale=sc1p[:, n, k:k + 1], bias=mfm[:, n, 0, k:k + 1]),
                      r=[('ps', p), 'sc1p', 'mfm'], w=[('uT', k)])
        kb.op('pool', lambda e: e.tensor_copy(out=uT[:, :, 0:256], in_=uT[:, :, 2048:2304]), r=UK, w=UK)


def stage_proj(kb, t, uT):
    nc = kb.nc
    UK = keys('uT', 16)
    with ExitStack() as es:
        sb = lambda name, shape, dt: es.enter_context(nc.sbuf_tensor(name, shape, dt))
        wblk = [sb('wblk%d' % i, [128, 16, 512], BF16) for i in range(2)]
        cw = sb('cw', [128, 3, 24], F32)
        cb = sb('cb', [128, 24], F32)
        ee = sb('ee', [128, 2], F32)
        cwe = sb('cwe', [128, 4, 24], F32)
        zs = [sb('zs%d' % i, [128, 2050], F32) for i in range(2)]
        yc = [sb('yc%d' % i, [128, 2048], F32) for i in range(2)]
        ytok = [sb('ytok%d' % i, [128, 16, 128], F32) for i in range(2)]
        qst = [sb('qst%d' % i, [128, 2560], BF16) for i in range(2)]
        vst = [sb('vst%d' % i, [128, 512], BF16) for i in range(2)]
        kb.dma('sp', lambda e: e.dma_start(out=cw[:], in_=t.conv_w.ap().rearrange("k (j p) -> p k j", p=128)), w=['cw'])
        kb.dma('sp', lambda e: e.dma_start(out=cb[:], in_=t.conv_b.ap().rearrange("o (j p) -> p (o j)", p=128)), w=['cb'])
        kb.dma('sp', lambda e: e.dma_start(out=ee[:], in_=bass.AP(t.e01, 0, [[0, 128], [1, 2]])), w=['ee'])
        for i, (ei, tap) in enumerate([(0, 0), (0, 2), (1, 0), (1, 2)]):
            kb.op('dve', lambda e: e.tensor_scalar(out=cwe[:, i, :], in0=cw[:, tap, :], scalar1=ee[:, ei:ei + 1], scalar2=-1.0,
                                                   op0=ALU.mult, op1=ALU.mult), r=['cw', 'ee'], w=['cwe'])
        wv = t.w_in.ap().rearrange("(k p) n -> p k n", p=128)
        hzv = t.hz_tok.ap().rearrange("(tt p) c -> p tt c", p=128)
        state = {'ev': 0}

        def load_blk(blk):
            kb.dma('pool', lambda e: e.dma_start(out=wblk[blk % 2][:], in_=wv[:, :, blk * 512:(blk + 1) * 512]), w=[('wblk', blk % 2)])

        def emit_P(j):
            blk, cc = j // 4, j % 4
            if cc == 0:
                load_blk(blk)
            wb, wk = wblk[blk % 2], ('wblk', blk % 2)
            z, zk = zs[j % 2], ('zs', j % 2)
            for tb in range(4):
                ps = t.ps[tb]
                for k in range(16):
                    kb.op('pe', lambda e: e.matmul(ps[:], lhsT=wb[:, k, cc * 128:(cc + 1) * 128],
                                                   rhs=uT[:, k, 256 + tb * 512:256 + (tb + 1) * 512],
                                                   start=(k == 0), stop=(k == 15)),
                          r=[wk, ('uT', k)], w=[('ps', tb)])
                kb.op('act', lambda e: e.activation(out=z[:, 1 + tb * 512:1 + (tb + 1) * 512], in_=ps[:], func=AF.Identity),
                      r=[('ps', tb)], w=[zk])
            kb.op('act', lambda e: e.activation(out=z[:, 0:1], in_=z[:, 2048:2049], func=AF.Identity), r=[zk], w=[zk])
            kb.op('act', lambda e: e.activation(out=z[:, 2049:2050], in_=z[:, 1:2], func=AF.Identity), r=[zk], w=[zk])

        def emit_C(j):
            z, zk = zs[j % 2], ('zs', j % 2)
            y, yk_ = yc[j % 2], ('yc', j % 2)
            kb.op('dve', lambda e: e.tensor_scalar(out=y[:], in0=z[:, 1:2049], scalar1=cw[:, 1, j:j + 1],
                                                   scalar2=cb[:, j:j + 1], op0=ALU.mult, op1=ALU.add),
                  r=[zk, 'cw', 'cb'], w=[yk_])
            kb.op('dve', lambda e: e.scalar_tensor_tensor(out=y[:], in0=z[:, 0:2048], scalar=cw[:, 0, j:j + 1], in1=y[:],
                                                          op0=ALU.mult, op1=ALU.add), r=[zk, 'cw'], w=[yk_])
            kb.op('dve', lambda e: e.scalar_tensor_tensor(out=y[:], in0=z[:, 2:2050], scalar=cw[:, 2, j:j + 1], in1=y[:],
                                                          op0=ALU.mult, op1=ALU.add), r=[zk, 'cw'], w=[yk_])
            for (ycol, zcol, ci) in [(0, 0, 0), (2047, 2049, 1), (1024, 1024, 2), (1023, 1025, 3)]:
                kb.op('dve', lambda e: e.scalar_tensor_tensor(out=y[:, ycol:ycol + 1], in0=z[:, zcol:zcol + 1],
                                                              scalar=cwe[:, ci, j:j + 1], in1=y[:, ycol:ycol + 1],
                                                              op0=ALU.mult, op1=ALU.add), r=[zk, 'cwe'], w=[yk_])

        def emit_T(j):
            y, yk_ = yc[j % 2], ('yc', j % 2)
            yt = ytok[j % 2]
            yk = ('ytok', j % 2)
            for g in range(4):
                ps = t.ps[4 + g]
                for i in range(4):
                    tt = g * 4 + i
                    kb.op('pe', lambda e: e.transpose(ps[:, i * 128:(i + 1) * 128], y[:, tt * 128:(tt + 1) * 128], t.ident[:]),
                          r=[yk_, 'ident'], w=[('ps', 4 + g)])
                eng = 'act' if (state['ev'] % 2 == 0) else 'dve'
                state['ev'] += 1
                if eng == 'act':
                    kb.op('act', lambda e: e.activation(out=yt[:, g * 4:(g + 1) * 4, :], in_=ps[:].rearrange("p (a b) -> p a b", b=128),
                                                        func=AF.Identity), r=[('ps', 4 + g)], w=[yk])
                else:
                    kb.op('dve', lambda e: e.tensor_copy(out=yt[:, g * 4:(g + 1) * 4, :], in_=ps[:].rearrange("p (a b) -> p a b", b=128)),
                          r=[('ps', 4 + g)], w=[yk])
            kb.dma('sp', lambda e: e.dma_start(out=hzv[:, :, j * 128:(j + 1) * 128], in_=yt[:]), r=[yk], w=[('hz_tok', j)])

        emit_P(0)
        for j in range(24):
            if j + 1 < 24:
                emit_P(j + 1)
            emit_C(j)
            emit_T(j)
        for blk in range(6, 12):
            wb = wblk[blk % 2]
            wk = ('wblk', blk % 2)
            load_blk(blk)
            if False:
                pass
            elif blk < 10:
                isq = blk < 8
                for hp in range(4):
                    h = (blk % 2) * 8 + hp * 2
                    st = qst[hp % 2]
                    sk = ('qst', hp % 2)
                    nb = 2 if isq else 5
                    for tb in range(nb):
                        p = tb % 4
                        ps = t.ps[p]
                        c0 = 256 + tb * 512 if isq else tb * 512
                        for k in range(16):
                            kb.op('pe', lambda e: e.matmul(ps[:], lhsT=wb[:, k, hp * 128:(hp + 1) * 128], rhs=uT[:, k, c0:c0 + 512],
                                                           start=(k == 0), stop=(k == 15)), r=[wk, ('uT', k)], w=[('ps', p)])
                        kb.op('act', lambda e: e.activation(out=st[:, tb * 512:(tb + 1) * 512], in_=ps[:], func=AF.Identity),
                              r=[('ps', p)], w=[sk])
                    if isq:
                        kb.dma('sp', lambda e: e.dma_start(out=t.QT_scr.ap()[h:h + 2].rearrange("h d n -> (h d) n"), in_=st[:, 0:1024]), r=[sk], w=[('QT_scr', h)])
                    else:
                        kb.dma('sp', lambda e: e.dma_start(out=t.KT_scr.ap()[h:h + 2].rearrange("h d n -> (h d) n"), in_=st[:, 0:2560]), r=[sk], w=[('KT_scr', h)])
            else:
                vb = blk - 10
                for rr in range(20):
                    p = rr % 4
                    ps = t.ps[p]
                    for k in range(16):
                        kb.op('pe', lambda e: e.matmul(ps[:], lhsT=uT[:, k, rr * 128:(rr + 1) * 128], rhs=wb[:, k, :],
                                                       start=(k == 0), stop=(k == 15)), r=[wk, ('uT', k)], w=[('ps', p)])
                    st = vst[rr % 2]
                    sk = ('vst', rr % 2)
                    kb.op('act', lambda e: e.activation(out=st[:], in_=ps[:], func=AF.Identity), r=[('ps', p)], w=[sk])
                    kb.dma('sp', lambda e: e.dma_start(out=t.V_scr.ap()[rr * 128:(rr + 1) * 128, vb * 512:(vb + 1) * 512], in_=st[:]),
                           r=[sk], w=[('V_scr', rr, vb)])


def att_rows():
    rows = []
    for i in range(16):
        if i < 4:
            rows.append((i, 0, 12, 3 - i, i))
        elif i <= 12:
            rows.append((i, i, 8, 3, None))
        else:
            rows.append((i, 12, 12, 15 - i, 4 + i - 13))
    return rows


def stage_bias(kb, t):
    nc = kb.nc
    with ExitStack() as es:
        sb = lambda name, shape, dt: es.enter_context(nc.sbuf_tensor(name, shape, dt))
        rpbT = sb('rpbT', [32, 240], F32)
        dhot = sb('dhot_sb', [32, 4096], F32)
        mst = sb('mst', [120, 4096], F32)
        kb.op('dve', lambda e: e.memset(rpbT[:], 1.0), w=['rpbT'])
        kb.dma('sp', lambda e: e.dma_start(out=rpbT[0:31, :], in_=t.na_rpb.ap().rearrange("h r d -> d (h r)")), w=['rpbT'], r=['rpbT'])
        kb.dma('sp', lambda e: e.dma_start(out=dhot[:], in_=t.dhot_d.ap()), w=['dhot'])
        mv = t.MBT_scr.ap()
        for c in range(2):
            for nb in range(8):
                ps = t.ps[nb % 4]
                kb.op('pe', lambda e: e.matmul(ps[0:120, :], lhsT=rpbT[:, c * 120:(c + 1) * 120], rhs=dhot[:, nb * 512:(nb + 1) * 512],
                                               start=True, stop=True), r=['rpbT', 'dhot'], w=[('ps', nb % 4)])
                kb.op('act', lambda e: e.activation(out=mst[:, nb * 512:(nb + 1) * 512], in_=ps[0:120, :], func=AF.Identity),
                      r=[('ps', nb % 4)], w=['mst'])
            kb.dma('sp', lambda e: e.dma_start(out=mv[c * 120:(c + 1) * 120, :], in_=mst[:]), r=['mst'], w=['MBT_scr'])


def stage_att(kb, t, mergedT):
    nc = kb.nc
    scale = 64 ** -0.5
    with ExitStack() as es:
        sb = lambda name, shape, dt: es.enter_context(nc.sbuf_tensor(name, shape, dt))
        KT = [sb('KTh%d' % i, [64, NTOK], BF16) for i in range(2)]
        QT = [sb('QTh%d' % i, [64, 1024], BF16) for i in range(2)]
        VH = [sb('VHh%d' % i, [64, 40, 65], BF16) for i in range(2)]
        MB = [sb('MBh%d' % i, [64, 15, 64], F32) for i in range(2)]
        rm = sb('rm_sb', [64, 7, 12, 64], F32)
        na_all = sb('na_all', [64, 16, 1024], F32)
        tmpA = [sb('tmpA%d' % i, [64, 512], F32) for i in range(3)]
        tmpB = [sb('tmpB%d' % i, [64, 256], F32) for i in range(3)]
        PTA = [sb('PTA%d' % i, [64, 512], BF16) for i in range(3)]
        PTB = [sb('PTB%d' % i, [64, 512], BF16) for i in range(3)]
        rz = [sb('rz%d' % i, [64, 1], F32) for i in range(3)]
        gna = sb('gna', [64, 1024], F32)
        ssq = sb('ssq', [64, 16], F32)
        sqj = sb('sqj', [64, 1024], F32)
        nrm = [sb('nrm%d' % i, [64, 1024], F32) for i in range(2)]
        kb.dma('sp', lambda e: e.dma_start(out=rm[:], in_=t.rowmask.ap()), w=['rm'])
        kb.dma('sp', lambda e: e.dma_start(out=gna[:], in_=bass.AP(t.na_norm_g, 0, [[0, 64], [1, 1024]])), w=['gna'])
        for i in range(2):
            kb.op('dve', lambda e: e.memset(VH[i][:], 1.0), w=[('VH', i)])
        vv = t.V_scr.ap().rearrange("(r kc) f -> kc r f", kc=64)
        mbv = t.MBT_scr.ap().rearrange("(h r) (kc q) -> h kc r q", r=15, q=64)
        rows = att_rows()
        items = [(h, r) for h in range(16) for r in rows]

        def emit_loads(h):
            hb = h % 2
            kb.dma('sp', lambda e: e.dma_start(out=KT[hb][:], in_=t.KT_scr.ap()[h]), r=['KT_scr'], w=[('KT', hb)])
            kb.dma('sp', lambda e: e.dma_start(out=QT[hb][:], in_=t.QT_scr.ap()[h]), r=['QT_scr'], w=[('QT', hb)])
            kb.dma('sp', lambda e: e.dma_start(out=VH[hb][:, :, 0:64], in_=vv[:, :, h * 64:(h + 1) * 64]), r=['V_scr'], w=[('VH', hb)])
            kb.dma('sp', lambda e: e.dma_start(out=MB[hb][:], in_=mbv[h]), r=['MBT_scr'], w=[('MB', hb)])

        def emit_st(n):
            h, (i, e0, ns, dr0, ci) = items[n]
            hb = h % 2
            nb = n % 3
            if i == 0:
                emit_loads(h)
            psA, psB = t.ps[nb], t.ps[3 + nb]
            q_ap = QT[hb][:, i * 64:(i + 1) * 64]
            rk = [('KT', hb), ('QT', hb)]
            for s_ in range(ns):
                if s_ < 8:
                    dst, dk = psA[0:64, s_ * 64:(s_ + 1) * 64], ('ps', nb)
                else:
                    dst, dk = psB[0:64, (s_ - 8) * 64:(s_ - 7) * 64], ('ps', 3 + nb)
                kb.op('pe', lambda e: e.matmul(dst, lhsT=KT[hb][:, (e0 + s_) * 64:(e0 + s_ + 1) * 64], rhs=q_ap, start=True, stop=True),
                      r=rk, w=[dk])
            for s_ in range(4):
                kb.op('pe', lambda e: e.matmul(psB[0:64, 256 + s_ * 64:256 + (s_ + 1) * 64], lhsT=KT[hb][:, (36 + s_) * 64:(37 + s_) * 64],
                                               rhs=q_ap, start=True, stop=True), r=rk, w=[('ps', 3 + nb)])

        def emit_rest(n):
            h, (i, e0, ns, dr0, ci) = items[n]
            hb = h % 2
            nb = n % 3
            so = n % 4
            ok_ = ('psO', so)
            psA, psB = t.ps[nb], t.ps[3 + nb]
            psO = t.ps[6][:, so * 128:(so + 1) * 128]
            kb.op('dve', lambda e: e.scalar_tensor_tensor(out=tmpA[nb][:], in0=psA[0:64, :], scalar=scale,
                                                          in1=MB[hb][:, dr0:dr0 + 8, :].rearrange("p a b -> p (a b)"),
                                                          op0=ALU.mult, op1=ALU.add), r=[('ps', nb), ('MB', hb)], w=[('tmpA', nb)])
            if ns == 12:
                kb.op('dve', lambda e: e.scalar_tensor_tensor(out=tmpB[nb][:], in0=psB[0:64, 0:256], scalar=scale,
                                                              in1=MB[hb][:, dr0 + 8:dr0 + 12, :].rearrange("p a b -> p (a b)"),
                                                              op0=ALU.mult, op1=ALU.add), r=[('ps', 3 + nb), ('MB', hb)], w=[('tmpB', nb)])
                kb.op('dve', lambda e: e.tensor_tensor(out=tmpA[nb][:], in0=tmpA[nb][:], in1=rm[:, ci, 0:8, :].rearrange("p a b -> p (a b)"),
                                                       op=ALU.add), r=['rm'], w=[('tmpA', nb)])
                kb.op('dve', lambda e: e.tensor_tensor(out=tmpB[nb][:], in0=tmpB[nb][:], in1=rm[:, ci, 8:12, :].rearrange("p a b -> p (a b)"),
                                                       op=ALU.add), r=['rm'], w=[('tmpB', nb)])
            kb.op('act', lambda e: e.activation(out=PTA[nb][:], in_=tmpA[nb][:], func=AF.Exp), r=[('tmpA', nb)], w=[('PTA', nb)])
            if ns == 12:
                kb.op('act', lambda e: e.activation(out=PTB[nb][:, 0:256], in_=tmpB[nb][:], func=AF.Exp), r=[('tmpB', nb)], w=[('PTB', nb)])
            kb.op('act', lambda e: e.activation(out=PTB[nb][:, 256:512], in_=psB[0:64, 256:512], func=AF.Exp, scale=scale),
                  r=[('ps', 3 + nb)], w=[('PTB', nb)])
            tot = ns + 4
            for s_ in range(tot):
                if s_ < 8:
                    lt = PTA[nb][:, s_ * 64:(s_ + 1) * 64]
                    vr = e0 + s_
                elif s_ < ns:
                    lt = PTB[nb][:, (s_ - 8) * 64:(s_ - 7) * 64]
                    vr = e0 + s_
                else:
                    lt = PTB[nb][:, 256 + (s_ - ns) * 64:256 + (s_ - ns + 1) * 64]
                    vr = 36 + (s_ - ns)
                kb.op('pe', lambda e: e.matmul(psO[0:64, 0:65], lhsT=lt, rhs=VH[hb][:, vr, :], start=(s_ == 0), stop=(s_ == tot - 1)),
                      r=[('PTA', nb), ('PTB', nb), ('VH', hb)], w=[ok_])
            kb.op('dve', lambda e: e.reciprocal(out=rz[nb][:], in_=psO[0:64, 64:65]), r=[ok_], w=[('rz', nb)])
            kb.op('act', lambda e: e.activation(out=na_all[:, i, h * 64:(h + 1) * 64], in_=psO[0:64, 0:64], func=AF.Identity,
                                                scale=rz[nb][:, 0:1]), r=[ok_, ('rz', nb)], w=[('na', i)])

        emit_st(0)
        emit_st(1)
        for n in range(len(items)):
            if n + 2 < len(items):
                emit_st(n + 2)
            emit_rest(n)
        if t.debug:
            kb.dma('sp', lambda e: e.dma_start(out=t.na_dbg.ap().rearrange("(i q) f -> q i f", q=64), in_=na_all[:]),
                   r=keys('na', 16), w=['na_dbg'])
        for i in range(16):
            kb.op('act', lambda e: e.activation(out=sqj[:], in_=na_all[:, i, :], func=AF.Square, accum_out=ssq[:, i:i + 1]),
                  r=[('na', i)], w=['sqj', ('ssq', i)])
        kb.op('dve', lambda e: e.tensor_scalar(out=ssq[:], in0=ssq[:], scalar1=1.0 / 1024, scalar2=LN_EPS, op0=ALU.mult, op1=ALU.add),
              r=keys('ssq', 16), w=['ssq2'])
        kb.op('act', lambda e: e.activation(out=ssq[:], in_=ssq[:], func=AF.Sqrt), r=['ssq2'], w=['ssq2'])
        kb.op('dve', lambda e: e.reciprocal(out=ssq[:], in_=ssq[:]), r=['ssq2'], w=['ssq2'])
        for i in range(16):
            nb = i % 2
            kb.op('dve', lambda e: e.scalar_tensor_tensor(out=nrm[nb][:], in0=na_all[:, i, :], scalar=ssq[:, i:i + 1], in1=gna[:],
                                                          op0=ALU.mult, op1=ALU.mult), r=[('na', i), 'ssq2', 'gna'], w=[('nrm', nb)])
            for g in range(2):
                ps = t.ps[g]
                for fc in range(4):
                    kb.op('pe', lambda e: e.transpose(ps[:, fc * 64:(fc + 1) * 64], nrm[nb][:, (g * 4 + fc) * 128:(g * 4 + fc + 1) * 128],
                                                      t.ident[0:64, 0:64]), r=[('nrm', nb), 'ident'], w=[('ps', g)])
                kb.op('act', lambda e: e.activation(out=mergedT[:, 8 + g * 4:8 + (g + 1) * 4, i * 64:(i + 1) * 64],
                                                    in_=ps[:, 0:256].rearrange("p (a b) -> p a b", b=64), func=AF.Identity),
                      r=[('ps', g)], w=['mergedT'])


def stage_filters(kb, t):
    nc = kb.nc
    PI = float(np.pi)
    with ExitStack() as es:
        sb = lambda name, shape, dt: es.enter_context(nc.sbuf_tensor(name, shape, dt))
        zT = sb('zT_sb', [33, 2048], F32)
        w1 = sb('fw1', [33, 64], F32)
        w23 = sb('fw23', [64, 2, 64], F32)
        w4 = sb('fw4', [64, 4096], F32)
        fr = sb('ffr', [64, 3], F32)
        fb = sb('ffb', [64, 3], F32)
        hA = sb('hA', [64, 2048], F32)
        hB = sb('hB', [64, 2048], F32)
        wrp = sb('wrp', [64, 2048], F32)
        ones_c = sb('ones_c', [128, 1], F32)
        ones_r = sb('ones_r', [1, 128], F32)
        nones_b = sb('nones_b', [1, 128], BF16)
        ee = sb('ee2', [1, 2], F32)
        hs = sb('hs', [128, 16, 1024], BF16)
        hd = sb('hd', [128, 16, 1024], BF16)
        Et = [sb('Et%d' % i, [128, 512], F32) for i in range(2)]
        pf = [sb('pf%d' % i, [128, 512], F32) for i in range(2)]
        pb = [sb('pb%d' % i, [128, 512], F32) for i in range(2)]
        af = [sb('af%d' % i, [128, 512], F32) for i in range(2)]
        ab = [sb('ab%d' % i, [128, 512], F32) for i in range(2)]
        bw0 = sb('bw0', [1, 2, 512], F32)
        nb0 = sb('nb0', [1, 2, 512], BF16)
        rcol = sb('rcol', [1, 512], F32)
        rinvb = sb('rinvb', [128, 2, 512], F32)
        wf = [sb('wfA%d' % i, [128, 16, 128], BF16) for i in range(4)]
        kout = [sb('kout%d' % i, [128, 512], F32) for i in range(4)]
        kb.dma('sp', lambda e: e.dma_start(out=zT[:], in_=t.zT_d.ap()), w=['zT'])
        kb.dma('sp', lambda e: e.dma_start(out=w1[:], in_=t.filt_w1.ap()), w=['fw1'])
        kb.dma('sp', lambda e: e.dma_start(out=w23[:, 0, :], in_=t.filt_w2.ap()), w=['fw23'])
        kb.dma('sp', lambda e: e.dma_start(out=w23[:, 1, :], in_=t.filt_w3.ap()), w=['fw23'], r=['fw23'])
        kb.dma('sp', lambda e: e.dma_start(out=w4[:], in_=t.filt_w4.ap()), w=['fw4'])
        kb.dma('sp', lambda e: e.dma_start(out=fr[:], in_=t.filt_freq.ap().rearrange("k j -> j k")), w=['ffr'])
        kb.dma('sp', lambda e: e.dma_start(out=fb[:], in_=t.filt_b.ap().rearrange("k j -> j k")), w=['ffb'])
        kb.dma('sp', lambda e: e.dma_start(out=ee[:], in_=t.e01.ap()), w=['ee2'])
        kb.op('dve', lambda e: e.tensor_tensor(out=fb[:], in0=fb[:], in1=fr[:], op=ALU.mult), r=['ffr', 'ffb'], w=['ffb'])
        kb.op('dve', lambda e: e.memset(ones_c[:], 1.0), w=['ones_c'])
        kb.op('dve', lambda e: e.memset(ones_r[:], 1.0), w=['ones_r'])
        kb.op('dve', lambda e: e.memset(nones_b[:], -1.0), w=['nones_b'])
        src, srck = zT, 'zT'
        for layer in range(3):
            dst, dstk = (hA, 'hA') if layer % 2 == 0 else (hB, 'hB')
            for blk in range(4):
                ps = t.ps[blk]
                lhs = w1[:, :] if layer == 0 else w23[:, layer - 1, :]
                kb.op('pe', lambda e: e.matmul(ps[0:64, :], lhsT=lhs, rhs=src[:, blk * 512:(blk + 1) * 512], start=True, stop=True),
                      r=['fw1', 'fw23', srck], w=[('ps', blk)])
                kb.op('act', lambda e: e.activation(out=dst[:, blk * 512:(blk + 1) * 512], in_=ps[0:64, :], func=AF.Identity,
                                                    scale=fr[:, layer:layer + 1], bias=fb[:, layer:layer + 1]),
                      r=[('ps', blk), 'ffr', 'ffb'], w=[dstk])
            for _ in range(2):
                for (cmp, thr, add) in ((ALU.is_gt, PI, -2 * PI), (ALU.is_lt, -PI, 2 * PI)):
                    kb.op('dve', lambda e: e.tensor_scalar(out=wrp[:], in0=dst[:], scalar1=thr, scalar2=add, op0=cmp, op1=ALU.mult),
                          r=[dstk], w=['wrp'])
                    kb.op('dve', lambda e: e.tensor_tensor(out=dst[:], in0=dst[:], in1=wrp[:], op=ALU.add), r=[dstk, 'wrp'], w=[dstk])
            kb.op('act', lambda e: e.activation(out=dst[:], in_=dst[:], func=AF.Sin), r=[dstk], w=[dstk])
            src, srck = dst, dstk
        h3, h3k = src, srck
        Ev = t.E_d.ap()
        n = 0
        wi = 0
        for o in range(2):
            for hf in range(2):
                hcs = slice(hf * 512, (hf + 1) * 512)
                cs = t.ps[7]
                for tt in range(16):
                    nb = n % 2
                    n += 1
                    psf, psb = t.ps[nb], t.ps[2 + nb]
                    for (ps_, dr, pk) in ((psf, 0, nb), (psb, 1, 2 + nb)):
                        c0 = o * 2048 + dr * 1024 + hf * 512
                        kb.op('pe', lambda e: e.matmul(ps_[:], lhsT=h3[:, tt * 128:(tt + 1) * 128], rhs=w4[:, c0:c0 + 512], start=True, stop=True),
                              r=[h3k, 'fw4'], w=[('ps', pk)])
                    kb.dma('sp', lambda e: e.dma_start(out=Et[nb][:], in_=Ev[tt * 128:(tt + 1) * 128, hcs]), w=[('Et', nb)])
                    kb.op('dve', lambda e: e.tensor_tensor(out=pf[nb][:], in0=psf[:], in1=Et[nb][:], op=ALU.mult), r=[('ps', nb), ('Et', nb)], w=[('pf', nb)])
                    kb.op('dve', lambda e: e.tensor_tensor(out=pb[nb][:], in0=psb[:], in1=Et[nb][:], op=ALU.mult), r=[('ps', 2 + nb), ('Et', nb)], w=[('pb', nb)])
                    kb.op('pool', lambda e: e.tensor_tensor(out=hs[:, tt, hcs], in0=pf[nb][:], in1=pb[nb][:], op=ALU.add), r=[('pf', nb), ('pb', nb)], w=[('hs', hf)])
                    kb.op('pool', lambda e: e.tensor_tensor(out=hd[:, tt, hcs], in0=pf[nb][:], in1=pb[nb][:], op=ALU.subtract), r=[('pf', nb), ('pb', nb)], w=[('hd', hf)])
                    kb.op('act', lambda e: e.activation(out=af[nb][:], in_=pf[nb][:], func=AF.Abs), r=[('pf', nb)], w=[('af', nb)])
                    kb.op('act', lambda e: e.activation(out=ab[nb][:], in_=pb[nb][:], func=AF.Abs), r=[('pb', nb)], w=[('ab', nb)])
                    kb.op('pe', lambda e: e.matmul(cs[0:1, :], lhsT=ones_c[:, :], rhs=af[nb][:], start=(tt == 0), stop=False),
                          r=[('af', nb), 'ones_c'], w=[('ps', 7)])
                    kb.op('pe', lambda e: e.matmul(cs[0:1, :], lhsT=ones_c[:, :], rhs=ab[nb][:], start=False, stop=(tt == 15)),
                          r=[('ab', nb), 'ones_c'], w=[('ps', 7)])
                    if tt in (0, 8):
                        kb.op('dve', lambda e: e.tensor_copy(out=bw0[:, tt // 8, :], in_=pb[nb][0:1, :]), r=[('pb', nb)], w=['bw0'])
                kb.op('dve', lambda e: e.reciprocal(out=rcol[:], in_=cs[0:1, :]), r=[('ps', 7)], w=['rcol'])
                kb.op('dve', lambda e: e.tensor_scalar(out=bw0[:, 0, :], in0=bw0[:, 0, :], scalar1=ee[:, 0:1], scalar2=None, op0=ALU.mult),
                      r=['bw0', 'ee2'], w=['bw0'])
                kb.op('dve', lambda e: e.scalar_tensor_tensor(out=nb0[:, hf, :], in0=bw0[:, 1, :], scalar=ee[:, 1:2], in1=bw0[:, 0, :],
                                                              op0=ALU.mult, op1=ALU.add), r=['bw0', 'ee2'], w=[('nb0', hf)])
                kb.op('pe', lambda e: e.matmul(t.ps[6][:], lhsT=ones_r[:, :], rhs=rcol[:], start=True, stop=True), r=['ones_r', 'rcol'], w=[('ps', 6)])
                kb.op('act', lambda e: e.activation(out=rinvb[:, hf, :], in_=t.ps[6][:], func=AF.Identity), r=[('ps', 6)], w=[('rinvb', hf)])
            kv = t.Kf_scr.ap()[o]
            kn = 0
            for fc in list(range(32)) + [32]:
                real_fc = 16 if fc == 32 else fc
                use_hs = (fc < 16) or (fc == 32)
                wb_ = wf[wi % 4]
                wk = ('wfA', wi % 4)
                wi += 1
                kb.dma('sp' if wi % 2 == 0 else 'pool', lambda e: e.dma_start(out=wb_[:], in_=t.Wf_d.ap()[real_fc]), w=[wk])
                for hf in range(2):
                    hcs = slice(hf * 512, (hf + 1) * 512)
                    p = 4 + (kn % 2)
                    ps = t.ps[p]
                    srcT, srcK = (hs, ('hs', hf)) if use_hs else (hd, ('hd', hf))
                    for k in range(16):
                        kb.op('pe', lambda e: e.matmul(ps[:], lhsT=wb_[:, k, :], rhs=srcT[:, k, hcs], start=(k == 0), stop=(k == 15 and not use_hs)),
                              r=[wk, srcK], w=[('ps', p)])
                    if use_hs:
                        kb.op('pe', lambda e: e.matmul(ps[:], lhsT=nones_b[:, :], rhs=nb0[:, hf, :], start=False, stop=True),
                              r=['nones_b', ('nb0', hf)], w=[('ps', p)])
                    ko, kk = kout[kn % 4], ('kout', kn % 4)
                    kn += 1
                    if fc == 32:
                        kb.op('dve', lambda e: e.tensor_tensor(out=ko[0:1, :], in0=ps[0:1, :], in1=rinvb[0:1, hf, :], op=ALU.mult),
                              r=[('ps', p), ('rinvb', hf)], w=[kk])
                        kb.dma('sp', lambda e: e.dma_start(out=kv[2048:2049, hcs], in_=ko[0:1, :]), r=[kk, ('Kf_scr', 16, hf)], w=[('Kf_scr', 16, hf)])
                    else:
                        kb.op('dve', lambda e: e.tensor_tensor(out=ko[:], in0=ps[:], in1=rinvb[:, hf, :], op=ALU.mult), r=[('ps', p), ('rinvb', hf)], w=[kk])
                        kb.dma('sp', lambda e: e.dma_start(out=kv[fc * 128:(fc + 1) * 128, hcs], in_=ko[:]), r=[kk], w=[('Kf_scr', fc, hf)])
        kb.lastw['Kf_scr'] = None


def stage_conv(kb, t):
    nc = kb.nc
    with ExitStack() as es:
        sb = lambda name, shape, dt: es.enter_context(nc.sbuf_tensor(name, shape, dt))
        vtok = sb('vtok', [128, 16, 1024], BF16)
        Yall = sb('Yall', [128, 32, 1024], BF16)
        skipb = sb('skipb', [128, 2, 1024], F32)
        wf = [sb('wfB%d' % i, [128, 16, 128], BF16) for i in range(4)]
        wi_ = [sb('wiB%d' % i, [128, 32, 128], BF16) for i in range(2)]
        kre = [sb('kre%d' % i, [128, 512], F32) for i in range(2)]
        kim = [sb('kim%d' % i, [128, 512], F32) for i in range(2)]
        vre = [sb('vre%d' % i, [128, 512], F32) for i in range(2)]
        vim = [sb('vim%d' % i, [128, 512], F32) for i in range(2)]
        t1 = [sb('t1_0', [128, 512], F32)] * 2
        t2 = [sb('t2_0', [128, 512], F32)] * 2
        xg = [sb('xgc%d' % i, [128, 512], F32) for i in range(2)]
        aa = [sb('aa%d' % i, [128, 512], F32) for i in range(2)]
        a0 = sb('a0', [1, 512], F32)
        bb = [sb('bbq%d' % i, [128, 512], F32) for i in range(2)]
        hyt = [sb('hyt%d' % i, [128, 512], F32) for i in range(2)]
        hzv = t.hz_tok.ap().rearrange("(tt p) c -> p tt c", p=128)
        for tt in range(16):
            kb.dma('pool', lambda e: e.dma_start(out=vtok[:, tt, :], in_=hzv[:, tt, 0:1024]), r=['hz_tok'], w=[('vtok', tt)])
        kb.dma('sp', lambda e: e.dma_start(out=skipb[:], in_=bass.AP(t.hy_skip, 0, [[0, 128], [1, 2048]])), w=['skipb'])
        wn = 0
        kn = 0
        xn = 0
        for conv in range(2):
            kv = t.Kf_scr.ap()[conv]
            for fc in range(16):
                wbs = []
                for part in range(2):
                    wb_ = wf[wn % 4]
                    wk = ('wfB', wn % 4)
                    kb.dma('sp' if part == 0 else 'pool', lambda e: e.dma_start(out=wb_[:], in_=t.Wf_d.ap()[fc + 16 * part]), w=[wk])
                    wn += 1
                    wbs.append((wb_, wk))
                for cb in range(2):
                    cs = slice(cb * 512, (cb + 1) * 512)
                    q = kn % 2
                    kn += 1
                    pss = []
                    for part in range(2):
                        wb_, wk = wbs[part]
                        p = part * 2 + q
                        ps = t.ps[p]
                        for k in range(16):
                            kb.op('pe', lambda e: e.matmul(ps[:], lhsT=wb_[:, k, :], rhs=vtok[:, k, cs],
                                                           start=(k == 0), stop=(k == 15)), r=[wk, ('vtok', k)], w=[('ps', p)])
                        pss.append((ps, p))
                    kb.dma('sp', lambda e: e.dma_start(out=kre[q][:], in_=kv[fc * 128:(fc + 1) * 128, cs]), r=['Kf_scr'], w=[('kre', q)])
                    kb.dma('sp', lambda e: e.dma_start(out=kim[q][:], in_=kv[2048 + fc * 128:2048 + (fc + 1) * 128, cs]), r=['Kf_scr'], w=[('kim', q)])
                    kb.op('act', lambda e: e.activation(out=vre[q][:], in_=pss[0][0][:], func=AF.Identity), r=[('ps', pss[0][1])], w=[('vre', q)])
                    kb.op('act', lambda e: e.activation(out=vim[q][:], in_=pss[1][0][:], func=AF.Identity), r=[('ps', pss[1][1])], w=[('vim', q)])
                    kb.op('dve', lambda e: e.tensor_tensor(out=t1[q][:], in0=vre[q][:], in1=kre[q][:], op=ALU.mult), r=[('vre', q), ('kre', q)], w=['t1'])
                    kb.op('pool', lambda e: e.tensor_tensor(out=t2[q][:], in0=vim[q][:], in1=kim[q][:], op=ALU.mult), r=[('vim', q), ('kim', q)], w=['t2'])
                    kb.op('dve', lambda e: e.tensor_tensor(out=Yall[:, fc, cs], in0=t1[q][:], in1=t2[q][:], op=ALU.subtract),
                          r=['t1', 't2'], w=[('Y', cb)])
                    if fc == 0:
                        kb.op('dve', lambda e: e.tensor_copy(out=Yall[0:1, 0, cs], in_=t1[q][0:1, :]), r=['t1'], w=[('Y', cb)])
                        kb.op('dve', lambda e: e.tensor_copy(out=a0[0:1, :], in_=t2[q][0:1, :]), r=['t2'], w=['a0'])
                    kb.op('dve', lambda e: e.tensor_tensor(out=t1[q][:], in0=vre[q][:], in1=kim[q][:], op=ALU.mult), r=[('vre', q), ('kim', q)], w=['t1'])
                    kb.op('pool', lambda e: e.tensor_tensor(out=t2[q][:], in0=vim[q][:], in1=kre[q][:], op=ALU.mult), r=[('vim', q), ('kre', q)], w=['t2'])
                    kb.op('dve', lambda e: e.tensor_tensor(out=Yall[:, 16 + fc, cs], in0=t1[q][:], in1=t2[q][:], op=ALU.add),
                          r=['t1', 't2'], w=[('Y', cb)])
                    if fc == 0:
                        kb.op('dve', lambda e: e.tensor_copy(out=Yall[0:1, 16, cs], in_=a0[0:1, :]), r=['a0'], w=[('Y', cb)])
            ntt = 16 if conv == 0 else 8
            for tt in range(ntt):
                wb_ = wi_[tt % 2]
                wk = ('wiB', tt % 2)
                kb.dma('sp' if tt % 2 == 0 else 'pool', lambda e: e.dma_start(out=wb_[:], in_=t.Wi_d.ap()[tt]), w=[wk])
                for cb in range(2):
                    cs = slice(cb * 512, (cb + 1) * 512)
                    q = xn % 2
                    xn += 1
                    xb, xk = xg[q], ('xgc', q)
                    hb_, hk = hyt[q], ('hyt', q)
                    c0 = (1 + conv) * 1024 + cb * 512
                    kb.dma('sp', lambda e: e.dma_start(out=xb[:], in_=t.hz_tok.ap()[tt * 128:(tt + 1) * 128, c0:c0 + 512]),
                           r=['hz_tok'], w=[xk])
                    p = 4 + q
                    ps = t.ps[p]
                    for rc in range(32):
                        kb.op('pe', lambda e: e.matmul(ps[:], lhsT=wb_[:, rc, :], rhs=Yall[:, rc, cs],
                                                       start=(rc == 0), stop=(rc == 31)), r=[wk, ('Y', cb)], w=[('ps', p)])
                    kb.op('pool', lambda e: e.tensor_tensor(out=aa[q][:], in0=vtok[:, tt, cs], in1=skipb[:, conv, cs],
                                                            op=ALU.mult), r=[('vtok', tt), 'skipb'], w=[('aa', q)])
                    kb.op('dve', lambda e: e.tensor_tensor(out=bb[q][:], in0=ps[:], in1=aa[q][:], op=ALU.add), r=[('ps', p), ('aa', q)], w=[('bbq', q)])
                    if conv == 0:
                        kb.op('dve', lambda e: e.tensor_tensor(out=vtok[:, tt, cs], in0=bb[q][:], in1=xb[:],
                                                               op=ALU.mult), r=[('bbq', q), xk], w=[('vtok', tt)])
                    else:
                        kb.op('dve', lambda e: e.tensor_tensor(out=hb_[:], in0=bb[q][:], in1=xb[:], op=ALU.mult), r=[('bbq', q), xk], w=[hk])
                        kb.dma('sp', lambda e: e.dma_start(out=t.hy_scr.ap()[tt * 128:(tt + 1) * 128, cs], in_=hb_[:]), r=[hk], w=[('hy_scr', tt, cb)])


def bcast_row(dt_handle, off, n, parts=128):
    return bass.AP(dt_handle, off, [[0, parts], [1, n]])


def layer_norm_tile(kb, nc, r, rk, st, mv, rs, g, gk, b, bk, tag):
    for k in range(4):
        kb.op('dve', lambda e: e.bn_stats(out=st[:, k, :], in_=r[:, k * 512:(k + 1) * 512]), r=[rk], w=[tag + 'st'])
    kb.op('dve', lambda e: e.bn_aggr(out=mv[:], in_=st[:].rearrange("p a b -> p (a b)")), r=[tag + 'st'], w=[tag + 'mv'])
    kb.op('dve', lambda e: e.tensor_scalar(out=rs[:], in0=mv[:, 1:2], scalar1=LN_EPS, scalar2=None, op0=ALU.add), r=[tag + 'mv'], w=[tag + 'rs'])
    kb.op('act', lambda e: e.activation(out=rs[:], in_=rs[:], func=AF.Sqrt), r=[tag + 'rs'], w=[tag + 'rs'])
    kb.op('dve', lambda e: e.reciprocal(out=rs[:], in_=rs[:]), r=[tag + 'rs'], w=[tag + 'rs'])
    kb.op('dve', lambda e: e.tensor_scalar(out=r[:], in0=r[:], scalar1=mv[:, 0:1], scalar2=rs[:, 0:1], op0=ALU.subtract, op1=ALU.mult),
          r=[rk, tag + 'mv', tag + 'rs'], w=[rk])
    kb.op('pool', lambda e: e.tensor_tensor(out=r[:], in0=r[:], in1=g[:], op=ALU.mult), r=[rk, gk], w=[rk])
    kb.op('dve', lambda e: e.tensor_tensor(out=r[:], in0=r[:], in1=b[:], op=ALU.add), r=[rk, bk], w=[rk])


def stage_merge_a(kb, t, mergedT):
    nc = kb.nc
    with ExitStack() as es:
        sb = lambda name, shape, dt: es.enter_context(nc.sbuf_tensor(name, shape, dt))
        ghy = sb('ghy', [128, 1024], F32)
        hyt = [sb('hym%d' % i, [128, 1024], F32) for i in range(2)]
        sqj = sb('sqj3', [128, 1024], F32)
        nrm = sb('nrm3', [128, 1024], F32)
        ssq = sb('ssq3', [128, 8], F32)
        kb.dma('sp', lambda e: e.dma_start(out=ghy[:], in_=bcast_row(t.hy_norm_g, 0, 1024)), w=['ghy'])
        for tt in range(8):
            hb_, hk = hyt[tt % 2], ('hym', tt % 2)
            kb.dma('sp', lambda e: e.dma_start(out=hb_[:], in_=t.hy_scr.ap()[tt * 128:(tt + 1) * 128, :]), r=['hy_scr'], w=[hk])
            kb.op('act', lambda e: e.activation(out=sqj[:], in_=hb_[:], func=AF.Square, accum_out=ssq[:, tt:tt + 1]), r=[hk], w=['sqj3', ('ssq3', tt)])
            kb.op('dve', lambda e: e.tensor_scalar(out=ssq[:, tt:tt + 1], in0=ssq[:, tt:tt + 1], scalar1=1.0 / 1024, scalar2=LN_EPS, op0=ALU.mult, op1=ALU.add),
                  r=[('ssq3', tt)], w=[('ssq3', tt)])
            kb.op('act', lambda e: e.activation(out=ssq[:, tt:tt + 1], in_=ssq[:, tt:tt + 1], func=AF.Sqrt), r=[('ssq3', tt)], w=[('ssq3', tt)])
            kb.op('dve', lambda e: e.reciprocal(out=ssq[:, tt:tt + 1], in_=ssq[:, tt:tt + 1]), r=[('ssq3', tt)], w=[('ssq3', tt)])
            kb.op('dve', lambda e: e.scalar_tensor_tensor(out=nrm[:], in0=hb_[:], scalar=ssq[:, tt:tt + 1], in1=ghy[:], op0=ALU.mult, op1=ALU.mult),
                  r=[hk, ('ssq3', tt), 'ghy'], w=['nrm3'])
            for g in range(2):
                ps = t.ps[6 + g]
                for fc in range(4):
                    kb.op('pe', lambda e: e.transpose(ps[:, fc * 128:(fc + 1) * 128], nrm[:, (g * 4 + fc) * 128:(g * 4 + fc + 1) * 128], t.ident[:]),
                          r=['nrm3', 'ident'], w=[('ps', 6 + g)])
                kb.op('act', lambda e: e.activation(out=mergedT[:, g * 4:(g + 1) * 4, tt * 128:(tt + 1) * 128],
                                                    in_=ps[:].rearrange("p (a b) -> p a b", b=128), func=AF.Identity),
                      r=[('ps', 6 + g)], w=['mergedT'])


def stage_merge_b(kb, t, mergedT):
    nc = kb.nc
    with ExitStack() as es:
        sb = lambda name, shape, dt: es.enter_context(nc.sbuf_tensor(name, shape, dt))
        wo = sb('wo', [128, 16, 2048], BF16)
        g1b = sb('g1b', [128, 2048], F32)
        l1g = sb('l1g', [128, 2048], F32)
        l1b = sb('l1b', [128, 2048], F32)
        s2p = sb('s2p', [128, 2048], F32)
        h2b = sb('h2b', [128, 2048], F32)
        xt = [sb('xtm%d' % i, [128, 2048], F32) for i in range(2)]
        rr = [sb('rrm%d' % i, [128, 2048], F32) for i in range(2)]
        st = sb('st1', [128, 4, 6], F32)
        mv = sb('mv1', [128, 2], F32)
        rs = sb('rs1', [128, 1], F32)
        wv = t.w_out.ap().rearrange("(k p) n -> p k n", p=128)
        for cb in range(4):
            kb.dma('pool', lambda e: e.dma_start(out=wo[:, :, cb * 512:(cb + 1) * 512], in_=wv[:, :, cb * 512:(cb + 1) * 512]), w=[('wo', cb)])
        kb.dma('sp', lambda e: e.dma_start(out=g1b[:], in_=bcast_row(t.m_scr, 2 * D, D)), r=['m_scr'], w=['g1b'])
        kb.dma('sp', lambda e: e.dma_start(out=s2p[:], in_=bcast_row(t.m_scr, 4 * D, D)), r=['m_scr'], w=['s2p'])
        kb.dma('sp', lambda e: e.dma_start(out=h2b[:], in_=bcast_row(t.m_scr, 3 * D, D)), r=['m_scr'], w=['h2b'])
        kb.dma('sp', lambda e: e.dma_start(out=l1g[:], in_=bcast_row(t.ln1_g, 0, D)), w=['l1g'])
        kb.dma('sp', lambda e: e.dma_start(out=l1b[:], in_=bcast_row(t.ln1_b, 0, D)), w=['l1b'])
        kb.op('dve', lambda e: e.tensor_scalar(out=s2p[:], in0=s2p[:], scalar1=1.0, scalar2=None, op0=ALU.add), r=['s2p'], w=['s2p'])
        for tt in range(8):
            xb, xk = xt[tt % 2], ('xtm', tt % 2)
            r, rk = rr[tt % 2], ('rrm', tt % 2)
            kb.dma('sp', lambda e: e.dma_start(out=xb[:], in_=t.xr.ap()[tt * 128:(tt + 1) * 128, :]), w=[xk])
            for cb in range(4):
                ps = t.ps[cb]
                for k in range(16):
                    kb.op('pe', lambda e: e.matmul(ps[:], lhsT=mergedT[:, k, tt * 128:(tt + 1) * 128], rhs=wo[:, k, cb * 512:(cb + 1) * 512],
                                                   start=(k == 0), stop=(k == 15)), r=['mergedT', ('wo', cb)], w=[('ps', cb)])
                kb.op('dve', lambda e: e.tensor_tensor(out=r[:, cb * 512:(cb + 1) * 512], in0=ps[:], in1=g1b[:, cb * 512:(cb + 1) * 512], op=ALU.mult),
                      r=[('ps', cb), 'g1b'], w=[rk])
            kb.op('dve', lambda e: e.scalar_tensor_tensor(out=r[:], in0=xb[:], scalar=ALPHA, in1=r[:], op0=ALU.mult, op1=ALU.add), r=[xk, rk], w=[rk])
            layer_norm_tile(kb, nc, r, rk, st, mv, rs, l1g, 'l1g', l1b, 'l1b', 'ln1')
            kb.dma('sp', lambda e: e.dma_start(out=t.x1_scr.ap()[tt * 128:(tt + 1) * 128, :], in_=r[:]), r=[rk], w=['x1_scr'])
            kb.op('pool', lambda e: e.tensor_tensor(out=r[:], in0=r[:], in1=s2p[:], op=ALU.mult), r=[rk, 's2p'], w=[rk])
            kb.op('dve', lambda e: e.tensor_tensor(out=r[:], in0=r[:], in1=h2b[:], op=ALU.add), r=[rk, 'h2b'], w=[rk])
            kb.dma('sp', lambda e: e.dma_start(out=t.u2_scr.ap()[tt * 128:(tt + 1) * 128, :], in_=r[:]), r=[rk], w=['u2_scr'])


def stage_peer_q(kb, t, u2T):
    nc = kb.nc
    with ExitStack() as es:
        sb = lambda name, shape, dt: es.enter_context(nc.sbuf_tensor(name, shape, dt))
        ut = [sb('utq%d' % i, [128, 2048], F32) for i in range(2)]
        wq = [sb('wq%d' % i, [128, 16, 512], BF16) for i in range(2)]
        qs = [sb('qs%d' % i, [128, LH], F32) for i in range(2)]
        pi = 0
        for tt in range(8):
            ub, uk = ut[tt % 2], ('utq', tt % 2)
            kb.dma('sp', lambda e: e.dma_start(out=ub[:], in_=t.u2_scr.ap()[tt * 128:(tt + 1) * 128, :]), r=['u2_scr'], w=[uk])
            for g in range(4):
                p = pi % 4
                pi += 1
                ps = t.ps[p]
                for i in range(4):
                    k = g * 4 + i
                    kb.op('pe', lambda e: e.transpose(ps[:, i * 128:(i + 1) * 128], ub[:, k * 128:(k + 1) * 128], t.ident[:]),
                          r=[uk, 'ident'], w=[('ps', p)])
                kb.op('act', lambda e: e.activation(out=u2T[:, g * 4:(g + 1) * 4, tt * 128:(tt + 1) * 128],
                                                    in_=ps[:].rearrange("p (a b) -> p a b", b=128), func=AF.Identity),
                      r=[('ps', p)], w=['u2T'])
        wv = t.peer_wq.ap().rearrange("(k p) n -> p k n", p=128)
        for blk in range(4):
            wb_, wk = wq[blk % 2], ('wq', blk % 2)
            kb.dma('pool', lambda e: e.dma_start(out=wb_[:], in_=wv[:, :, blk * 512:(blk + 1) * 512]), w=[wk])
            for cc in range(4):
                c = blk * 4 + cc
                qb, qk = qs[c % 2], ('qs', c % 2)
                for tb in range(2):
                    p = 4 + tb
                    ps = t.ps[p]
                    for k in range(16):
                        kb.op('pe', lambda e: e.matmul(ps[:], lhsT=wb_[:, k, cc * 128:(cc + 1) * 128], rhs=u2T[:, k, tb * 512:(tb + 1) * 512],
                                                       start=(k == 0), stop=(k == 15)), r=[wk, 'u2T'], w=[('ps', p)])
                    kb.op('act', lambda e: e.activation(out=qb[:, tb * 512:(tb + 1) * 512], in_=ps[:], func=AF.Identity), r=[('ps', p)], w=[qk])
                kb.dma('sp', lambda e: e.dma_start(out=t.qT_scr.ap()[c], in_=qb[:]), r=[qk], w=['qT_scr'])


def stage_peer(kb, t, u2T):
    nc = kb.nc
    pst = lambda a: list(a.ap[0])
    with ExitStack() as es0:
        with ExitStack() as es:
            sb = lambda name, shape, dt: es.enter_context(nc.sbuf_tensor(name, shape, dt))
            keysT = sb('keysT', [128, 16, 128], F32)
            kld = sb('kld', [128, 16, 128], F32)
            iota16 = sb('iota16_sb', [128, 16], F32)
            iota128 = sb('iota128_sb', [128, 128], F32)
            qTt = sb('qTt', [128, 16, 128], F32)
            S = sb('S', [128, 16, 128], F32)
            S2 = sb('S2', [128, 16, 128], F32)
            V16 = sb('V16', [128, 16, 16], F32)
            I16 = sb('I16', [128, 16, 16], U32)
            I16f = sb('I16f', [128, 16, 16], F32)
            cand = sb('cand', [128, 8, 256], F32)
            cand2 = sb('cand2', [128, 8, 256], F32)
            TS = sb('TS', [128, 8, 16], F32)
            P16 = sb('P16', [128, 8, 16], U32)
            Pi = sb('Pi', [128, 8, 16], U32)
            Pj = sb('Pj', [128, 8, 16], U32)
            Pif = sb('Pif', [128, 8, 16], F32)
            Pjf = sb('Pjf', [128, 8, 16], F32)
            eq = sb('eq', [128, 8, 16, 16], F32)
            ia = sb('ia', [128, 8, 16], F32)
            ib = sb('ib', [128, 8, 16], F32)
            nmax = sb('nmax', [128, 8], F32)
            Gt = sb('Gt', [128, 8, 16], F32)
            Z = sb('Z', [128, 8], F32)
            tr3 = sb('tr3', [128, 3, 128], F32)
            At = [sb('At%d' % i, [128, 128], F32) for i in range(4)]
            Bt = [sb('Bt%d' % i, [128, 2, 128], F32) for i in range(4)]
            Gst = [sb('Gst%d' % i, [128, 128, 128], BF16) for i in range(2)]
            kb.dma('sp', lambda e: e.dma_start(out=iota16[:], in_=t.iota16_d.ap()), w=['iota16'])
            kb.dma('sp', lambda e: e.dma_start(out=iota128[:], in_=t.iota128_d.ap()), w=['iota128'])
            kb.dma('sp', lambda e: e.dma_start(out=kld[:], in_=t.peer_keys.ap().rearrange("c n d -> n c d")), w=['kld'])
            for g in range(4):
                ps = t.ps[g]
                for i in range(4):
                    c = g * 4 + i
                    kb.op('pe', lambda e: e.transpose(ps[:, i * 128:(i + 1) * 128], kld[:, c, :], t.ident[:]), r=['kld', 'ident'], w=[('ps', g)])
                kb.op('act', lambda e: e.activation(out=keysT[:, g * 4:(g + 1) * 4, :], in_=ps[:].rearrange("p (a b) -> p a b", b=128), func=AF.Identity),
                      r=[('ps', g)], w=['keysT'])
            gsv = t.G_scr.ap().rearrange("a b n -> b a n")
            gq = 0
            for tt in range(8):
                kb.dma('sp', lambda e: e.dma_start(out=qTt[:], in_=t.qT_scr.ap()[:, :, tt * 128:(tt + 1) * 128].rearrange("c d n -> d c n")),
                       r=['qT_scr'], w=['qTt'])
                for g in range(4):
                    ps = t.ps[4 + g]
                    for i in range(4):
                        c = g * 4 + i
                        kb.op('pe', lambda e: e.matmul(ps[:, i * 128:(i + 1) * 128], lhsT=qTt[:, c, :], rhs=keysT[:, c, :], start=True, stop=True),
                              r=['qTt', 'keysT'], w=[('ps', 4 + g)])
                    kb.op('act', lambda e: e.activation(out=S[:, g * 4:(g + 1) * 4, :], in_=ps[:].rearrange("p (a b) -> p a b", b=128), func=AF.Identity),
                          r=[('ps', 4 + g)], w=['S'])
                for c in range(16):
                    kb.op('dve', lambda e: e.max(out=V16[:, c, 0:8], in_=S[:, c, :]), r=['S'], w=['V16'])
                    kb.op('dve', lambda e: e.max_index(out=I16[:, c, 0:8], in_max=V16[:, c, 0:8], in_values=S[:, c, :]), r=['S', 'V16'], w=['I16'])
                    kb.op('dve', lambda e: e.match_replace(out=S2[:, c, :], in_to_replace=V16[:, c, 0:8], in_values=S[:, c, :], imm_value=-1e30),
                          r=['S', 'V16'], w=['S2'])
                    kb.op('dve', lambda e: e.max(out=V16[:, c, 8:16], in_=S2[:, c, :]), r=['S2'], w=['V16'])
                    kb.op('dve', lambda e: e.max_index(out=I16[:, c, 8:16], in_max=V16[:, c, 8:16], in_values=S2[:, c, :]), r=['S2', 'V16'], w=['I16'])
                kb.op('dve', lambda e: e.tensor_copy(out=I16f[:], in_=I16[:]), r=['I16'], w=['I16f'])
                va = bass.AP(V16[:].tensor, V16[:].offset, [pst(V16[:]), [32, 8], [1, 16], [0, 16]])
                vb = bass.AP(V16[:].tensor, V16[:].offset + 16, [pst(V16[:]), [32, 8], [0, 16], [1, 16]])
                kb.op('dve', lambda e: e.tensor_tensor(out=cand[:].rearrange("p h (i j) -> p h i j", j=16), in0=va, in1=vb, op=ALU.add),
                      r=['V16'], w=['cand'])
                for h in range(8):
                    kb.op('dve', lambda e: e.max(out=TS[:, h, 0:8], in_=cand[:, h, :]), r=['cand'], w=['TS'])
                    kb.op('dve', lambda e: e.max_index(out=P16[:, h, 0:8], in_max=TS[:, h, 0:8], in_values=cand[:, h, :]), r=['cand', 'TS'], w=['P16'])
                    kb.op('dve', lambda e: e.match_replace(out=cand2[:, h, :], in_to_replace=TS[:, h, 0:8], in_values=cand[:, h, :], imm_value=-1e30),
                          r=['cand', 'TS'], w=['cand2'])
                    kb.op('dve', lambda e: e.max(out=TS[:, h, 8:16], in_=cand2[:, h, :]), r=['cand2'], w=['TS'])
                    kb.op('dve', lambda e: e.max_index(out=P16[:, h, 8:16], in_max=TS[:, h, 8:16], in_values=cand2[:, h, :]), r=['cand2', 'TS'], w=['P16'])
                kb.op('dve', lambda e: e.tensor_single_scalar(out=Pi[:], in_=P16[:], scalar=4, op=ALU.logical_shift_right), r=['P16'], w=['Pi'])
                kb.op('dve', lambda e: e.tensor_single_scalar(out=Pj[:], in_=P16[:], scalar=15, op=ALU.bitwise_and), r=['P16'], w=['Pj'])
                kb.op('dve', lambda e: e.tensor_copy(out=Pif[:], in_=Pi[:]), r=['Pi'], w=['Pif'])
                kb.op('dve', lambda e: e.tensor_copy(out=Pjf[:], in_=Pj[:]), r=['Pj'], w=['Pjf'])
                io = bass.AP(iota16[:].tensor, iota16[:].offset, [pst(iota16[:]), [0, 8], [0, 16], [1, 16]])
                for (Pf, pk, off, dst, dk) in ((Pif, 'Pif', 0, ia, 'ia'), (Pjf, 'Pjf', 16, ib, 'ib')):
                    pfb = bass.AP(Pf[:].tensor, Pf[:].offset, [pst(Pf[:]), [16, 8], [1, 16], [0, 16]])
                    ifb = bass.AP(I16f[:].tensor, I16f[:].offset + off, [pst(I16f[:]), [32, 8], [0, 16], [1, 16]])
                    kb.op('dve', lambda e: e.tensor_tensor(out=eq[:], in0=io, in1=pfb, op=ALU.is_equal), r=['iota16', pk], w=['eq'])
                    kb.op('dve', lambda e: e.tensor_tensor(out=eq[:], in0=eq[:], in1=ifb, op=ALU.mult), r=['eq', 'I16f'], w=['eq'])
                    kb.op('dve', lambda e: e.tensor_reduce(out=dst[:], in_=eq[:], axis=AX.X, op=ALU.add), r=['eq'], w=[dk])
                kb.op('dve', lambda e: e.tensor_scalar(out=nmax[:], in0=TS[:, :, 0], scalar1=-1.0, scalar2=None, op0=ALU.mult), r=['TS'], w=['nmax'])
                for h in range(8):
                    kb.op('act', lambda e: e.activation(out=Gt[:, h, :], in_=TS[:, h, :], func=AF.Exp, bias=nmax[:, h:h + 1], accum_out=Z[:, h:h + 1]),
                          r=['TS', 'nmax'], w=['Gt', 'Z'])
                kb.op('dve', lambda e: e.reciprocal(out=Z[:], in_=Z[:]), r=['Z'], w=['Z'])
                zb = bass.AP(Z[:].tensor, Z[:].offset, [pst(Z[:]), [1, 8], [0, 16]])
                kb.op('dve', lambda e: e.tensor_tensor(out=Gt[:], in0=Gt[:], in1=zb, op=ALU.mult), r=['Gt', 'Z'], w=['Gt'])
                psx = t.ps[0]
                for i, (src, sk) in enumerate(((ia, 'ia'), (ib, 'ib'), (Gt, 'Gt'))):
                    kb.op('pe', lambda e: e.transpose(psx[:, i * 128:(i + 1) * 128], src[:].rearrange("p h k -> p (h k)"), t.ident[:]),
                          r=[sk, 'ident'], w=[('ps', 0)])
                kb.op('act', lambda e: e.activation(out=tr3[:], in_=psx[:, 0:384].rearrange("p (a b) -> p a b", b=128), func=AF.Identity),
                      r=[('ps', 0)], w=['tr3'])
                gs_, gk = Gst[tt % 2], ('Gst', tt % 2)
                for tok in range(128):
                    ab = tok % 4
                    ohk = ('Bt', ab)
                    in1 = bass.AP(tr3[:].tensor, tr3[:].offset + tok, [pst(tr3[:]), [128, 2], [0, 128]])
                    in0 = bass.AP(iota128[:].tensor, iota128[:].offset, [pst(iota128[:]), [0, 2], [1, 128]])
                    kb.op('dve', lambda e: e.tensor_tensor(out=Bt[ab][:], in0=in0, in1=in1, op=ALU.is_equal), r=['iota128', 'tr3'], w=[ohk])
                    kb.op('act', lambda e: e.activation(out=At[ab][:], in_=Bt[ab][:, 0, :], func=AF.Copy, scale=tr3[:, 2, tok:tok + 1]),
                          r=[ohk, 'tr3'], w=[('At', ab)])
                    pg = 1 + (gq % 2)
                    psg = t.ps[pg]
                    oap = bass.AP(psg[:].tensor, psg[:].offset + ab, [pst(psg[:]), [4, 128]])
                    kb.op('pe', lambda e: e.matmul(oap, lhsT=Bt[ab][:, 1, :], rhs=At[ab][:], start=True, stop=True),
                          r=[('At', ab), ('Bt', ab)], w=[('ps', pg)])
                    if ab == 3:
                        t4 = tok // 4
                        kb.op('act', lambda e: e.activation(out=gs_[:, :, t4 * 4:(t4 + 1) * 4], in_=psg[:].rearrange("p (a b) -> p a b", b=4),
                                                            func=AF.Identity), r=[('ps', pg)], w=[gk])
                        gq += 1
                kb.dma('sp', lambda e: e.dma_start(out=gsv[:, :, tt * 128:(tt + 1) * 128], in_=gs_[:]), r=[gk], w=['G_scr'])
        kb.barrier()
        acc = es0.enter_context(nc.sbuf_tensor('acc', [128, 8, 2048], F32))
        GRP = 4
        with ExitStack() as es:
            sb = lambda name, shape, dt: es.enter_context(nc.sbuf_tensor(name, shape, dt))
            GA = [sb('GA%d' % i, [128, GRP, 1024], BF16) for i in range(2)]
            Vb = [sb('Vb%d' % i, [128, GRP, 2048], BF16) for i in range(2)]
            Ur = [sb('Ur%d' % i, [128, 2048], F32) for i in range(3)]
            UT = [sb('UT%d' % i, [128, 16, 128], BF16) for i in range(2)]
            gsb = [sb('gsb%d' % i, [128, 1024], BF16) for i in range(2)]
            Gl = [sb('Gl%d' % i, [128, 1024], BF16) for i in range(2)]
            state = {'n': 0, 'ev': 0}

            def emit_scores(grp):
                gb_ = grp % 2
                for ai in range(GRP):
                    a = grp * GRP + ai
                    nb = state['n'] % 3
                    state['n'] += 1
                    kb.dma('sp', lambda e: e.dma_start(out=Ur[nb][:], in_=t.peer_u.ap()[a * 128:(a + 1) * 128, :]), w=[('Ur', nb)])
                    kb.dma('pool', lambda e: e.dma_start(out=Vb[gb_][:, ai, :], in_=t.peer_v.ap()[a * 128:(a + 1) * 128, :]), w=[('Vb', gb_)])
                    kb.dma('sp', lambda e: e.dma_start(out=Gl[nb % 2][:], in_=t.G_scr.ap()[a]), r=['G_scr'], w=[('Gl', nb % 2)])
                    for g in range(4):
                        p = 4 + g
                        ps = t.ps[p]
                        for i in range(4):
                            k = g * 4 + i
                            kb.op('pe', lambda e: e.transpose(ps[:, i * 128:(i + 1) * 128], Ur[nb][:, k * 128:(k + 1) * 128], t.ident[:]),
                                  r=[('Ur', nb), 'ident'], w=[('ps', p)])
                        eng = 'act' if state['ev'] % 2 == 0 else 'dve'
                        state['ev'] += 1
                        if eng == 'act':
                            kb.op('act', lambda e: e.activation(out=UT[nb % 2][:, g * 4:(g + 1) * 4, :], in_=ps[:].rearrange("p (a b) -> p a b", b=128),
                                                                func=AF.Identity), r=[('ps', p)], w=[('UT', nb % 2)])
                        else:
                            kb.op('dve', lambda e: e.tensor_copy(out=UT[nb % 2][:, g * 4:(g + 1) * 4, :], in_=ps[:].rearrange("p (a b) -> p a b", b=128)),
                                  r=[('ps', p)], w=[('UT', nb % 2)])
                    for tb in range(2):
                        p = 2 + tb
                        ps = t.ps[p]
                        for k in range(16):
                            kb.op('pe', lambda e: e.matmul(ps[:], lhsT=UT[nb % 2][:, k, :], rhs=u2T[:, k, tb * 512:(tb + 1) * 512],
                                                           start=(k == 0), stop=(k == 15)), r=[('UT', nb % 2), 'u2T'], w=[('ps', p)])
                        kb.op('act', lambda e: e.activation(out=gsb[nb % 2][:, tb * 512:(tb + 1) * 512], in_=ps[:], func=AF.Gelu),
                              r=[('ps', p)], w=[('gsb', nb % 2)])
                    kb.op('dve', lambda e: e.tensor_tensor(out=GA[gb_][:, ai, :], in0=gsb[nb % 2][:], in1=Gl[nb % 2][:], op=ALU.mult),
                          r=[('gsb', nb % 2), ('Gl', nb % 2)], w=[('GA', gb_)])

            def emit_out(grp):
                gb_ = grp % 2
                for tt in range(8):
                    for db in range(4):
                        p = (tt * 4 + db) % 2
                        ps = t.ps[p]
                        for ai in range(GRP):
                            kb.op('pe', lambda e: e.matmul(ps[:], lhsT=GA[gb_][:, ai, tt * 128:(tt + 1) * 128], rhs=Vb[gb_][:, ai, db * 512:(db + 1) * 512],
                                                           start=(ai == 0), stop=(ai == GRP - 1)), r=[('GA', gb_), ('Vb', gb_)], w=[('ps', p)])
                        ak = ('acc', tt * 4 + db)
                        if grp == 0:
                            kb.op('act', lambda e: e.activation(out=acc[:, tt, db * 512:(db + 1) * 512], in_=ps[:], func=AF.Identity), r=[('ps', p)], w=[ak])
                        else:
                            kb.op('dve', lambda e: e.tensor_tensor(out=acc[:, tt, db * 512:(db + 1) * 512], in0=ps[:], in1=acc[:, tt, db * 512:(db + 1) * 512],
                                                                   op=ALU.add), r=[('ps', p), ak], w=[ak])

            NG = 128 // GRP
            emit_scores(0)
            for grp in range(NG):
                if grp + 1 < NG:
                    emit_scores(grp + 1)
                emit_out(grp)
        kb.barrier()
        with ExitStack() as es:
            sb = lambda name, shape, dt: es.enter_context(nc.sbuf_tensor(name, shape, dt))
            g2b = sb('g2b', [128, 2048], F32)
            l2g = sb('l2g', [128, 2048], F32)
            l2b = sb('l2b', [128, 2048], F32)
            x1t = [sb('x1t%d' % i, [128, 2048], F32) for i in range(2)]
            rr = [sb('rr2_%d' % i, [128, 2048], F32) for i in range(2)]
            st = sb('st2', [128, 4, 6], F32)
            mv = sb('mv2', [128, 2], F32)
            rs = sb('rs2', [128, 1], F32)
            kb.dma('sp', lambda e: e.dma_start(out=g2b[:], in_=bcast_row(t.m_scr, 5 * D, D)), r=['m_scr'], w=['g2b'])
            kb.dma('sp', lambda e: e.dma_start(out=l2g[:], in_=bcast_row(t.ln2_g, 0, D)), w=['l2g'])
            kb.dma('sp', lambda e: e.dma_start(out=l2b[:], in_=bcast_row(t.ln2_b, 0, D)), w=['l2b'])
            AK = keys('acc', 32)
            for tt in range(8):
                xb, xk = x1t[tt % 2], ('x1t', tt % 2)
                r, rk = rr[tt % 2], ('rr2', tt % 2)
                kb.dma('sp', lambda e: e.dma_start(out=xb[:], in_=t.x1_scr.ap()[tt * 128:(tt + 1) * 128, :]), r=['x1_scr'], w=[xk])
                kb.op('pool', lambda e: e.tensor_tensor(out=r[:], in0=acc[:, tt, :], in1=g2b[:], op=ALU.mult), r=AK + ['g2b'], w=[rk])
                kb.op('dve', lambda e: e.scalar_tensor_tensor(out=r[:], in0=xb[:], scalar=ALPHA, in1=r[:], op0=ALU.mult, op1=ALU.add), r=[xk, rk], w=[rk])
                layer_norm_tile(kb, nc, r, rk, st, mv, rs, l2g, 'l2g', l2b, 'l2b', 'ln2')
                kb.dma('sp', lambda e: e.dma_start(out=t.out.ap()[tt * 128:(tt + 1) * 128, :], in_=r[:]), r=[rk], w=['out'])


ALL_STAGES = ('ada', 'uT', 'proj', 'bias', 'att', 'filt', 'conv', 'merge', 'peer')


def build_program(stages=ALL_STAGES, debug=False):
    nc = bass.Bass("TRN2", target_bir_lowering=False)
    t = T()
    t.debug = debug
    dt_in = lambda name, shape, dt=F32: nc.dram_tensor(name, shape, dt, kind="ExternalInput")
    scr_kind = "ExternalOutput" if debug else "Internal"
    dt_scr = lambda name, shape, dt=F32: nc.dram_tensor(name, shape, dt, kind=scr_kind)
    t.xr = dt_in('xr', [L, D])
    t.c2 = dt_in('c2', [2, D])
    t.ctxb = dt_in('ctxb', [CTX, D])
    t.w_ada = dt_in('w_ada', [D, 6 * D])
    t.b_ada = dt_in('b_ada', [1, 6 * D])
    t.w_in = dt_in('w_in', [D, 6144])
    t.conv_w = dt_in('conv_w', [3, 3072])
    t.conv_b = dt_in('conv_b', [1, 3072])
    t.e01 = dt_in('e01', [1, 2])
    t.ident_d = dt_in('ident', [128, 128])
    t.na_rpb = dt_in('na_rpb', [16, 15, 31])
    t.dhot_d = dt_in('dhot', [32, 4096])
    t.rowmask = dt_in('rowmask', [64, 7, 12, 64])
    t.na_norm_g = dt_in('na_norm_g', [1, 1024])
    t.zT_d = dt_in('zT', [33, 2048])
    t.E_d = dt_in('Edec', [L, 1024])
    t.Wf_d = dt_in('Wf', [32, 128, 16, 128], BF16)
    t.Wi_d = dt_in('Wi', [16, 128, 32, 128], BF16)
    t.filt_w1 = dt_in('filt_w1', [33, 64])
    t.filt_w2 = dt_in('filt_w2', [64, 64])
    t.filt_w3 = dt_in('filt_w3', [64, 64])
    t.filt_w4 = dt_in('filt_w4', [64, 4096])
    t.filt_b = dt_in('filt_b', [3, 64])
    t.filt_freq = dt_in('filt_freq', [3, 64])
    t.hy_skip = dt_in('hy_skip', [1, 2048])
    t.hy_norm_g = dt_in('hy_norm_g', [1, 1024])
    t.w_out = dt_in('w_out', [D, D])
    t.ln1_g = dt_in('ln1_g', [1, D])
    t.ln1_b = dt_in('ln1_b', [1, D])
    t.ln2_g = dt_in('ln2_g', [1, D])
    t.ln2_b = dt_in('ln2_b', [1, D])
    t.peer_wq = dt_in('peer_wq', [D, D])
    t.peer_keys = dt_in('peer_keys', [16, 128, 128])
    t.peer_u = dt_in('peer_u', [16384, D])
    t.peer_v = dt_in('peer_v', [16384, D])
    t.iota16_d = dt_in('iota16', [128, 16])
    t.iota128_d = dt_in('iota128', [128, 128])
    t.out = nc.dram_tensor('out', [LH, D], F32, kind="ExternalOutput")
    t.m_scr = dt_scr('m_scr', [2, 6 * D])
    t.hz_tok = dt_scr('hz_tok', [L, 3072])
    t.QT_scr = dt_scr('QT_scr', [16, 64, 1024], BF16)
    t.KT_scr = dt_scr('KT_scr', [16, 64, NTOK], BF16)
    t.V_scr = dt_scr('V_scr', [NTOK, 1024], BF16)
    t.MBT_scr = dt_scr('MBT_scr', [240, 4096])
    t.Kf_scr = dt_scr('Kf_scr', [2, 4096, 1024])
    t.hy_scr = dt_scr('hy_scr', [LH, 1024])
    t.x1_scr = dt_scr('x1_scr', [LH, D])
    t.u2_scr = dt_scr('u2_scr', [LH, D])
    t.qT_scr = dt_scr('qT_scr', [16, 128, LH])
    t.G_scr = dt_scr('G_scr', [128, 128, LH], BF16)
    if debug:
        t.na_dbg = dt_scr('na_dbg', [LH, 1024])
    with ExitStack() as es:
        es.enter_context(nc.allow_non_contiguous_dma(reason="small strided parameter loads"))
        es.enter_context(nc.allow_low_precision(reason="bf16 matmul operands"))
        kb = KB(nc, es)
        t.ps = [es.enter_context(nc.psum_tensor('ps%d' % i, [128, 512], F32)) for i in range(8)]
        t.ident = es.enter_context(nc.sbuf_tensor('ident_sb', [128, 128], F32))
        kb.dma('sp', lambda e: e.dma_start(out=t.ident[:], in_=t.ident_d.ap()), w=['ident'])
        if 'ada' in stages:
            stage_ada(kb, t)
        kb.barrier()
        with ExitStack() as es2:
            uT = es2.enter_context(nc.sbuf_tensor('uT', [128, 16, NTOK], BF16))
            if 'uT' in stages:
                stage_uT(kb, t, uT)
                kb.barrier()
            if 'proj' in stages:
                stage_proj(kb, t, uT)
        kb.barrier()
        if 'bias' in stages:
            stage_bias(kb, t)
        kb.barrier()
        with ExitStack() as es3:
            mergedT = es3.enter_context(nc.sbuf_tensor('mergedT', [128, 16, LH], BF16))
            if 'att' in stages:
                stage_att(kb, t, mergedT)
            kb.barrier()
            if 'filt' in stages:
                stage_filters(kb, t)
            kb.barrier()
            if 'conv' in stages:
                stage_conv(kb, t)
            kb.barrier()
            if 'merge' in stages:
                stage_merge_a(kb, t, mergedT)
                kb.barrier()
                stage_merge_b(kb, t, mergedT)
                kb.barrier()
        u2T = es.enter_context(nc.sbuf_tensor('u2T', [128, 16, LH], BF16))
        if 'merge' in stages:
            stage_peer_q(kb, t, u2T)
            kb.barrier()
        if 'peer' in stages:
            stage_peer(kb, t, u2T)
        kb.finish(['m_scr', 'hz_tok', 'QT_scr', 'KT_scr', 'V_scr', 'MBT_scr', 'na_dbg', 'Kf_scr', 'hy_scr', 'x1_scr', 'u2_scr', 'qT_scr', 'G_scr', 'out'])
        print("instructions:", kb.ninst)
    return nc


def host_consts(half):
    c = {}
    dh = np.zeros((32, 64, 64), np.float32)
    for kc in range(64):
        for q in range(64):
            d = kc - q + 15
            if 0 <= d <= 30:
                dh[d, kc, q] = 1.0
            ws = min(max(q - 8, 0), 48)
            dh[31, kc, q] = 0.0 if ws <= kc < ws + 16 else NEG
    c['dhot'] = dh.reshape(32, 4096)
    rm = np.full((7, 12), NEG, np.float32)
    for ci, i in enumerate([0, 1, 2, 3, 13, 14, 15]):
        for k in range(12):
            if i < 4:
                ok = (4 <= k < 12) if half == 0 else (i <= k < i + 8)
            else:
                ok = (i - 12 <= k < i - 4) if half == 0 else (0 <= k < 8)
            if ok:
                rm[ci, k] = 0.0
    T0 = half * LH
    sg = (np.arange(L) + T0) % L
    tt_ = np.linspace(0.0, 1.0, L, dtype=np.float32)[sg][:, None]
    wv_ = (2.0 * np.pi * sg.astype(np.float32) / L).astype(np.float32)[:, None]
    ff_ = np.linspace(1e-4, 15.0, 16, dtype=np.float32)[None, :]
    zz = np.concatenate([tt_, np.cos(ff_ * wv_), -np.sin(ff_ * wv_)], axis=-1).astype(np.float32)
    c['zT'] = np.ascontiguousarray(zz.T)
    deltas = np.abs(np.linspace(np.log(1e-2) / 1.5, np.log(1e-2) / 0.3, DHY, dtype=np.float32))
    c['Edec'] = np.exp(-tt_ * deltas[None, :]).astype(np.float32)
    rr = np.arange(NFFT)
    fq = np.where(rr <= 2048, rr, rr - 2048).astype(np.int64)
    ang = 2.0 * np.pi * ((sg[:, None].astype(np.int64) * fq[None, :]) % NFFT) / NFFT
    Wfull = np.where(rr[None, :] <= 2048, np.cos(ang), -np.sin(ang))
    c['Wf'] = np.ascontiguousarray(Wfull.reshape(16, 128, 32, 128).transpose(2, 1, 0, 3)).astype(ml_dtypes.bfloat16)
    wgt = np.where((rr == 0) | (rr == 2048), 1.0, 2.0) / NFFT
    Winv = (np.where(rr[None, :] <= 2048, np.cos(ang), -np.sin(ang)) * wgt[None, :]).T
    c['Wi'] = np.ascontiguousarray(Winv.reshape(32, 128, 16, 128).transpose(2, 1, 0, 3)).astype(ml_dtypes.bfloat16)
    c['rowmask'] = np.ascontiguousarray(np.broadcast_to(rm[None, :, :, None], (64, 7, 12, 64))).astype(np.float32)
    return c


def make_core_inputs(inputs, b, half):
    T0 = half * LH
    f = lambda a: np.ascontiguousarray(a, dtype=np.float32)
    m = {}
    m['xr'] = f(np.roll(inputs['x'][b], -T0, axis=0))
    m['c2'] = f(np.stack([inputs['c'][b], inputs['c_ctx']]))
    m['ctxb'] = f(inputs['ctx'][b])
    m['w_ada'] = f(inputs['w_ada'][0])
    m['b_ada'] = f(inputs['b_ada'][0][None])
    m['w_in'] = f(inputs['w_in'][0])
    m['conv_w'] = f(inputs['conv_w'][0])
    m['conv_b'] = f(inputs['conv_b'][0][None])
    m['e01'] = np.array([[1.0, 0.0]] if half == 0 else [[0.0, 1.0]], dtype=np.float32)
    m['ident'] = np.eye(128, dtype=np.float32)
    m['na_rpb'] = f(inputs['na_rpb'][0])
    m['na_norm_g'] = f(inputs['na_norm_g'][0][None])
    for k_ in ('filt_w1', 'filt_w2', 'filt_w3', 'filt_w4', 'filt_freq'):
        m[k_] = f(inputs[k_][0])
    m['filt_b'] = f(np.stack([inputs['filt_b1'][0], inputs['filt_b2'][0], inputs['filt_b3'][0]]))
    m['hy_skip'] = f(inputs['hy_skip'][0].reshape(1, 2048))
    m['hy_norm_g'] = f(inputs['hy_norm_g'][0][None])
    m['w_out'] = f(inputs['w_out'][0])
    for k_ in ('ln1_g', 'ln1_b', 'ln2_g', 'ln2_b'):
        m[k_] = f(inputs[k_][0][None])
    m['peer_wq'] = f(inputs['peer_wq'][0])
    m['peer_keys'] = f(inputs['peer_keys'][0].reshape(16, 128, 128))
    m['peer_u'] = f(inputs['peer_u'][0])
    m['peer_v'] = f(inputs['peer_v'][0])
    m['iota16'] = np.ascontiguousarray(np.broadcast_to(np.arange(16, dtype=np.float32)[None], (128, 16)))
    m['iota128'] = np.ascontiguousarray(np.broadcast_to(np.arange(128, dtype=np.float32)[None], (128, 128)))
    m.update(host_consts(half))
    return m


def kernel(**inputs):
    nc = build_program()
    in_maps = [make_core_inputs(inputs, c // 2, c % 2) for c in range(8)]
    res = run_bass_kernel_spmd(nc, in_maps, core_ids=list(range(8)))
    out = np.zeros((4, L, D), dtype=np.float32)
    for c in range(8):
        b, half = c // 2, c % 2
        out[b, half * LH:(half + 1) * LH] = res.results[c]['out']
    return out
```

```python
import numpy as np
import ml_dtypes
from contextlib import ExitStack
import concourse.bass as bass
import concourse.mybir as mybir
from concourse.bass_utils import run_bass_kernel_spmd

F32 = mybir.dt.float32
BF16 = mybir.dt.bfloat16
U32 = mybir.dt.uint32
I32 = mybir.dt.int32
AF = mybir.ActivationFunctionType
ALU = mybir.AluOpType
AX = mybir.AxisListType

D = 2048
L = 2048
LH = 1024
CTX = 256
NTOK = 2560
DHY = 1024
NEG = -30000.0
ALPHA = 2.0 ** 0.25
LN_EPS = 1e-5
NFFT = 4096
STRICT_SAME_ENGINE = True


class KB:
    NDS = 12

    def __init__(self, nc, es):
        self.nc = nc
        self.E = {'pe': nc.tensor, 'act': nc.scalar, 'dve': nc.vector, 'pool': nc.gpsimd, 'sp': nc.sync}
        self.sem = {e: es.enter_context(nc.semaphore('sem_' + e)) for e in ('pe', 'act', 'dve', 'pool')}
        self.cnt = dict.fromkeys(self.sem, 0)
        self.dsem = {q: [es.enter_context(nc.semaphore('dq_%s_%d' % (q, i))) for i in range(self.NDS)]
                     for q in ('sp', 'pool')}
        self.dcnt = {q: [0] * self.NDS for q in ('sp', 'pool')}
        self.dnext = {'sp': 0, 'pool': 0}
        self.seen = {e: {} for e in self.E}
        self.lastw = {}
        self.readers = {}
        self.ninst = 0

    def _wait(self, e, tok):
        sem, val = tok
        if e in self.sem and sem is self.sem[e] and (e == 'pe' or not STRICT_SAME_ENGINE):
            return
        if self.seen[e].get(id(sem), 0) >= val:
            return
        self.E[e].wait_ge(sem, val)
        self.seen[e][id(sem)] = val

    def _deps(self, e, r, w):
        for key in r:
            t = self.lastw.get(key)
            if t:
                self._wait(e, t)
        for key in w:
            t = self.lastw.get(key)
            if t:
                self._wait(e, t)
            for t in self.readers.get(key, {}).values():
                self._wait(e, t)

    def _record(self, tok, r, w):
        for key in r:
            d = self.readers.setdefault(key, {})
            old = d.get(id(tok[0]))
            if old is None or old[1] < tok[1]:
                d[id(tok[0])] = tok
        for key in w:
            self.lastw[key] = tok
            self.readers[key] = {}

    def op(self, e, fn, r=(), w=()):
        self._deps(e, r, w)
        inst = fn(self.E[e])
        self.cnt[e] += 1
        inst.then_inc(self.sem[e], 1)
        self._record((self.sem[e], self.cnt[e]), r, w)
        self.ninst += 1

    def dma(self, q, fn, r=(), w=()):
        slot = self.dnext[q] % self.NDS
        self.dnext[q] += 1
        sem = self.dsem[q][slot]
        if self.dcnt[q][slot] > 0:
            self._wait(q, (sem, 16 * self.dcnt[q][slot]))
        self._deps(q, r, w)
        inst = fn(self.E[q])
        self.dcnt[q][slot] += 1
        inst.then_inc(sem, 16)
        self._record((sem, 16 * self.dcnt[q][slot]), r, w)
        self.ninst += 1

    def barrier(self):
        toks = [(self.sem[e], self.cnt[e]) for e in self.sem if self.cnt[e] > 0]
        for q in self.dsem:
            for i, sem in enumerate(self.dsem[q]):
                if self.dcnt[q][i] > 0:
                    toks.append((sem, 16 * self.dcnt[q][i]))
        for e in self.E:
            for tok in toks:
                self._wait(e, tok)

    def finish(self, keys):
        for key in keys:
            t = self.lastw.get(key)
            if t:
                self._wait('sp', t)


class T:
    pass


def keys(name, n):
    return [(name, i) for i in range(n)]


def stage_ada(kb, t):
    nc = kb.nc
    with ExitStack() as es:
        sb = lambda name, shape, dt: es.enter_context(nc.sbuf_tensor(name, shape, dt))
        cT = sb('cT', [128, 2, 16], F32)
        scT = sb('scT', [128, 16, 2], F32)
        wbuf = [sb('wada%d' % i, [128, 16, 512], F32) for i in range(2)]
        bada = sb('bada', [2, 12288], F32)
        mall = sb('mall', [2, 12288], F32)
        kb.dma('sp', lambda e: e.dma_start(out=cT[:], in_=t.c2.ap().rearrange("n (k p) -> p n k", p=128)), w=['cT'])
        kb.op('act', lambda e: e.activation(out=scT[:].rearrange("p k n -> p n k"), in_=cT[:], func=AF.Silu),
              r=['cT'], w=['scT'])
        kb.dma('sp', lambda e: e.dma_start(out=bada[:], in_=bass.AP(t.b_ada, 0, [[0, 2], [1, 12288]])), w=['bada'])
        wv = t.w_ada.ap().rearrange("(k p) n -> p k n", p=128)
        for j in range(24):
            wb = wbuf[j % 2]
            kb.dma('sp', lambda e: e.dma_start(out=wb[:], in_=wv[:, :, j * 512:(j + 1) * 512]), w=[('wada', j % 2)])
            ps = t.ps[j % 2]
            for k in range(16):
                kb.op('pe', lambda e: e.matmul(ps[0:2, :], lhsT=scT[:, k, :], rhs=wb[:, k, :],
                                               start=(k == 0), stop=(k == 15)),
                      r=['scT', ('wada', j % 2)], w=[('ps', j % 2)])
            kb.op('dve', lambda e: e.tensor_tensor(out=mall[:, j * 512:(j + 1) * 512], in0=ps[0:2, :],
                                                   in1=bada[:, j * 512:(j + 1) * 512], op=ALU.add),
                  r=[('ps', j % 2), 'bada'], w=['mall'])
        kb.dma('sp', lambda e: e.dma_start(out=t.m_scr.ap(), in_=mall[:]), r=['mall'], w=['m_scr'])


def stage_uT(kb, t, uT):
    nc = kb.nc
    UK = keys('uT', 16)
    with ExitStack() as es:
        sb = lambda name, shape, dt: es.enter_context(nc.sbuf_tensor(name, shape, dt))
        mfm = sb('mfm', [128, 2, 6, 16], F32)
        sc1p = sb('sc1p', [128, 2, 16], F32)
        xg = [sb('xg%d' % i, [128, 4, 2048], F32) for i in range(2)]
        kb.dma('sp', lambda e: e.dma_start(out=mfm[:], in_=bass.AP(t.m_scr, 0, [[1, 128], [12288, 2], [2048, 6], [128, 16]])),
               r=['m_scr'], w=['mfm'])
        kb.op('dve', lambda e: e.tensor_scalar(out=sc1p[:], in0=mfm[:, :, 1, :], scalar1=1.0, scalar2=None, op0=ALU.add),
              r=['mfm'], w=['sc1p'])
        xv = t.xr.ap().rearrange("(g t p) d -> g p t d", t=4, p=128)
        pi = 0
        for g in range(5):
            xb = xg[g % 2]
            nt = 4 if g < 4 else 2
            if g < 4:
                kb.dma('sp', lambda e: e.dma_start(out=xb[:], in_=xv[g]), w=[('xg', g % 2)])
                n = 0
                col0 = 256 + g * 512
            else:
                kb.dma('sp', lambda e: e.dma_start(out=xb[:, 0:2, :], in_=t.ctxb.ap().rearrange("(t p) d -> p t d", p=128)),
                       w=[('xg', g % 2)])
                n = 1
                col0 = 2304
            for k in range(16):
                p = pi % 4
                pi += 1
                ps = t.ps[p]
                for tt in range(nt):
                    kb.op('pe', lambda e: e.transpose(ps[:, tt * 128:(tt + 1) * 128], xb[:, tt, k * 128:(k + 1) * 128], t.ident[:]),
                          r=[('xg', g % 2), 'ident'], w=[('ps', p)])
                kb.op('act', lambda e: e.activation(out=uT[:, k, col0:col0 + nt * 128], in_=ps[:, 0:nt * 128], func=AF.Identity,
                                                    scale=sc1p[:, n, k:k + 1], bias=mfm[:, n, 0, k:k + 1]),
                      r=[('ps', p), 'sc1p', 'mfm'], w=[('uT', k)])
        kb.op('pool', lambda e: e.tensor_copy(out=uT[:, :, 0:256], in_=uT[:, :, 2048:2304]), r=UK, w=UK)


def stage_proj(kb, t, uT):
    nc = kb.nc
    UK = keys('uT', 16)
    with ExitStack() as es:
        sb = lambda name, shape, dt: es.enter_context(nc.sbuf_tensor(name, shape, dt))
        wblk = [sb('wblk%d' % i, [128, 16, 512], BF16) for i in range(2)]
        cw = sb('cw', [128, 3, 24], F32)
        cb = sb('cb', [128, 24], F32)
        ee = sb('ee', [128, 2], F32)
        cwe = sb('cwe', [128, 4, 24], F32)
        zs = [sb('zs%d' % i, [128, 2050], F32) for i in range(2)]
        yc = [sb('yc%d' % i, [128, 2048], F32) for i in range(2)]
        ytok = [sb('ytok%d' % i, [128, 16, 128], F32) for i in range(2)]
        qst = [sb('qst%d' % i, [128, 2560], BF16) for i in range(2)]
        vst = [sb('vst%d' % i, [128, 512], BF16) for i in range(2)]
        kb.dma('sp', lambda e: e.dma_start(out=cw[:], in_=t.conv_w.ap().rearrange("k (j p) -> p k j", p=128)), w=['cw'])
        kb.dma('sp', lambda e: e.dma_start(out=cb[:], in_=t.conv_b.ap().rearrange("o (j p) -> p (o j)", p=128)), w=['cb'])
        kb.dma('sp', lambda e: e.dma_start(out=ee[:], in_=bass.AP(t.e01, 0, [[0, 128], [1, 2]])), w=['ee'])
        for i, (ei, tap) in enumerate([(0, 0), (0, 2), (1, 0), (1, 2)]):
            kb.op('dve', lambda e: e.tensor_scalar(out=cwe[:, i, :], in0=cw[:, tap, :], scalar1=ee[:, ei:ei + 1], scalar2=-1.0,
                                                   op0=ALU.mult, op1=ALU.mult), r=['cw', 'ee'], w=['cwe'])
        wv = t.w_in.ap().rearrange("(k p) n -> p k n", p=128)
        hzv = t.hz_tok.ap().rearrange("(tt p) c -> p tt c", p=128)
        state = {'ev': 0}

        def load_blk(blk):
            kb.dma('pool', lambda e: e.dma_start(out=wblk[blk % 2][:], in_=wv[:, :, blk * 512:(blk + 1) * 512]), w=[('wblk', blk % 2)])

        def emit_P(j):
            blk, cc = j // 4, j % 4
            if cc == 0:
                load_blk(blk)
            wb, wk = wblk[blk % 2], ('wblk', blk % 2)
            z, zk = zs[j % 2], ('zs', j % 2)
            for tb in range(4):
                ps = t.ps[tb]
                for k in range(16):
                    kb.op('pe', lambda e: e.matmul(ps[:], lhsT=wb[:, k, cc * 128:(cc + 1) * 128],
                                                   rhs=uT[:, k, 256 + tb * 512:256 + (tb + 1) * 512],
                                                   start=(k == 0), stop=(k == 15)),
                          r=[wk, ('uT', k)], w=[('ps', tb)])
                kb.op('act', lambda e: e.activation(out=z[:, 1 + tb * 512:1 + (tb + 1) * 512], in_=ps[:], func=AF.Identity),
                      r=[('ps', tb)], w=[zk])
            kb.op('act', lambda e: e.activation(out=z[:, 0:1], in_=z[:, 2048:2049], func=AF.Identity), r=[zk], w=[zk])
            kb.op('act', lambda e: e.activation(out=z[:, 2049:2050], in_=z[:, 1:2], func=AF.Identity), r=[zk], w=[zk])

        def emit_C(j):
            z, zk = zs[j % 2], ('zs', j % 2)
            y, yk_ = yc[j % 2], ('yc', j % 2)
            kb.op('dve', lambda e: e.tensor_scalar(out=y[:], in0=z[:, 1:2049], scalar1=cw[:, 1, j:j + 1],
                                                   scalar2=cb[:, j:j + 1], op0=ALU.mult, op1=ALU.add),
                  r=[zk, 'cw', 'cb'], w=[yk_])
            kb.op('dve', lambda e: e.scalar_tensor_tensor(out=y[:], in0=z[:, 0:2048], scalar=cw[:, 0, j:j + 1], in1=y[:],
                                                          op0=ALU.mult, op1=ALU.add), r=[zk, 'cw'], w=[yk_])
            kb.op('dve', lambda e: e.scalar_tensor_tensor(out=y[:], in0=z[:, 2:2050], scalar=cw[:, 2, j:j + 1], in1=y[:],
                                                          op0=ALU.mult, op1=ALU.add), r=[zk, 'cw'], w=[yk_])
            for (ycol, zcol, ci) in [(0, 0, 0), (2047, 2049, 1), (1024, 1024, 2), (1023, 1025, 3)]:
                kb.op('dve', lambda e: e.scalar_tensor_tensor(out=y[:, ycol:ycol + 1], in0=z[:, zcol:zcol + 1],
                                                              scalar=cwe[:, ci, j:j + 1], in1=y[:, ycol:ycol + 1],
                                                              op0=ALU.mult, op1=ALU.add), r=[zk, 'cwe'], w=[yk_])

        def emit_T(j):
            y, yk_ = yc[j % 2], ('yc', j % 2)
            yt = ytok[j % 2]
            yk = ('ytok', j % 2)
            for g in range(4):
                ps = t.ps[4 + g]
                for i in range(4):
                    tt = g * 4 + i
                    kb.op('pe', lambda e: e.transpose(ps[:, i * 128:(i + 1) * 128], y[:, tt * 128:(tt + 1) * 128], t.ident[:]),
                          r=[yk_, 'ident'], w=[('ps', 4 + g)])
                eng = 'act' if (state['ev'] % 2 == 0) else 'dve'
                state['ev'] += 1
                if eng == 'act':
                    kb.op('act', lambda e: e.activation(out=yt[:, g * 4:(g + 1) * 4, :], in_=ps[:].rearrange("p (a b) -> p a b", b=128),
                                                        func=AF.Identity), r=[('ps', 4 + g)], w=[yk])
                else:
                    kb.op('dve', lambda e: e.tensor_copy(out=yt[:, g * 4:(g + 1) * 4, :], in_=ps[:].rearrange("p (a b) -> p a b", b=128)),
                          r=[('ps', 4 + g)], w=[yk])
            kb.dma('sp', lambda e: e.dma_start(out=hzv[:, :, j * 128:(j + 1) * 128], in_=yt[:]), r=[yk], w=[('hz_tok', j)])

        emit_P(0)
        for j in range(24):
            if j + 1 < 24:
                emit_P(j + 1)
            emit_C(j)
            emit_T(j)
        for blk in range(6, 12):
            wb = wblk[blk % 2]
            wk = ('wblk', blk % 2)
            load_blk(blk)
            if False:
                pass
            elif blk < 10:
                isq = blk < 8
                for hp in range(4):
                    h = (blk % 2) * 8 + hp * 2
                    st = qst[hp % 2]
                    sk = ('qst', hp % 2)
                    nb = 2 if isq else 5
                    for tb in range(nb):
                        p = tb % 4
                        ps = t.ps[p]
                        c0 = 256 + tb * 512 if isq else tb * 512
                        for k in range(16):
                            kb.op('pe', lambda e: e.matmul(ps[:], lhsT=wb[:, k, hp * 128:(hp + 1) * 128], rhs=uT[:, k, c0:c0 + 512],
                                                           start=(k == 0), stop=(k == 15)), r=[wk, ('uT', k)], w=[('ps', p)])
                        kb.op('act', lambda e: e.activation(out=st[:, tb * 512:(tb + 1) * 512], in_=ps[:], func=AF.Identity),
                              r=[('ps', p)], w=[sk])
                    if isq:
                        kb.dma('sp', lambda e: e.dma_start(out=t.QT_scr.ap()[h:h + 2].rearrange("h d n -> (h d) n"), in_=st[:, 0:1024]), r=[sk], w=[('QT_scr', h)])
                    else:
                        kb.dma('sp', lambda e: e.dma_start(out=t.KT_scr.ap()[h:h + 2].rearrange("h d n -> (h d) n"), in_=st[:, 0:2560]), r=[sk], w=[('KT_scr', h)])
            else:
                vb = blk - 10
                for rr in range(20):
                    p = rr % 4
                    ps = t.ps[p]
                    for k in range(16):
                        kb.op('pe', lambda e: e.matmul(ps[:], lhsT=uT[:, k, rr * 128:(rr + 1) * 128], rhs=wb[:, k, :],
                                                       start=(k == 0), stop=(k == 15)), r=[wk, ('uT', k)], w=[('ps', p)])
                    st = vst[rr % 2]
                    sk = ('vst', rr % 2)
                    kb.op('act', lambda e: e.activation(out=st[:], in_=ps[:], func=AF.Identity), r=[('ps', p)], w=[sk])
                    kb.dma('sp', lambda e: e.dma_start(out=t.V_scr.ap()[rr * 128:(rr + 1) * 128, vb * 512:(vb + 1) * 512], in_=st[:]),
                           r=[sk], w=[('V_scr', rr, vb)])


def att_rows():
    rows = []
    for i in range(16):
        if i < 4:
            rows.append((i, 0, 12, 3 - i, i))
        elif i <= 12:
            rows.append((i, i, 8, 3, None))
        else:
            rows.append((i, 12, 12, 15 - i, 4 + i - 13))
    return rows


def stage_bias(kb, t):
    nc = kb.nc
    with ExitStack() as es:
        sb = lambda name, shape, dt: es.enter_context(nc.sbuf_tensor(name, shape, dt))
        rpbT = sb('rpbT', [32, 240], F32)
        dhot = sb('dhot_sb', [32, 4096], F32)
        mst = sb('mst', [120, 4096], F32)
        kb.op('dve', lambda e: e.memset(rpbT[:], 1.0), w=['rpbT'])
        kb.dma('sp', lambda e: e.dma_start(out=rpbT[0:31, :], in_=t.na_rpb.ap().rearrange("h r d -> d (h r)")), w=['rpbT'], r=['rpbT'])
        kb.dma('sp', lambda e: e.dma_start(out=dhot[:], in_=t.dhot_d.ap()), w=['dhot'])
        mv = t.MBT_scr.ap()
        for c in range(2):
            for nb in range(8):
                ps = t.ps[nb % 4]
                kb.op('pe', lambda e: e.matmul(ps[0:120, :], lhsT=rpbT[:, c * 120:(c + 1) * 120], rhs=dhot[:, nb * 512:(nb + 1) * 512],
                                               start=True, stop=True), r=['rpbT', 'dhot'], w=[('ps', nb % 4)])
                kb.op('act', lambda e: e.activation(out=mst[:, nb * 512:(nb + 1) * 512], in_=ps[0:120, :], func=AF.Identity),
                      r=[('ps', nb % 4)], w=['mst'])
            kb.dma('sp', lambda e: e.dma_start(out=mv[c * 120:(c + 1) * 120, :], in_=mst[:]), r=['mst'], w=['MBT_scr'])


def stage_att(kb, t, mergedT):
    nc = kb.nc
    scale = 64 ** -0.5
    with ExitStack() as es:
        sb = lambda name, shape, dt: es.enter_context(nc.sbuf_tensor(name, shape, dt))
        KT = [sb('KTh%d' % i, [64, NTOK], BF16) for i in range(2)]
        QT = [sb('QTh%d' % i, [64, 1024], BF16) for i in range(2)]
        VH = [sb('VHh%d' % i, [64, 40, 65], BF16) for i in range(2)]
        MB = [sb('MBh%d' % i, [64, 15, 64], F32) for i in range(2)]
        rm = sb('rm_sb', [64, 7, 12, 64], F32)
        na_all = sb('na_all', [64, 16, 1024], F32)
        tmpA = [sb('tmpA%d' % i, [64, 512], F32) for i in range(3)]
        tmpB = [sb('tmpB%d' % i, [64, 256], F32) for i in range(3)]
        PTA = [sb('PTA%d' % i, [64, 512], BF16) for i in range(3)]
        PTB = [sb('PTB%d' % i, [64, 512], BF16) for i in range(3)]
        rz = [sb('rz%d' % i, [64, 1], F32) for i in range(3)]
        gna = sb('gna', [64, 1024], F32)
        ssq = sb('ssq', [64, 16], F32)
        sqj = sb('sqj', [64, 1024], F32)
        nrm = [sb('nrm%d' % i, [64, 1024], F32) for i in range(2)]
        kb.dma('sp', lambda e: e.dma_start(out=rm[:], in_=t.rowmask.ap()), w=['rm'])
        kb.dma('sp', lambda e: e.dma_start(out=gna[:], in_=bass.AP(t.na_norm_g, 0, [[0, 64], [1, 1024]])), w=['gna'])
        for i in range(2):
            kb.op('dve', lambda e: e.memset(VH[i][:], 1.0), w=[('VH', i)])
        vv = t.V_scr.ap().rearrange("(r kc) f -> kc r f", kc=64)
        mbv = t.MBT_scr.ap().rearrange("(h r) (kc q) -> h kc r q", r=15, q=64)
        rows = att_rows()
        items = [(h, r) for h in range(16) for r in rows]

        def emit_loads(h):
            hb = h % 2
            kb.dma('sp', lambda e: e.dma_start(out=KT[hb][:], in_=t.KT_scr.ap()[h]), r=['KT_scr'], w=[('KT', hb)])
            kb.dma('sp', lambda e: e.dma_start(out=QT[hb][:], in_=t.QT_scr.ap()[h]), r=['QT_scr'], w=[('QT', hb)])
            kb.dma('sp', lambda e: e.dma_start(out=VH[hb][:, :, 0:64], in_=vv[:, :, h * 64:(h + 1) * 64]), r=['V_scr'], w=[('VH', hb)])
            kb.dma('sp', lambda e: e.dma_start(out=MB[hb][:], in_=mbv[h]), r=['MBT_scr'], w=[('MB', hb)])

        def emit_st(n):
            h, (i, e0, ns, dr0, ci) = items[n]
            hb = h % 2
            nb = n % 3
            if i == 0:
                emit_loads(h)
            psA, psB = t.ps[nb], t.ps[3 + nb]
            q_ap = QT[hb][:, i * 64:(i + 1) * 64]
            rk = [('KT', hb), ('QT', hb)]
            for s_ in range(ns):
                if s_ < 8:
                    dst, dk = psA[0:64, s_ * 64:(s_ + 1) * 64], ('ps', nb)
                else:
                    dst, dk = psB[0:64, (s_ - 8) * 64:(s_ - 7) * 64], ('ps', 3 + nb)
                kb.op('pe', lambda e: e.matmul(dst, lhsT=KT[hb][:, (e0 + s_) * 64:(e0 + s_ + 1) * 64], rhs=q_ap, start=True, stop=True),
                      r=rk, w=[dk])
            for s_ in range(4):
                kb.op('pe', lambda e: e.matmul(psB[0:64, 256 + s_ * 64:256 + (s_ + 1) * 64], lhsT=KT[hb][:, (36 + s_) * 64:(37 + s_) * 64],
                                               rhs=q_ap, start=True, stop=True), r=rk, w=[('ps', 3 + nb)])

        def emit_rest(n):
            h, (i, e0, ns, dr0, ci) = items[n]
            hb = h % 2
            nb = n % 3
            so = n % 4
            ok_ = ('psO', so)
            psA, psB = t.ps[nb], t.ps[3 + nb]
            psO = t.ps[6][:, so * 128:(so + 1) * 128]
            kb.op('dve', lambda e: e.scalar_tensor_tensor(out=tmpA[nb][:], in0=psA[0:64, :], scalar=scale,
                                                          in1=MB[hb][:, dr0:dr0 + 8, :].rearrange("p a b -> p (a b)"),
                                                          op0=ALU.mult, op1=ALU.add), r=[('ps', nb), ('MB', hb)], w=[('tmpA', nb)])
            if ns == 12:
                kb.op('dve', lambda e: e.scalar_tensor_tensor(out=tmpB[nb][:], in0=psB[0:64, 0:256], scalar=scale,
                                                              in1=MB[hb][:, dr0 + 8:dr0 + 12, :].rearrange("p a b -> p (a b)"),
                                                              op0=ALU.mult, op1=ALU.add), r=[('ps', 3 + nb), ('MB', hb)], w=[('tmpB', nb)])
                kb.op('dve', lambda e: e.tensor_tensor(out=tmpA[nb][:], in0=tmpA[nb][:], in1=rm[:, ci, 0:8, :].rearrange("p a b -> p (a b)"),
                                                       op=ALU.add), r=['rm'], w=[('tmpA', nb)])
                kb.op('dve', lambda e: e.tensor_tensor(out=tmpB[nb][:], in0=tmpB[nb][:], in1=rm[:, ci, 8:12, :].rearrange("p a b -> p (a b)"),
                                                       op=ALU.add), r=['rm'], w=[('tmpB', nb)])
            kb.op('act', lambda e: e.activation(out=PTA[nb][:], in_=tmpA[nb][:], func=AF.Exp), r=[('tmpA', nb)], w=[('PTA', nb)])
            if ns == 12:
                kb.op('act', lambda e: e.activation(out=PTB[nb][:, 0:256], in_=tmpB[nb][:], func=AF.Exp), r=[('tmpB', nb)], w=[('PTB', nb)])
            kb.op('act', lambda e: e.activation(out=PTB[nb][:, 256:512], in_=psB[0:64, 256:512], func=AF.Exp, scale=scale),
                  r=[('ps', 3 + nb)], w=[('PTB', nb)])
            tot = ns + 4
            for s_ in range(tot):
                if s_ < 8:
                    lt = PTA[nb][:, s_ * 64:(s_ + 1) * 64]
                    vr = e0 + s_
                elif s_ < ns:
                    lt = PTB[nb][:, (s_ - 8) * 64:(s_ - 7) * 64]
                    vr = e0 + s_
                else:
                    lt = PTB[nb][:, 256 + (s_ - ns) * 64:256 + (s_ - ns + 1) * 64]
                    vr = 36 + (s_ - ns)
                kb.op('pe', lambda e: e.matmul(psO[0:64, 0:65], lhsT=lt, rhs=VH[hb][:, vr, :], start=(s_ == 0), stop=(s_ == tot - 1)),
                      r=[('PTA', nb), ('PTB', nb), ('VH', hb)], w=[ok_])
            kb.op('dve', lambda e: e.reciprocal(out=rz[nb][:], in_=psO[0:64, 64:65]), r=[ok_], w=[('rz', nb)])
            kb.op('act', lambda e: e.activation(out=na_all[:, i, h * 64:(h + 1) * 64], in_=psO[0:64, 0:64], func=AF.Identity,
                                                scale=rz[nb][:, 0:1]), r=[ok_, ('rz', nb)], w=[('na', i)])

        emit_st(0)
        emit_st(1)
        for n in range(len(items)):
            if n + 2 < len(items):
                emit_st(n + 2)
            emit_rest(n)
        if t.debug:
            kb.dma('sp', lambda e: e.dma_start(out=t.na_dbg.ap().rearrange("(i q) f -> q i f", q=64), in_=na_all[:]),
                   r=keys('na', 16), w=['na_dbg'])
        for i in range(16):
            kb.op('act', lambda e: e.activation(out=sqj[:], in_=na_all[:, i, :], func=AF.Square, accum_out=ssq[:, i:i + 1]),
                  r=[('na', i)], w=['sqj', ('ssq', i)])
        kb.op('dve', lambda e: e.tensor_scalar(out=ssq[:], in0=ssq[:], scalar1=1.0 / 1024, scalar2=LN_EPS, op0=ALU.mult, op1=ALU.add),
              r=keys('ssq', 16), w=['ssq2'])
        kb.op('act', lambda e: e.activation(out=ssq[:], in_=ssq[:], func=AF.Sqrt), r=['ssq2'], w=['ssq2'])
        kb.op('dve', lambda e: e.reciprocal(out=ssq[:], in_=ssq[:]), r=['ssq2'], w=['ssq2'])
        for i in range(16):
            nb = i % 2
            kb.op('dve', lambda e: e.scalar_tensor_tensor(out=nrm[nb][:], in0=na_all[:, i, :], scalar=ssq[:, i:i + 1], in1=gna[:],
                                                          op0=ALU.mult, op1=ALU.mult), r=[('na', i), 'ssq2', 'gna'], w=[('nrm', nb)])
            for g in range(2):
                ps = t.ps[g]
                for fc in range(4):
                    kb.op('pe', lambda e: e.transpose(ps[:, fc * 64:(fc + 1) * 64], nrm[nb][:, (g * 4 + fc) * 128:(g * 4 + fc + 1) * 128],
                                                      t.ident[0:64, 0:64]), r=[('nrm', nb), 'ident'], w=[('ps', g)])
                kb.op('act', lambda e: e.activation(out=mergedT[:, 8 + g * 4:8 + (g + 1) * 4, i * 64:(i + 1) * 64],
                                                    in_=ps[:, 0:256].rearrange("p (a b) -> p a b", b=64), func=AF.Identity),
                      r=[('ps', g)], w=['mergedT'])


def stage_filters(kb, t):
    nc = kb.nc
    PI = float(np.pi)
    with ExitStack() as es:
        sb = lambda name, shape, dt: es.enter_context(nc.sbuf_tensor(name, shape, dt))
        zT = sb('zT_sb', [33, 2048], F32)
        w1 = sb('fw1', [33, 64], F32)
        w23 = sb('fw23', [64, 2, 64], F32)
        w4 = sb('fw4', [64, 4096], F32)
        fr = sb('ffr', [64, 3], F32)
        fb = sb('ffb', [64, 3], F32)
        hA = sb('hA', [64, 2048], F32)
        hB = sb('hB', [64, 2048], F32)
        wrp = sb('wrp', [64, 2048], F32)
        ones_c = sb('ones_c', [128, 1], F32)
        ones_r = sb('ones_r', [1, 128], F32)
        nones_b = sb('nones_b', [1, 128], BF16)
        ee = sb('ee2', [1, 2], F32)
        hs = sb('hs', [128, 16, 1024], BF16)
        hd = sb('hd', [128, 16, 1024], BF16)
        Et = [sb('Et%d' % i, [128, 512], F32) for i in range(2)]
        pf = [sb('pf%d' % i, [128, 512], F32) for i in range(2)]
        pb = [sb('pb%d' % i, [128, 512], F32) for i in range(2)]
        af = [sb('af%d' % i, [128, 512], F32) for i in range(2)]
        ab = [sb('ab%d' % i, [128, 512], F32) for i in range(2)]
        bw0 = sb('bw0', [1, 2, 512], F32)
        nb0 = sb('nb0', [1, 2, 512], BF16)
        rcol = sb('rcol', [1, 512], F32)
        rinvb = sb('rinvb', [128, 2, 512], F32)
        wf = [sb('wfA%d' % i, [128, 16, 128], BF16) for i in range(4)]
        kout = [sb('kout%d' % i, [128, 512], F32) for i in range(4)]
        kb.dma('sp', lambda e: e.dma_start(out=zT[:], in_=t.zT_d.ap()), w=['zT'])
        kb.dma('sp', lambda e: e.dma_start(out=w1[:], in_=t.filt_w1.ap()), w=['fw1'])
        kb.dma('sp', lambda e: e.dma_start(out=w23[:, 0, :], in_=t.filt_w2.ap()), w=['fw23'])
        kb.dma('sp', lambda e: e.dma_start(out=w23[:, 1, :], in_=t.filt_w3.ap()), w=['fw23'], r=['fw23'])
        kb.dma('sp', lambda e: e.dma_start(out=w4[:], in_=t.filt_w4.ap()), w=['fw4'])
        kb.dma('sp', lambda e: e.dma_start(out=fr[:], in_=t.filt_freq.ap().rearrange("k j -> j k")), w=['ffr'])
        kb.dma('sp', lambda e: e.dma_start(out=fb[:], in_=t.filt_b.ap().rearrange("k j -> j k")), w=['ffb'])
        kb.dma('sp', lambda e: e.dma_start(out=ee[:], in_=t.e01.ap()), w=['ee2'])
        kb.op('dve', lambda e: e.tensor_tensor(out=fb[:], in0=fb[:], in1=fr[:], op=ALU.mult), r=['ffr', 'ffb'], w=['ffb'])
        kb.op('dve', lambda e: e.memset(ones_c[:], 1.0), w=['ones_c'])
        kb.op('dve', lambda e: e.memset(ones_r[:], 1.0), w=['ones_r'])
        kb.op('dve', lambda e: e.memset(nones_b[:], -1.0), w=['nones_b'])
        src, srck = zT, 'zT'
        for layer in range(3):
            dst, dstk = (hA, 'hA') if layer % 2 == 0 else (hB, 'hB')
            for blk in range(4):
                ps = t.ps[blk]
                lhs = w1[:, :] if layer == 0 else w23[:, layer - 1, :]
                kb.op('pe', lambda e: e.matmul(ps[0:64, :], lhsT=lhs, rhs=src[:, blk * 512:(blk + 1) * 512], start=True, stop=True),
                      r=['fw1', 'fw23', srck], w=[('ps', blk)])
                kb.op('act', lambda e: e.activation(out=dst[:, blk * 512:(blk + 1) * 512], in_=ps[0:64, :], func=AF.Identity,
                                                    scale=fr[:, layer:layer + 1], bias=fb[:, layer:layer + 1]),
                      r=[('ps', blk), 'ffr', 'ffb'], w=[dstk])
            for _ in range(2):
                for (cmp, thr, add) in ((ALU.is_gt, PI, -2 * PI), (ALU.is_lt, -PI, 2 * PI)):
                    kb.op('dve', lambda e: e.tensor_scalar(out=wrp[:], in0=dst[:], scalar1=thr, scalar2=add, op0=cmp, op1=ALU.mult),
                          r=[dstk], w=['wrp'])
                    kb.op('dve', lambda e: e.tensor_tensor(out=dst[:], in0=dst[:], in1=wrp[:], op=ALU.add), r=[dstk, 'wrp'], w=[dstk])
            kb.op('act', lambda e: e.activation(out=dst[:], in_=dst[:], func=AF.Sin), r=[dstk], w=[dstk])
            src, srck = dst, dstk
        h3, h3k = src, srck
        Ev = t.E_d.ap()
        n = 0
        wi = 0
        for o in range(2):
            for hf in range(2):
                hcs = slice(hf * 512, (hf + 1) * 512)
                cs = t.ps[7]
                for tt in range(16):
                    nb = n % 2
                    n += 1
                    psf, psb = t.ps[nb], t.ps[2 + nb]
                    for (ps_, dr, pk) in ((psf, 0, nb), (psb, 1, 2 + nb)):
                        c0 = o * 2048 + dr * 1024 + hf * 512
                        kb.op('pe', lambda e: e.matmul(ps_[:], lhsT=h3[:, tt * 128:(tt + 1) * 128], rhs=w4[:, c0:c0 + 512], start=True, stop=True),
                              r=[h3k, 'fw4'], w=[('ps', pk)])
                    kb.dma('sp', lambda e: e.dma_start(out=Et[nb][:], in_=Ev[tt * 128:(tt + 1) * 128, hcs]), w=[('Et', nb)])
                    kb.op('dve', lambda e: e.tensor_tensor(out=pf[nb][:], in0=psf[:], in1=Et[nb][:], op=ALU.mult), r=[('ps', nb), ('Et', nb)], w=[('pf', nb)])
                    kb.op('dve', lambda e: e.tensor_tensor(out=pb[nb][:], in0=psb[:], in1=Et[nb][:], op=ALU.mult), r=[('ps', 2 + nb), ('Et', nb)], w=[('pb', nb)])
                    kb.op('pool', lambda e: e.tensor_tensor(out=hs[:, tt, hcs], in0=pf[nb][:], in1=pb[nb][:], op=ALU.add), r=[('pf', nb), ('pb', nb)], w=[('hs', hf)])
                    kb.op('pool', lambda e: e.tensor_tensor(out=hd[:, tt, hcs], in0=pf[nb][:], in1=pb[nb][:], op=ALU.subtract), r=[('pf', nb), ('pb', nb)], w=[('hd', hf)])
                    kb.op('act', lambda e: e.activation(out=af[nb][:], in_=pf[nb][:], func=AF.Abs), r=[('pf', nb)], w=[('af', nb)])
                    kb.op('act', lambda e: e.activation(out=ab[nb][:], in_=pb[nb][:], func=AF.Abs), r=[('pb', nb)], w=[('ab', nb)])
                    kb.op('pe', lambda e: e.matmul(cs[0:1, :], lhsT=ones_c[:, :], rhs=af[nb][:], start=(tt == 0), stop=False),
                          r=[('af', nb), 'ones_c'], w=[('ps', 7)])
                    kb.op('pe', lambda e: e.matmul(cs[0:1, :], lhsT=ones_c[:, :], rhs=ab[nb][:], start=False, stop=(tt == 15)),
                          r=[('ab', nb), 'ones_c'], w=[('ps', 7)])
                    if tt in (0, 8):
                        kb.op('dve', lambda e: e.tensor_copy(out=bw0[:, tt // 8, :], in_=pb[nb][0:1, :]), r=[('pb', nb)], w=['bw0'])
                kb.op('dve', lambda e: e.reciprocal(out=rcol[:], in_=cs[0:1, :]), r=[('ps', 7)], w=['rcol'])
                kb.op('dve', lambda e: e.tensor_scalar(out=bw0[:, 0, :], in0=bw0[:, 0, :], scalar1=ee[:, 0:1], scalar2=None, op0=ALU.mult),
                      r=['bw0', 'ee2'], w=['bw0'])
                kb.op('dve', lambda e: e.scalar_tensor_tensor(out=nb0[:, hf, :], in0=bw0[:, 1, :], scalar=ee[:, 1:2], in1=bw0[:, 0, :],
                                                              op0=ALU.mult, op1=ALU.add), r=['bw0', 'ee2'], w=[('nb0', hf)])
                kb.op('pe', lambda e: e.matmul(t.ps[6][:], lhsT=ones_r[:, :], rhs=rcol[:], start=True, stop=True), r=['ones_r', 'rcol'], w=[('ps', 6)])
                kb.op('act', lambda e: e.activation(out=rinvb[:, hf, :], in_=t.ps[6][:], func=AF.Identity), r=[('ps', 6)], w=[('rinvb', hf)])
            kv = t.Kf_scr.ap()[o]
            kn = 0
            for fc in list(range(32)) + [32]:
                real_fc = 16 if fc == 32 else fc
                use_hs = (fc < 16) or (fc == 32)
                wb_ = wf[wi % 4]
                wk = ('wfA', wi % 4)
                wi += 1
                kb.dma('sp' if wi % 2 == 0 else 'pool', lambda e: e.dma_start(out=wb_[:], in_=t.Wf_d.ap()[real_fc]), w=[wk])
                for hf in range(2):
                    hcs = slice(hf * 512, (hf + 1) * 512)
                    p = 4 + (kn % 2)
                    ps = t.ps[p]
                    srcT, srcK = (hs, ('hs', hf)) if use_hs else (hd, ('hd', hf))
                    for k in range(16):
                        kb.op('pe', lambda e: e.matmul(ps[:], lhsT=wb_[:, k, :], rhs=srcT[:, k, hcs], start=(k == 0), stop=(k == 15 and not use_hs)),
                              r=[wk, srcK], w=[('ps', p)])
                    if use_hs:
                        kb.op('pe', lambda e: e.matmul(ps[:], lhsT=nones_b[:, :], rhs=nb0[:, hf, :], start=False, stop=True),
                              r=['nones_b', ('nb0', hf)], w=[('ps', p)])
                    ko, kk = kout[kn % 4], ('kout', kn % 4)
                    kn += 1
                    if fc == 32:
                        kb.op('dve', lambda e: e.tensor_tensor(out=ko[0:1, :], in0=ps[0:1, :], in1=rinvb[0:1, hf, :], op=ALU.mult),
                              r=[('ps', p), ('rinvb', hf)], w=[kk])
                        kb.dma('sp', lambda e: e.dma_start(out=kv[2048:2049, hcs], in_=ko[0:1, :]), r=[kk, ('Kf_scr', 16, hf)], w=[('Kf_scr', 16, hf)])
                    else:
                        kb.op('dve', lambda e: e.tensor_tensor(out=ko[:], in0=ps[:], in1=rinvb[:, hf, :], op=ALU.mult), r=[('ps', p), ('rinvb', hf)], w=[kk])
                        kb.dma('sp', lambda e: e.dma_start(out=kv[fc * 128:(fc + 1) * 128, hcs], in_=ko[:]), r=[kk], w=[('Kf_scr', fc, hf)])
        kb.lastw['Kf_scr'] = None


def stage_conv(kb, t):
    nc = kb.nc
    with ExitStack() as es:
        sb = lambda name, shape, dt: es.enter_context(nc.sbuf_tensor(name, shape, dt))
        vtok = sb('vtok', [128, 16, 1024], BF16)
        Yall = sb('Yall', [128, 32, 1024], BF16)
        skipb = sb('skipb', [128, 2, 1024], F32)
        wf = [sb('wfB%d' % i, [128, 16, 128], BF16) for i in range(4)]
        wi_ = [sb('wiB%d' % i, [128, 32, 128], BF16) for i in range(2)]
        kre = [sb('kre%d' % i, [128, 512], F32) for i in range(2)]
        kim = [sb('kim%d' % i, [128, 512], F32) for i in range(2)]
        vre = [sb('vre%d' % i, [128, 512], F32) for i in range(2)]
        vim = [sb('vim%d' % i, [128, 512], F32) for i in range(2)]
        t1 = [sb('t1_0', [128, 512], F32)] * 2
        t2 = [sb('t2_0', [128, 512], F32)] * 2
        xg = [sb('xgc%d' % i, [128, 512], F32) for i in range(2)]
        aa = [sb('aa%d' % i, [128, 512], F32) for i in range(2)]
        a0 = sb('a0', [1, 512], F32)
        bb = [sb('bbq%d' % i, [128, 512], F32) for i in range(2)]
        hyt = [sb('hyt%d' % i, [128, 512], F32) for i in range(2)]
        hzv = t.hz_tok.ap().rearrange("(tt p) c -> p tt c", p=128)
        for tt in range(16):
            kb.dma('pool', lambda e: e.dma_start(out=vtok[:, tt, :], in_=hzv[:, tt, 0:1024]), r=['hz_tok'], w=[('vtok', tt)])
        kb.dma('sp', lambda e: e.dma_start(out=skipb[:], in_=bass.AP(t.hy_skip, 0, [[0, 128], [1, 2048]])), w=['skipb'])
        wn = 0
        kn = 0
        xn = 0
        for conv in range(2):
            kv = t.Kf_scr.ap()[conv]
            for fc in range(16):
                wbs = []
                for part in range(2):
                    wb_ = wf[wn % 4]
                    wk = ('wfB', wn % 4)
                    kb.dma('sp' if part == 0 else 'pool', lambda e: e.dma_start(out=wb_[:], in_=t.Wf_d.ap()[fc + 16 * part]), w=[wk])
                    wn += 1
                    wbs.append((wb_, wk))
                for cb in range(2):
                    cs = slice(cb * 512, (cb + 1) * 512)
                    q = kn % 2
                    kn += 1
                    pss = []
                    for part in range(2):
                        wb_, wk = wbs[part]
                        p = part * 2 + q
                        ps = t.ps[p]
                        for k in range(16):
                            kb.op('pe', lambda e: e.matmul(ps[:], lhsT=wb_[:, k, :], rhs=vtok[:, k, cs],
                                                           start=(k == 0), stop=(k == 15)), r=[wk, ('vtok', k)], w=[('ps', p)])
                        pss.append((ps, p))
                    kb.dma('sp', lambda e: e.dma_start(out=kre[q][:], in_=kv[fc * 128:(fc + 1) * 128, cs]), r=['Kf_scr'], w=[('kre', q)])
                    kb.dma('sp', lambda e: e.dma_start(out=kim[q][:], in_=kv[2048 + fc * 128:2048 + (fc + 1) * 128, cs]), r=['Kf_scr'], w=[('kim', q)])
                    kb.op('act', lambda e: e.activation(out=vre[q][:], in_=pss[0][0][:], func=AF.Identity), r=[('ps', pss[0][1])], w=[('vre', q)])
                    kb.op('act', lambda e: e.activation(out=vim[q][:], in_=pss[1][0][:], func=AF.Identity), r=[('ps', pss[1][1])], w=[('vim', q)])
                    kb.op('dve', lambda e: e.tensor_tensor(out=t1[q][:], in0=vre[q][:], in1=kre[q][:], op=ALU.mult), r=[('vre', q), ('kre', q)], w=['t1'])
                    kb.op('pool', lambda e: e.tensor_tensor(out=t2[q][:], in0=vim[q][:], in1=kim[q][:], op=ALU.mult), r=[('vim', q), ('kim', q)], w=['t2'])
                    kb.op('dve', lambda e: e.tensor_tensor(out=Yall[:, fc, cs], in0=t1[q][:], in1=t2[q][:], op=ALU.subtract),
                          r=['t1', 't2'], w=[('Y', cb)])
                    if fc == 0:
                        kb.op('dve', lambda e: e.tensor_copy(out=Yall[0:1, 0, cs], in_=t1[q][0:1, :]), r=['t1'], w=[('Y', cb)])
                        kb.op('dve', lambda e: e.tensor_copy(out=a0[0:1, :], in_=t2[q][0:1, :]), r=['t2'], w=['a0'])
                    kb.op('dve', lambda e: e.tensor_tensor(out=t1[q][:], in0=vre[q][:], in1=kim[q][:], op=ALU.mult), r=[('vre', q), ('kim', q)], w=['t1'])
                    kb.op('pool', lambda e: e.tensor_tensor(out=t2[q][:], in0=vim[q][:], in1=kre[q][:], op=ALU.mult), r=[('vim', q), ('kre', q)], w=['t2'])
                    kb.op('dve', lambda e: e.tensor_tensor(out=Yall[:, 16 + fc, cs], in0=t1[q][:], in1=t2[q][:], op=ALU.add),
                          r=['t1', 't2'], w=[('Y', cb)])
                    if fc == 0:
                        kb.op('dve', lambda e: e.tensor_copy(out=Yall[0:1, 16, cs], in_=a0[0:1, :]), r=['a0'], w=[('Y', cb)])
            ntt = 16 if conv == 0 else 8
            for tt in range(ntt):
                wb_ = wi_[tt % 2]
                wk = ('wiB', tt % 2)
                kb.dma('sp' if tt % 2 == 0 else 'pool', lambda e: e.dma_start(out=wb_[:], in_=t.Wi_d.ap()[tt]), w=[wk])
                for cb in range(2):
                    cs = slice(cb * 512, (cb + 1) * 512)
                    q = xn % 2
                    xn += 1
                    xb, xk = xg[q], ('xgc', q)
                    hb_, hk = hyt[q], ('hyt', q)
                    c0 = (1 + conv) * 1024 + cb * 512
                    kb.dma('sp', lambda e: e.dma_start(out=xb[:], in_=t.hz_tok.ap()[tt * 128:(tt + 1) * 128, c0:c0 + 512]),
                           r=['hz_tok'], w=[xk])
                    p = 4 + q
                    ps = t.ps[p]
                    for rc in range(32):
                        kb.op('pe', lambda e: e.matmul(ps[:], lhsT=wb_[:, rc, :], rhs=Yall[:, rc, cs],
                                                       start=(rc == 0), stop=(rc == 31)), r=[wk, ('Y', cb)], w=[('ps', p)])
                    kb.op('pool', lambda e: e.tensor_tensor(out=aa[q][:], in0=vtok[:, tt, cs], in1=skipb[:, conv, cs],
                                                            op=ALU.mult), r=[('vtok', tt), 'skipb'], w=[('aa', q)])
                    kb.op('dve', lambda e: e.tensor_tensor(out=bb[q][:], in0=ps[:], in1=aa[q][:], op=ALU.add), r=[('ps', p), ('aa', q)], w=[('bbq', q)])
                    if conv == 0:
                        kb.op('dve', lambda e: e.tensor_tensor(out=vtok[:, tt, cs], in0=bb[q][:], in1=xb[:],
                                                               op=ALU.mult), r=[('bbq', q), xk], w=[('vtok', tt)])
                    else:
                        kb.op('dve', lambda e: e.tensor_tensor(out=hb_[:], in0=bb[q][:], in1=xb[:], op=ALU.mult), r=[('bbq', q), xk], w=[hk])
                        kb.dma('sp', lambda e: e.dma_start(out=t.hy_scr.ap()[tt * 128:(tt + 1) * 128, cs], in_=hb_[:]), r=[hk], w=[('hy_scr', tt, cb)])


def bcast_row(dt_handle, off, n, parts=128):
    return bass.AP(dt_handle, off, [[0, parts], [1, n]])


def layer_norm_tile(kb, nc, r, rk, st, mv, rs, g, gk, b, bk, tag):
    for k in range(4):
        kb.op('dve', lambda e: e.bn_stats(out=st[:, k, :], in_=r[:, k * 512:(k + 1) * 512]), r=[rk], w=[tag + 'st'])
    kb.op('dve', lambda e: e.bn_aggr(out=mv[:], in_=st[:].rearrange("p a b -> p (a b)")), r=[tag + 'st'], w=[tag + 'mv'])
    kb.op('dve', lambda e: e.tensor_scalar(out=rs[:], in0=mv[:, 1:2], scalar1=LN_EPS, scalar2=None, op0=ALU.add), r=[tag + 'mv'], w=[tag + 'rs'])
    kb.op('act', lambda e: e.activation(out=rs[:], in_=rs[:], func=AF.Sqrt), r=[tag + 'rs'], w=[tag + 'rs'])
    kb.op('dve', lambda e: e.reciprocal(out=rs[:], in_=rs[:]), r=[tag + 'rs'], w=[tag + 'rs'])
    kb.op('dve', lambda e: e.tensor_scalar(out=r[:], in0=r[:], scalar1=mv[:, 0:1], scalar2=rs[:, 0:1], op0=ALU.subtract, op1=ALU.mult),
          r=[rk, tag + 'mv', tag + 'rs'], w=[rk])
    kb.op('pool', lambda e: e.tensor_tensor(out=r[:], in0=r[:], in1=g[:], op=ALU.mult), r=[rk, gk], w=[rk])
    kb.op('dve', lambda e: e.tensor_tensor(out=r[:], in0=r[:], in1=b[:], op=ALU.add), r=[rk, bk], w=[rk])


def stage_merge_a(kb, t, mergedT):
    nc = kb.nc
    with ExitStack() as es:
        sb = lambda name, shape, dt: es.enter_context(nc.sbuf_tensor(name, shape, dt))
        ghy = sb('ghy', [128, 1024], F32)
        hyt = [sb('hym%d' % i, [128, 1024], F32) for i in range(2)]
        sqj = sb('sqj3', [128, 1024], F32)
        nrm = sb('nrm3', [128, 1024], F32)
        ssq = sb('ssq3', [128, 8], F32)
        kb.dma('sp', lambda e: e.dma_start(out=ghy[:], in_=bcast_row(t.hy_norm_g, 0, 1024)), w=['ghy'])
        for tt in range(8):
            hb_, hk = hyt[tt % 2], ('hym', tt % 2)
            kb.dma('sp', lambda e: e.dma_start(out=hb_[:], in_=t.hy_scr.ap()[tt * 128:(tt + 1) * 128, :]), r=['hy_scr'], w=[hk])
            kb.op('act', lambda e: e.activation(out=sqj[:], in_=hb_[:], func=AF.Square, accum_out=ssq[:, tt:tt + 1]), r=[hk], w=['sqj3', ('ssq3', tt)])
            kb.op('dve', lambda e: e.tensor_scalar(out=ssq[:, tt:tt + 1], in0=ssq[:, tt:tt + 1], scalar1=1.0 / 1024, scalar2=LN_EPS, op0=ALU.mult, op1=ALU.add),
                  r=[('ssq3', tt)], w=[('ssq3', tt)])
            kb.op('act', lambda e: e.activation(out=ssq[:, tt:tt + 1], in_=ssq[:, tt:tt + 1], func=AF.Sqrt), r=[('ssq3', tt)], w=[('ssq3', tt)])
            kb.op('dve', lambda e: e.reciprocal(out=ssq[:, tt:tt + 1], in_=ssq[:, tt:tt + 1]), r=[('ssq3', tt)], w=[('ssq3', tt)])
            kb.op('dve', lambda e: e.scalar_tensor_tensor(out=nrm[:], in0=hb_[:], scalar=ssq[:, tt:tt + 1], in1=ghy[:], op0=ALU.mult, op1=ALU.mult),
                  r=[hk, ('ssq3', tt), 'ghy'], w=['nrm3'])
            for g in range(2):
                ps = t.ps[6 + g]
                for fc in range(4):
                    kb.op('pe', lambda e: e.transpose(ps[:, fc * 128:(fc + 1) * 128], nrm[:, (g * 4 + fc) * 128:(g * 4 + fc + 1) * 128], t.ident[:]),
                          r=['nrm3', 'ident'], w=[('ps', 6 + g)])
                kb.op('act', lambda e: e.activation(out=mergedT[:, g * 4:(g + 1) * 4, tt * 128:(tt + 1) * 128],
                                                    in_=ps[:].rearrange("p (a b) -> p a b", b=128), func=AF.Identity),
                      r=[('ps', 6 + g)], w=['mergedT'])


def stage_merge_b(kb, t, mergedT):
    nc = kb.nc
    with ExitStack() as es:
        sb = lambda name, shape, dt: es.enter_context(nc.sbuf_tensor(name, shape, dt))
        wo = sb('wo', [128, 16, 2048], BF16)
        g1b = sb('g1b', [128, 2048], F32)
        l1g = sb('l1g', [128, 2048], F32)
        l1b = sb('l1b', [128, 2048], F32)
        s2p = sb('s2p', [128, 2048], F32)
        h2b = sb('h2b', [128, 2048], F32)
        xt = [sb('xtm%d' % i, [128, 2048], F32) for i in range(2)]
        rr = [sb('rrm%d' % i, [128, 2048], F32) for i in range(2)]
        st = sb('st1', [128, 4, 6], F32)
        mv = sb('mv1', [128, 2], F32)
        rs = sb('rs1', [128, 1], F32)
        wv = t.w_out.ap().rearrange("(k p) n -> p k n", p=128)
        for cb in range(4):
            kb.dma('pool', lambda e: e.dma_start(out=wo[:, :, cb * 512:(cb + 1) * 512], in_=wv[:, :, cb * 512:(cb + 1) * 512]), w=[('wo', cb)])
        kb.dma('sp', lambda e: e.dma_start(out=g1b[:], in_=bcast_row(t.m_scr, 2 * D, D)), r=['m_scr'], w=['g1b'])
        kb.dma('sp', lambda e: e.dma_start(out=s2p[:], in_=bcast_row(t.m_scr, 4 * D, D)), r=['m_scr'], w=['s2p'])
        kb.dma('sp', lambda e: e.dma_start(out=h2b[:], in_=bcast_row(t.m_scr, 3 * D, D)), r=['m_scr'], w=['h2b'])
        kb.dma('sp', lambda e: e.dma_start(out=l1g[:], in_=bcast_row(t.ln1_g, 0, D)), w=['l1g'])
        kb.dma('sp', lambda e: e.dma_start(out=l1b[:], in_=bcast_row(t.ln1_b, 0, D)), w=['l1b'])
        kb.op('dve', lambda e: e.tensor_scalar(out=s2p[:], in0=s2p[:], scalar1=1.0, scalar2=None, op0=ALU.add), r=['s2p'], w=['s2p'])
        for tt in range(8):
            xb, xk = xt[tt % 2], ('xtm', tt % 2)
            r, rk = rr[tt % 2], ('rrm', tt % 2)
            kb.dma('sp', lambda e: e.dma_start(out=xb[:], in_=t.xr.ap()[tt * 128:(tt + 1) * 128, :]), w=[xk])
            for cb in range(4):
                ps = t.ps[cb]
                for k in range(16):
                    kb.op('pe', lambda e: e.matmul(ps[:], lhsT=mergedT[:, k, tt * 128:(tt + 1) * 128], rhs=wo[:, k, cb * 512:(cb + 1) * 512],
                                                   start=(k == 0), stop=(k == 15)), r=['mergedT', ('wo', cb)], w=[('ps', cb)])
                kb.op('dve', lambda e: e.tensor_tensor(out=r[:, cb * 512:(cb + 1) * 512], in0=ps[:], in1=g1b[:, cb * 512:(cb + 1) * 512], op=ALU.mult),
                      r=[('ps', cb), 'g1b'], w=[rk])
            kb.op('dve', lambda e: e.scalar_tensor_tensor(out=r[:], in0=xb[:], scalar=ALPHA, in1=r[:], op0=ALU.mult, op1=ALU.add), r=[xk, rk], w=[rk])
            layer_norm_tile(kb, nc, r, rk, st, mv, rs, l1g, 'l1g', l1b, 'l1b', 'ln1')
            kb.dma('sp', lambda e: e.dma_start(out=t.x1_scr.ap()[tt * 128:(tt + 1) * 128, :], in_=r[:]), r=[rk], w=['x1_scr'])
            kb.op('pool', lambda e: e.tensor_tensor(out=r[:], in0=r[:], in1=s2p[:], op=ALU.mult), r=[rk, 's2p'], w=[rk])
            kb.op('dve', lambda e: e.tensor_tensor(out=r[:], in0=r[:], in1=h2b[:], op=ALU.add), r=[rk, 'h2b'], w=[rk])
            kb.dma('sp', lambda e: e.dma_start(out=t.u2_scr.ap()[tt * 128:(tt + 1) * 128, :], in_=r[:]), r=[rk], w=['u2_scr'])


def stage_peer_q(kb, t, u2T):
    nc = kb.nc
    with ExitStack() as es:
        sb = lambda name, shape, dt: es.enter_context(nc.sbuf_tensor(name, shape, dt))
        ut = [sb('utq%d' % i, [128, 2048], F32) for i in range(2)]
        wq = [sb('wq%d' % i, [128, 16, 512], BF16) for i in range(2)]
        qs = [sb('qs%d' % i, [128, LH], F32) for i in range(2)]
        pi = 0
        for tt in range(8):
            ub, uk = ut[tt % 2], ('utq', tt % 2)
            kb.dma('sp', lambda e: e.dma_start(out=ub[:], in_=t.u2_scr.ap()[tt * 128:(tt + 1) * 128, :]), r=['u2_scr'], w=[uk])
            for g in range(4):
                p = pi % 4
                pi += 1
                ps = t.ps[p]
                for i in range(4):
                    k = g * 4 + i
                    kb.op('pe', lambda e: e.transpose(ps[:, i * 128:(i + 1) * 128], ub[:, k * 128:(k + 1) * 128], t.ident[:]),
                          r=[uk, 'ident'], w=[('ps', p)])
                kb.op('act', lambda e: e.activation(out=u2T[:, g * 4:(g + 1) * 4, tt * 128:(tt + 1) * 128],
                                                    in_=ps[:].rearrange("p (a b) -> p a b", b=128), func=AF.Identity),
                      r=[('ps', p)], w=['u2T'])
        wv = t.peer_wq.ap().rearrange("(k p) n -> p k n", p=128)
        for blk in range(4):
            wb_, wk = wq[blk % 2], ('wq', blk % 2)
            kb.dma('pool', lambda e: e.dma_start(out=wb_[:], in_=wv[:, :, blk * 512:(blk + 1) * 512]), w=[wk])
            for cc in range(4):
                c = blk * 4 + cc
                qb, qk = qs[c % 2], ('qs', c % 2)
                for tb in range(2):
                    p = 4 + tb
                    ps = t.ps[p]
                    for k in range(16):
                        kb.op('pe', lambda e: e.matmul(ps[:], lhsT=wb_[:, k, cc * 128:(cc + 1) * 128], rhs=u2T[:, k, tb * 512:(tb + 1) * 512],
                                                       start=(k == 0), stop=(k == 15)), r=[wk, 'u2T'], w=[('ps', p)])
                    kb.op('act', lambda e: e.activation(out=qb[:, tb * 512:(tb + 1) * 512], in_=ps[:], func=AF.Identity), r=[('ps', p)], w=[qk])
                kb.dma('sp', lambda e: e.dma_start(out=t.qT_scr.ap()[c], in_=qb[:]), r=[qk], w=['qT_scr'])


def stage_peer(kb, t, u2T):
    nc = kb.nc
    pst = lambda a: list(a.ap[0])
    with ExitStack() as es0:
        with ExitStack() as es:
            sb = lambda name, shape, dt: es.enter_context(nc.sbuf_tensor(name, shape, dt))
            keysT = sb('keysT', [128, 16, 128], F32)
            kld = sb('kld', [128, 16, 128], F32)
            iota16 = sb('iota16_sb', [128, 16], F32)
            iota128 = sb('iota128_sb', [128, 128], F32)
            qTt = sb('qTt', [128, 16, 128], F32)
            S = sb('S', [128, 16, 128], F32)
            S2 = sb('S2', [128, 16, 128], F32)
            V16 = sb('V16', [128, 16, 16], F32)
            I16 = sb('I16', [128, 16, 16], U32)
            I16f = sb('I16f', [128, 16, 16], F32)
            cand = sb('cand', [128, 8, 256], F32)
            cand2 = sb('cand2', [128, 8, 256], F32)
            TS = sb('TS', [128, 8, 16], F32)
            P16 = sb('P16', [128, 8, 16], U32)
            Pi = sb('Pi', [128, 8, 16], U32)
            Pj = sb('Pj', [128, 8, 16], U32)
            Pif = sb('Pif', [128, 8, 16], F32)
            Pjf = sb('Pjf', [128, 8, 16], F32)
            eq = sb('eq', [128, 8, 16, 16], F32)
            ia = sb('ia', [128, 8, 16], F32)
            ib = sb('ib', [128, 8, 16], F32)
            nmax = sb('nmax', [128, 8], F32)
            Gt = sb('Gt', [128, 8, 16], F32)
            Z = sb('Z', [128, 8], F32)
            tr3 = sb('tr3', [128, 3, 128], F32)
            At = [sb('At%d' % i, [128, 128], F32) for i in range(4)]
            Bt = [sb('Bt%d' % i, [128, 128], F32) for i in range(8)]
            nb4 = sb('nb4', [128, 128], F32)
            Gst = [sb('Gst%d' % i, [128, 128, 128], BF16) for i in range(2)]
            kb.dma('sp', lambda e: e.dma_start(out=iota16[:], in_=t.iota16_d.ap()), w=['iota16'])
            kb.dma('sp', lambda e: e.dma_start(out=iota128[:], in_=t.iota128_d.ap()), w=['iota128'])
            kb.dma('sp', lambda e: e.dma_start(out=kld[:], in_=t.peer_keys.ap().rearrange("c n d -> n c d")), w=['kld'])
            for g in range(4):
                ps = t.ps[g]
                for i in range(4):
                    c = g * 4 + i
                    kb.op('pe', lambda e: e.transpose(ps[:, i * 128:(i + 1) * 128], kld[:, c, :], t.ident[:]), r=['kld', 'ident'], w=[('ps', g)])
                kb.op('act', lambda e: e.activation(out=keysT[:, g * 4:(g + 1) * 4, :], in_=ps[:].rearrange("p (a b) -> p a b", b=128), func=AF.Identity),
                      r=[('ps', g)], w=['keysT'])
            gsv = t.G_scr.ap().rearrange("a b n -> b a n")
            gq = 0
            for tt in range(8):
                kb.dma('sp', lambda e: e.dma_start(out=qTt[:], in_=t.qT_scr.ap()[:, :, tt * 128:(tt + 1) * 128].rearrange("c d n -> d c n")),
                       r=['qT_scr'], w=['qTt'])
                for g in range(4):
                    ps = t.ps[4 + g]
                    for i in range(4):
                        c = g * 4 + i
                        kb.op('pe', lambda e: e.matmul(ps[:, i * 128:(i + 1) * 128], lhsT=qTt[:, c, :], rhs=keysT[:, c, :], start=True, stop=True),
                              r=['qTt', 'keysT'], w=[('ps', 4 + g)])
                    kb.op('act', lambda e: e.activation(out=S[:, g * 4:(g + 1) * 4, :], in_=ps[:].rearrange("p (a b) -> p a b", b=128), func=AF.Identity),
                          r=[('ps', 4 + g)], w=['S'])
                for c in range(16):
                    kb.op('dve', lambda e: e.max(out=V16[:, c, 0:8], in_=S[:, c, :]), r=['S'], w=['V16'])
                    kb.op('dve', lambda e: e.max_index(out=I16[:, c, 0:8], in_max=V16[:, c, 0:8], in_values=S[:, c, :]), r=['S', 'V16'], w=['I16'])
                    kb.op('dve', lambda e: e.match_replace(out=S2[:, c, :], in_to_replace=V16[:, c, 0:8], in_values=S[:, c, :], imm_value=-1e30),
                          r=['S', 'V16'], w=['S2'])
                    kb.op('dve', lambda e: e.max(out=V16[:, c, 8:16], in_=S2[:, c, :]), r=['S2'], w=['V16'])
                    kb.op('dve', lambda e: e.max_index(out=I16[:, c, 8:16], in_max=V16[:, c, 8:16], in_values=S2[:, c, :]), r=['S2', 'V16'], w=['I16'])
                kb.op('dve', lambda e: e.tensor_copy(out=I16f[:], in_=I16[:]), r=['I16'], w=['I16f'])
                va = bass.AP(V16[:].tensor, V16[:].offset, [pst(V16[:]), [32, 8], [1, 16], [0, 16]])
                vb = bass.AP(V16[:].tensor, V16[:].offset + 16, [pst(V16[:]), [32, 8], [0, 16], [1, 16]])
                kb.op('dve', lambda e: e.tensor_tensor(out=cand[:].rearrange("p h (i j) -> p h i j", j=16), in0=va, in1=vb, op=ALU.add),
                      r=['V16'], w=['cand'])
                for h in range(8):
                    kb.op('dve', lambda e: e.max(out=TS[:, h, 0:8], in_=cand[:, h, :]), r=['cand'], w=['TS'])
                    kb.op('dve', lambda e: e.max_index(out=P16[:, h, 0:8], in_max=TS[:, h, 0:8], in_values=cand[:, h, :]), r=['cand', 'TS'], w=['P16'])
                    kb.op('dve', lambda e: e.match_replace(out=cand2[:, h, :], in_to_replace=TS[:, h, 0:8], in_values=cand[:, h, :], imm_value=-1e30),
                          r=['cand', 'TS'], w=['cand2'])
                    kb.op('dve', lambda e: e.max(out=TS[:, h, 8:16], in_=cand2[:, h, :]), r=['cand2'], w=['TS'])
                    kb.op('dve', lambda e: e.max_index(out=P16[:, h, 8:16], in_max=TS[:, h, 8:16], in_values=cand2[:, h, :]), r=['cand2', 'TS'], w=['P16'])
                kb.op('dve', lambda e: e.tensor_single_scalar(out=Pi[:], in_=P16[:], scalar=4, op=ALU.logical_shift_right), r=['P16'], w=['Pi'])
                kb.op('dve', lambda e: e.tensor_single_scalar(out=Pj[:], in_=P16[:], scalar=15, op=ALU.bitwise_and), r=['P16'], w=['Pj'])
                kb.op('dve', lambda e: e.tensor_copy(out=Pif[:], in_=Pi[:]), r=['Pi'], w=['Pif'])
                kb.op('dve', lambda e: e.tensor_copy(out=Pjf[:], in_=Pj[:]), r=['Pj'], w=['Pjf'])
                io = bass.AP(iota16[:].tensor, iota16[:].offset, [pst(iota16[:]), [0, 8], [0, 16], [1, 16]])
                for (Pf, pk, off, dst, dk) in ((Pif, 'Pif', 0, ia, 'ia'), (Pjf, 'Pjf', 16, ib, 'ib')):
                    pfb = bass.AP(Pf[:].tensor, Pf[:].offset, [pst(Pf[:]), [16, 8], [1, 16], [0, 16]])
                    ifb = bass.AP(I16f[:].tensor, I16f[:].offset + off, [pst(I16f[:]), [32, 8], [0, 16], [1, 16]])
                    kb.op('dve', lambda e: e.tensor_tensor(out=eq[:], in0=io, in1=pfb, op=ALU.is_equal), r=['iota16', pk], w=['eq'])
                    kb.op('dve', lambda e: e.tensor_tensor(out=eq[:], in0=eq[:], in1=ifb, op=ALU.mult), r=['eq', 'I16f'], w=['eq'])
                    kb.op('dve', lambda e: e.tensor_reduce(out=dst[:], in_=eq[:], axis=AX.X, op=ALU.add), r=['eq'], w=[dk])
                kb.op('dve', lambda e: e.tensor_scalar(out=nmax[:], in0=TS[:, :, 0], scalar1=-1.0, scalar2=None, op0=ALU.mult), r=['TS'], w=['nmax'])
                for h in range(8):
                    kb.op('act', lambda e: e.activation(out=Gt[:, h, :], in_=TS[:, h, :], func=AF.Exp, bias=nmax[:, h:h + 1], accum_out=Z[:, h:h + 1]),
                          r=['TS', 'nmax'], w=['Gt', 'Z'])
                kb.op('dve', lambda e: e.reciprocal(out=Z[:], in_=Z[:]), r=['Z'], w=['Z'])
                zb = bass.AP(Z[:].tensor, Z[:].offset, [pst(Z[:]), [1, 8], [0, 16]])
                kb.op('dve', lambda e: e.tensor_tensor(out=Gt[:], in0=Gt[:], in1=zb, op=ALU.mult), r=['Gt', 'Z'], w=['Gt'])
                psx = t.ps[0]
                for i, (src, sk) in enumerate(((ia, 'ia'), (ib, 'ib'), (Gt, 'Gt'))):
                    kb.op('pe', lambda e: e.transpose(psx[:, i * 128:(i + 1) * 128], src[:].rearrange("p h k -> p (h k)"), t.ident[:]),
                          r=[sk, 'ident'], w=[('ps', 0)])
                kb.op('act', lambda e: e.activation(out=tr3[:], in_=psx[:, 0:384].rearrange("p (a b) -> p a b", b=128), func=AF.Identity),
                      r=[('ps', 0)], w=['tr3'])
                gs_, gk = Gst[tt % 2], ('Gst', tt % 2)
                kb.op('dve', lambda e: e.tensor_scalar(out=nb4[:], in0=tr3[:, 1, :], scalar1=-4.0, scalar2=None, op0=ALU.mult), r=['tr3'], w=['nb4'])

                def emit_B(tok):
                    bb_ = tok % 8
                    kb.op('act', lambda e: e.activation(out=Bt[bb_][:], in_=iota128[:], func=AF.Derivative_Erf, scale=4.0, bias=nb4[:, tok:tok + 1]),
                          r=['iota128', 'nb4'], w=[('Bt', bb_)])

                for tok in range(4):
                    emit_B(tok)
                for g4 in range(32):
                    pg = 1 + (gq % 2)
                    psg = t.ps[pg]
                    for tok in range(g4 * 4, g4 * 4 + 4):
                        ab = tok % 4
                        kb.op('dve', lambda e: e.tensor_scalar(out=At[ab][:], in0=iota128[:], scalar1=tr3[:, 0, tok:tok + 1], scalar2=tr3[:, 2, tok:tok + 1],
                                                               op0=ALU.is_equal, op1=ALU.mult), r=['iota128', 'tr3'], w=[('At', ab)])
                        oap = bass.AP(psg[:].tensor, psg[:].offset + ab, [pst(psg[:]), [4, 128]])
                        kb.op('pe', lambda e: e.matmul(oap, lhsT=Bt[tok % 8][:], rhs=At[ab][:], start=True, stop=True),
                              r=[('At', ab), ('Bt', tok % 8)], w=[('ps', pg)])
                    if g4 + 1 < 32:
                        for tok in range((g4 + 1) * 4, (g4 + 1) * 4 + 4):
                            emit_B(tok)
                    kb.op('act', lambda e: e.activation(out=gs_[:, :, g4 * 4:(g4 + 1) * 4], in_=psg[:].rearrange("p (a b) -> p a b", b=4),
                                                        func=AF.Identity, scale=0.8862269254527580), r=[('ps', pg)], w=[gk])
                    gq += 1
                kb.dma('sp', lambda e: e.dma_start(out=gsv[:, :, tt * 128:(tt + 1) * 128], in_=gs_[:]), r=[gk], w=['G_scr'])
        kb.barrier()
        acc = es0.enter_context(nc.sbuf_tensor('acc', [128, 8, 2048], F32))
        GRP = 4
        with ExitStack() as es:
            sb = lambda name, shape, dt: es.enter_context(nc.sbuf_tensor(name, shape, dt))
            GA = [sb('GA%d' % i, [128, GRP, 1024], BF16) for i in range(2)]
            Vb = [sb('Vb%d' % i, [128, GRP, 2048], BF16) for i in range(2)]
            Ur = [sb('Ur%d' % i, [128, 2048], F32) for i in range(3)]
            UT = [sb('UT%d' % i, [128, 16, 128], BF16) for i in range(2)]
            gsb = [sb('gsb%d' % i, [128, 1024], BF16) for i in range(2)]
            Gl = [sb('Gl%d' % i, [128, 1024], BF16) for i in range(2)]
            state = {'n': 0, 'ev': 0}

            def emit_scores(grp):
                gb_ = grp % 2
                for ai in range(GRP):
                    a = grp * GRP + ai
                    nb = state['n'] % 3
                    state['n'] += 1
                    kb.dma('sp', lambda e: e.dma_start(out=Ur[nb][:], in_=t.peer_u.ap()[a * 128:(a + 1) * 128, :]), w=[('Ur', nb)])
                    kb.dma('pool', lambda e: e.dma_start(out=Vb[gb_][:, ai, :], in_=t.peer_v.ap()[a * 128:(a + 1) * 128, :]), w=[('Vb', gb_)])
                    kb.dma('sp', lambda e: e.dma_start(out=Gl[nb % 2][:], in_=t.G_scr.ap()[a]), r=['G_scr'], w=[('Gl', nb % 2)])
                    for g in range(4):
                        p = 4 + g
                        ps = t.ps[p]
                        for i in range(4):
                            k = g * 4 + i
                            kb.op('pe', lambda e: e.transpose(ps[:, i * 128:(i + 1) * 128], Ur[nb][:, k * 128:(k + 1) * 128], t.ident[:]),
                                  r=[('Ur', nb), 'ident'], w=[('ps', p)])
                        eng = 'act' if state['ev'] % 2 == 0 else 'dve'
                        state['ev'] += 1
                        if eng == 'act':
                            kb.op('act', lambda e: e.activation(out=UT[nb % 2][:, g * 4:(g + 1) * 4, :], in_=ps[:].rearrange("p (a b) -> p a b", b=128),
                                                                func=AF.Identity), r=[('ps', p)], w=[('UT', nb % 2)])
                        else:
                            kb.op('dve', lambda e: e.tensor_copy(out=UT[nb % 2][:, g * 4:(g + 1) * 4, :], in_=ps[:].rearrange("p (a b) -> p a b", b=128)),
                                  r=[('ps', p)], w=[('UT', nb % 2)])
                    for tb in range(2):
                        p = 2 + tb
                        ps = t.ps[p]
                        for k in range(16):
                            kb.op('pe', lambda e: e.matmul(ps[:], lhsT=UT[nb % 2][:, k, :], rhs=u2T[:, k, tb * 512:(tb + 1) * 512],
                                                           start=(k == 0), stop=(k == 15)), r=[('UT', nb % 2), 'u2T'], w=[('ps', p)])
                        kb.op('act', lambda e: e.activation(out=gsb[nb % 2][:, tb * 512:(tb + 1) * 512], in_=ps[:], func=AF.Gelu),
                              r=[('ps', p)], w=[('gsb', nb % 2)])
                    kb.op('dve', lambda e: e.tensor_tensor(out=GA[gb_][:, ai, :], in0=gsb[nb % 2][:], in1=Gl[nb % 2][:], op=ALU.mult),
                          r=[('gsb', nb % 2), ('Gl', nb % 2)], w=[('GA', gb_)])

            def emit_out(grp):
                gb_ = grp % 2
                for tt in range(8):
                    for db in range(4):
                        p = (tt * 4 + db) % 2
                        ps = t.ps[p]
                        for ai in range(GRP):
                            kb.op('pe', lambda e: e.matmul(ps[:], lhsT=GA[gb_][:, ai, tt * 128:(tt + 1) * 128], rhs=Vb[gb_][:, ai, db * 512:(db + 1) * 512],
                                                           start=(ai == 0), stop=(ai == GRP - 1)), r=[('GA', gb_), ('Vb', gb_)], w=[('ps', p)])
                        ak = ('acc', tt * 4 + db)
                        if grp == 0:
                            kb.op('act', lambda e: e.activation(out=acc[:, tt, db * 512:(db + 1) * 512], in_=ps[:], func=AF.Identity), r=[('ps', p)], w=[ak])
                        else:
                            kb.op('dve', lambda e: e.tensor_tensor(out=acc[:, tt, db * 512:(db + 1) * 512], in0=ps[:], in1=acc[:, tt, db * 512:(db + 1) * 512],
                                                                   op=ALU.add), r=[('ps', p), ak], w=[ak])

            NG = 128 // GRP
            emit_scores(0)
            for grp in range(NG):
                if grp + 1 < NG:
                    emit_scores(grp + 1)
                emit_out(grp)
        kb.barrier()
        with ExitStack() as es:
            sb = lambda name, shape, dt: es.enter_context(nc.sbuf_tensor(name, shape, dt))
            g2b = sb('g2b', [128, 2048], F32)
            l2g = sb('l2g', [128, 2048], F32)
            l2b = sb('l2b', [128, 2048], F32)
            x1t = [sb('x1t%d' % i, [128, 2048], F32) for i in range(2)]
            rr = [sb('rr2_%d' % i, [128, 2048], F32) for i in range(2)]
            st = sb('st2', [128, 4, 6], F32)
            mv = sb('mv2', [128, 2], F32)
            rs = sb('rs2', [128, 1], F32)
            kb.dma('sp', lambda e: e.dma_start(out=g2b[:], in_=bcast_row(t.m_scr, 5 * D, D)), r=['m_scr'], w=['g2b'])
            kb.dma('sp', lambda e: e.dma_start(out=l2g[:], in_=bcast_row(t.ln2_g, 0, D)), w=['l2g'])
            kb.dma('sp', lambda e: e.dma_start(out=l2b[:], in_=bcast_row(t.ln2_b, 0, D)), w=['l2b'])
            AK = keys('acc', 32)
            for tt in range(8):
                xb, xk = x1t[tt % 2], ('x1t', tt % 2)
                r, rk = rr[tt % 2], ('rr2', tt % 2)
                kb.dma('sp', lambda e: e.dma_start(out=xb[:], in_=t.x1_scr.ap()[tt * 128:(tt + 1) * 128, :]), r=['x1_scr'], w=[xk])
                kb.op('pool', lambda e: e.tensor_tensor(out=r[:], in0=acc[:, tt, :], in1=g2b[:], op=ALU.mult), r=AK + ['g2b'], w=[rk])
                kb.op('dve', lambda e: e.scalar_tensor_tensor(out=r[:], in0=xb[:], scalar=ALPHA, in1=r[:], op0=ALU.mult, op1=ALU.add), r=[xk, rk], w=[rk])
                layer_norm_tile(kb, nc, r, rk, st, mv, rs, l2g, 'l2g', l2b, 'l2b', 'ln2')
                kb.dma('sp', lambda e: e.dma_start(out=t.out.ap()[tt * 128:(tt + 1) * 128, :], in_=r[:]), r=[rk], w=['out'])


ALL_STAGES = ('ada', 'uT', 'proj', 'bias', 'att', 'filt', 'conv', 'merge', 'peer')


def build_program(stages=ALL_STAGES, debug=False):
    nc = bass.Bass("TRN2", target_bir_lowering=False)
    t = T()
    t.debug = debug
    dt_in = lambda name, shape, dt=F32: nc.dram_tensor(name, shape, dt, kind="ExternalInput")
    scr_kind = "ExternalOutput" if debug else "Internal"
    dt_scr = lambda name, shape, dt=F32: nc.dram_tensor(name, shape, dt, kind=scr_kind)
    t.xr = dt_in('xr', [L, D])
    t.c2 = dt_in('c2', [2, D])
    t.ctxb = dt_in('ctxb', [CTX, D])
    t.w_ada = dt_in('w_ada', [D, 6 * D])
    t.b_ada = dt_in('b_ada', [1, 6 * D])
    t.w_in = dt_in('w_in', [D, 6144])
    t.conv_w = dt_in('conv_w', [3, 3072])
    t.conv_b = dt_in('conv_b', [1, 3072])
    t.e01 = dt_in('e01', [1, 2])
    t.ident_d = dt_in('ident', [128, 128])
    t.na_rpb = dt_in('na_rpb', [16, 15, 31])
    t.dhot_d = dt_in('dhot', [32, 4096])
    t.rowmask = dt_in('rowmask', [64, 7, 12, 64])
    t.na_norm_g = dt_in('na_norm_g', [1, 1024])
    t.zT_d = dt_in('zT', [33, 2048])
    t.E_d = dt_in('Edec', [L, 1024])
    t.Wf_d = dt_in('Wf', [32, 128, 16, 128], BF16)
    t.Wi_d = dt_in('Wi', [16, 128, 32, 128], BF16)
    t.filt_w1 = dt_in('filt_w1', [33, 64])
    t.filt_w2 = dt_in('filt_w2', [64, 64])
    t.filt_w3 = dt_in('filt_w3', [64, 64])
    t.filt_w4 = dt_in('filt_w4', [64, 4096])
    t.filt_b = dt_in('filt_b', [3, 64])
    t.filt_freq = dt_in('filt_freq', [3, 64])
    t.hy_skip = dt_in('hy_skip', [1, 2048])
    t.hy_norm_g = dt_in('hy_norm_g', [1, 1024])
    t.w_out = dt_in('w_out', [D, D])
    t.ln1_g = dt_in('ln1_g', [1, D])
    t.ln1_b = dt_in('ln1_b', [1, D])
    t.ln2_g = dt_in('ln2_g', [1, D])
    t.ln2_b = dt_in('ln2_b', [1, D])
    t.peer_wq = dt_in('peer_wq', [D, D])
    t.peer_keys = dt_in('peer_keys', [16, 128, 128])
    t.peer_u = dt_in('peer_u', [16384, D])
    t.peer_v = dt_in('peer_v', [16384, D])
    t.iota16_d = dt_in('iota16', [128, 16])
    t.iota128_d = dt_in('iota128', [128, 128])
    t.out = nc.dram_tensor('out', [LH, D], F32, kind="ExternalOutput")
    t.m_scr = dt_scr('m_scr', [2, 6 * D])
    t.hz_tok = dt_scr('hz_tok', [L, 3072])
    t.QT_scr = dt_scr('QT_scr', [16, 64, 1024], BF16)
    t.KT_scr = dt_scr('KT_scr', [16, 64, NTOK], BF16)
    t.V_scr = dt_scr('V_scr', [NTOK, 1024], BF16)
    t.MBT_scr = dt_scr('MBT_scr', [240, 4096])
    t.Kf_scr = dt_scr('Kf_scr', [2, 4096, 1024])
    t.hy_scr = dt_scr('hy_scr', [LH, 1024])
    t.x1_scr = dt_scr('x1_scr', [LH, D])
    t.u2_scr = dt_scr('u2_scr', [LH, D])
    t.qT_scr = dt_scr('qT_scr', [16, 128, LH])
    t.G_scr = dt_scr('G_scr', [128, 128, LH], BF16)
    if debug:
        t.na_dbg = dt_scr('na_dbg', [LH, 1024])
    with ExitStack() as es:
        es.enter_context(nc.allow_non_contiguous_dma(reason="small strided parameter loads"))
        es.enter_context(nc.allow_low_precision(reason="bf16 matmul operands"))
        kb = KB(nc, es)
        t.ps = [es.enter_context(nc.psum_tensor('ps%d' % i, [128, 512], F32)) for i in range(8)]
        t.ident = es.enter_context(nc.sbuf_tensor('ident_sb', [128, 128], F32))
        kb.dma('sp', lambda e: e.dma_start(out=t.ident[:], in_=t.ident_d.ap()), w=['ident'])
        if 'ada' in stages:
            stage_ada(kb, t)
        kb.barrier()
        with ExitStack() as es2:
            uT = es2.enter_context(nc.sbuf_tensor('uT', [128, 16, NTOK], BF16))
            if 'uT' in stages:
                stage_uT(kb, t, uT)
                kb.barrier()
            if 'proj' in stages:
                stage_proj(kb, t, uT)
        kb.barrier()
        if 'bias' in stages:
            stage_bias(kb, t)
        kb.barrier()
        with ExitStack() as es3:
            mergedT = es3.enter_context(nc.sbuf_tensor('mergedT', [128, 16, LH], BF16))
            if 'att' in stages:
                stage_att(kb, t, mergedT)
            kb.barrier()
            if 'filt' in stages:
                stage_filters(kb, t)
            kb.barrier()
            if 'conv' in stages:
                stage_conv(kb, t)
            kb.barrier()
            if 'merge' in stages:
                stage_merge_a(kb, t, mergedT)
                kb.barrier()
                stage_merge_b(kb, t, mergedT)
                kb.barrier()
        u2T = es.enter_context(nc.sbuf_tensor('u2T', [128, 16, LH], BF16))
        if 'merge' in stages:
            stage_peer_q(kb, t, u2T)
            kb.barrier()
        if 'peer' in stages:
            stage_peer(kb, t, u2T)
        kb.finish(['m_scr', 'hz_tok', 'QT_scr', 'KT_scr', 'V_scr', 'MBT_scr', 'na_dbg', 'Kf_scr', 'hy_scr', 'x1_scr', 'u2_scr', 'qT_scr', 'G_scr', 'out'])
        print("instructions:", kb.ninst)
    return nc


def host_consts(half):
    c = {}
    dh = np.zeros((32, 64, 64), np.float32)
    for kc in range(64):
        for q in range(64):
            d = kc - q + 15
            if 0 <= d <= 30:
                dh[d, kc, q] = 1.0
            ws = min(max(q - 8, 0), 48)
            dh[31, kc, q] = 0.0 if ws <= kc < ws + 16 else NEG
    c['dhot'] = dh.reshape(32, 4096)
    rm = np.full((7, 12), NEG, np.float32)
    for ci, i in enumerate([0, 1, 2, 3, 13, 14, 15]):
        for k in range(12):
            if i < 4:
                ok = (4 <= k < 12) if half == 0 else (i <= k < i + 8)
            else:
                ok = (i - 12 <= k < i - 4) if half == 0 else (0 <= k < 8)
            if ok:
                rm[ci, k] = 0.0
    T0 = half * LH
    sg = (np.arange(L) + T0) % L
    tt_ = np.linspace(0.0, 1.0, L, dtype=np.float32)[sg][:, None]
    wv_ = (2.0 * np.pi * sg.astype(np.float32) / L).astype(np.float32)[:, None]
    ff_ = np.linspace(1e-4, 15.0, 16, dtype=np.float32)[None, :]
    zz = np.concatenate([tt_, np.cos(ff_ * wv_), -np.sin(ff_ * wv_)], axis=-1).astype(np.float32)
    c['zT'] = np.ascontiguousarray(zz.T)
    deltas = np.abs(np.linspace(np.log(1e-2) / 1.5, np.log(1e-2) / 0.3, DHY, dtype=np.float32))
    c['Edec'] = np.exp(-tt_ * deltas[None, :]).astype(np.float32)
    rr = np.arange(NFFT)
    fq = np.where(rr <= 2048, rr, rr - 2048).astype(np.int64)
    ang = 2.0 * np.pi * ((sg[:, None].astype(np.int64) * fq[None, :]) % NFFT) / NFFT
    Wfull = np.where(rr[None, :] <= 2048, np.cos(ang), -np.sin(ang))
    c['Wf'] = np.ascontiguousarray(Wfull.reshape(16, 128, 32, 128).transpose(2, 1, 0, 3)).astype(ml_dtypes.bfloat16)
    wgt = np.where((rr == 0) | (rr == 2048), 1.0, 2.0) / NFFT
    Winv = (np.where(rr[None, :] <= 2048, np.cos(ang), -np.sin(ang)) * wgt[None, :]).T
    c['Wi'] = np.ascontiguousarray(Winv.reshape(32, 128, 16, 128).transpose(2, 1, 0, 3)).astype(ml_dtypes.bfloat16)
    c['rowmask'] = np.ascontiguousarray(np.broadcast_to(rm[None, :, :, None], (64, 7, 12, 64))).astype(np.float32)
    return c


def make_core_inputs(inputs, b, half):
    T0 = half * LH
    f = lambda a: np.ascontiguousarray(a, dtype=np.float32)
    m = {}
    m['xr'] = f(np.roll(inputs['x'][b], -T0, axis=0))
    m['c2'] = f(np.stack([inputs['c'][b], inputs['c_ctx']]))
    m['ctxb'] = f(inputs['ctx'][b])
    m['w_ada'] = f(inputs['w_ada'][0])
    m['b_ada'] = f(inputs['b_ada'][0][None])
    m['w_in'] = f(inputs['w_in'][0])
    m['conv_w'] = f(inputs['conv_w'][0])
    m['conv_b'] = f(inputs['conv_b'][0][None])
    m['e01'] = np.array([[1.0, 0.0]] if half == 0 else [[0.0, 1.0]], dtype=np.float32)
    m['ident'] = np.eye(128, dtype=np.float32)
    m['na_rpb'] = f(inputs['na_rpb'][0])
    m['na_norm_g'] = f(inputs['na_norm_g'][0][None])
    for k_ in ('filt_w1', 'filt_w2', 'filt_w3', 'filt_w4', 'filt_freq'):
        m[k_] = f(inputs[k_][0])
    m['filt_b'] = f(np.stack([inputs['filt_b1'][0], inputs['filt_b2'][0], inputs['filt_b3'][0]]))
    m['hy_skip'] = f(inputs['hy_skip'][0].reshape(1, 2048))
    m['hy_norm_g'] = f(inputs['hy_norm_g'][0][None])
    m['w_out'] = f(inputs['w_out'][0])
    for k_ in ('ln1_g', 'ln1_b', 'ln2_g', 'ln2_b'):
        m[k_] = f(inputs[k_][0][None])
    m['peer_wq'] = f(inputs['peer_wq'][0])
    m['peer_keys'] = f(inputs['peer_keys'][0].reshape(16, 128, 128))
    m['peer_u'] = f(inputs['peer_u'][0])
    m['peer_v'] = f(inputs['peer_v'][0])
    m['iota16'] = np.ascontiguousarray(np.broadcast_to(np.arange(16, dtype=np.float32)[None], (128, 16)))
    m['iota128'] = np.ascontiguousarray(np.broadcast_to(np.arange(128, dtype=np.float32)[None], (128, 128)))
    m.update(host_consts(half))
    return m


def kernel(**inputs):
    nc = build_program()
    in_maps = [make_core_inputs(inputs, c // 2, c % 2) for c in range(8)]
    res = run_bass_kernel_spmd(nc, in_maps, core_ids=list(range(8)))
    out = np.zeros((4, L, D), dtype=np.float32)
    for c in range(8):
        b, half = c // 2, c % 2
        out[b, half * LH:(half + 1) * LH] = res.results[c]['out']
    return out
```

```python
import numpy as np
import ml_dtypes
from contextlib import ExitStack
import concourse.bass as bass
import concourse.mybir as mybir
from concourse.bass_utils import run_bass_kernel_spmd

F32 = mybir.dt.float32
BF16 = mybir.dt.bfloat16
U32 = mybir.dt.uint32
I32 = mybir.dt.int32
AF = mybir.ActivationFunctionType
ALU = mybir.AluOpType
AX = mybir.AxisListType

D = 2048
L = 2048
LH = 1024
CTX = 256
NTOK = 2560
DHY = 1024
NEG = -30000.0
ALPHA = 2.0 ** 0.25
LN_EPS = 1e-5
NFFT = 4096
STRICT_SAME_ENGINE = True


class KB:
    NDS = 12

    def __init__(self, nc, es):
        self.nc = nc
        self.E = {'pe': nc.tensor, 'act': nc.scalar, 'dve': nc.vector, 'pool': nc.gpsimd, 'sp': nc.sync}
        self.sem = {e: es.enter_context(nc.semaphore('sem_' + e)) for e in ('pe', 'act', 'dve', 'pool')}
        self.cnt = dict.fromkeys(self.sem, 0)
        self.dsem = {q: [es.enter_context(nc.semaphore('dq_%s_%d' % (q, i))) for i in range(self.NDS)]
                     for q in ('sp', 'pool')}
        self.dcnt = {q: [0] * self.NDS for q in ('sp', 'pool')}
        self.dnext = {'sp': 0, 'pool': 0}
        self.seen = {e: {} for e in self.E}
        self.lastw = {}
        self.readers = {}
        self.ninst = 0

    def _wait(self, e, tok):
        sem, val = tok
        if e in self.sem and sem is self.sem[e] and (e == 'pe' or not STRICT_SAME_ENGINE):
            return
        if self.seen[e].get(id(sem), 0) >= val:
            return
        self.E[e].wait_ge(sem, val)
        self.seen[e][id(sem)] = val

    def _deps(self, e, r, w):
        for key in r:
            t = self.lastw.get(key)
            if t:
                self._wait(e, t)
        for key in w:
            t = self.lastw.get(key)
            if t:
                self._wait(e, t)
            for t in self.readers.get(key, {}).values():
                self._wait(e, t)

    def _record(self, tok, r, w):
        for key in r:
            d = self.readers.setdefault(key, {})
            old = d.get(id(tok[0]))
            if old is None or old[1] < tok[1]:
                d[id(tok[0])] = tok
        for key in w:
            self.lastw[key] = tok
            self.readers[key] = {}

    def op(self, e, fn, r=(), w=()):
        self._deps(e, r, w)
        inst = fn(self.E[e])
        self.cnt[e] += 1
        inst.then_inc(self.sem[e], 1)
        self._record((self.sem[e], self.cnt[e]), r, w)
        self.ninst += 1

    def dma(self, q, fn, r=(), w=()):
        slot = self.dnext[q] % self.NDS
        self.dnext[q] += 1
        sem = self.dsem[q][slot]
        if self.dcnt[q][slot] > 0:
            self._wait(q, (sem, 16 * self.dcnt[q][slot]))
        self._deps(q, r, w)
        inst = fn(self.E[q])
        self.dcnt[q][slot] += 1
        inst.then_inc(sem, 16)
        self._record((sem, 16 * self.dcnt[q][slot]), r, w)
        self.ninst += 1

    def barrier(self):
        toks = [(self.sem[e], self.cnt[e]) for e in self.sem if self.cnt[e] > 0]
        for q in self.dsem:
            for i, sem in enumerate(self.dsem[q]):
                if self.dcnt[q][i] > 0:
                    toks.append((sem, 16 * self.dcnt[q][i]))
        for e in self.E:
            for tok in toks:
                self._wait(e, tok)

    def finish(self, keys):
        for key in keys:
            t = self.lastw.get(key)
            if t:
                self._wait('sp', t)


class T:
    pass


def keys(name, n):
    return [(name, i) for i in range(n)]


def stage_ada(kb, t):
    nc = kb.nc
    with ExitStack() as es:
        sb = lambda name, shape, dt: es.enter_context(nc.sbuf_tensor(name, shape, dt))
        cT = sb('cT', [128, 2, 16], F32)
        scT = sb('scT', [128, 16, 2], F32)
        wbuf = [sb('wada%d' % i, [128, 16, 512], F32) for i in range(2)]
        bada = sb('bada', [2, 12288], F32)
        mall = sb('mall', [2, 12288], F32)
        kb.dma('sp', lambda e: e.dma_start(out=cT[:], in_=t.c2.ap().rearrange("n (k p) -> p n k", p=128)), w=['cT'])
        kb.op('act', lambda e: e.activation(out=scT[:].rearrange("p k n -> p n k"), in_=cT[:], func=AF.Silu),
              r=['cT'], w=['scT'])
        kb.dma('sp', lambda e: e.dma_start(out=bada[:], in_=bass.AP(t.b_ada, 0, [[0, 2], [1, 12288]])), w=['bada'])
        wv = t.w_ada.ap().rearrange("(k p) n -> p k n", p=128)
        for j in range(24):
            wb = wbuf[j % 2]
            kb.dma('sp', lambda e: e.dma_start(out=wb[:], in_=wv[:, :, j * 512:(j + 1) * 512]), w=[('wada', j % 2)])
            ps = t.ps[j % 2]
            for k in range(16):
                kb.op('pe', lambda e: e.matmul(ps[0:2, :], lhsT=scT[:, k, :], rhs=wb[:, k, :],
                                               start=(k == 0), stop=(k == 15)),
                      r=['scT', ('wada', j % 2)], w=[('ps', j % 2)])
            kb.op('dve', lambda e: e.tensor_tensor(out=mall[:, j * 512:(j + 1) * 512], in0=ps[0:2, :],
                                                   in1=bada[:, j * 512:(j + 1) * 512], op=ALU.add),
                  r=[('ps', j % 2), 'bada'], w=['mall'])
        kb.dma('sp', lambda e: e.dma_start(out=t.m_scr.ap(), in_=mall[:]), r=['mall'], w=['m_scr'])


def stage_uT(kb, t, uT):
    nc = kb.nc
    UK = keys('uT', 16)
    with ExitStack() as es:
        sb = lambda name, shape, dt: es.enter_context(nc.sbuf_tensor(name, shape, dt))
        mfm = sb('mfm', [128, 2, 6, 16], F32)
        sc1p = sb('sc1p', [128, 2, 16], F32)
        xg = [sb('xg%d' % i, [128, 4, 2048], F32) for i in range(2)]
        kb.dma('sp', lambda e: e.dma_start(out=mfm[:], in_=bass.AP(t.m_scr, 0, [[1, 128], [12288, 2], [2048, 6], [128, 16]])),
               r=['m_scr'], w=['mfm'])
        kb.op('dve', lambda e: e.tensor_scalar(out=sc1p[:], in0=mfm[:, :, 1, :], scalar1=1.0, scalar2=None, op0=ALU.add),
              r=['mfm'], w=['sc1p'])
        xv = t.xr.ap().rearrange("(g t p) d -> g p t d", t=4, p=128)
        pi = 0
        for g in range(5):
            xb = xg[g % 2]
            nt = 4 if g < 4 else 2
            if g < 4:
                kb.dma('sp', lambda e: e.dma_start(out=xb[:], in_=xv[g]), w=[('xg', g % 2)])
                n = 0
                col0 = 256 + g * 512
            else:
                kb.dma('sp', lambda e: e.dma_start(out=xb[:, 0:2, :], in_=t.ctxb.ap().rearrange("(t p) d -> p t d", p=128)),
                       w=[('xg', g % 2)])
                n = 1
                col0 = 2304
            for k in range(16):
                p = pi % 4
                pi += 1
                ps = t.ps[p]
                for tt in range(nt):
                    kb.op('pe', lambda e: e.transpose(ps[:, tt * 128:(tt + 1) * 128], xb[:, tt, k * 128:(k + 1) * 128], t.ident[:]),
                          r=[('xg', g % 2), 'ident'], w=[('ps', p)])
                kb.op('act', lambda e: e.activation(out=uT[:, k, col0:col0 + nt * 128], in_=ps[:, 0:nt * 128], func=AF.Identity,
                                                    scale=sc1p[:, n, k:k + 1], bias=mfm[:, n, 0, k:k + 1]),
                      r=[('ps', p), 'sc1p', 'mfm'], w=[('uT', k)])
        kb.op('pool', lambda e: e.tensor_copy(out=uT[:, :, 0:256], in_=uT[:, :, 2048:2304]), r=UK, w=UK)


def stage_proj(kb, t, uT):
    nc = kb.nc
    UK = keys('uT', 16)
    with ExitStack() as es:
        sb = lambda name, shape, dt: es.enter_context(nc.sbuf_tensor(name, shape, dt))
        wblk = [sb('wblk%d' % i, [128, 16, 512], BF16) for i in range(2)]
        cw = sb('cw', [128, 3, 24], F32)
        cb = sb('cb', [128, 24], F32)
        ee = sb('ee', [128, 2], F32)
        cwe = sb('cwe', [128, 4, 24], F32)
        zs = [sb('zs%d' % i, [128, 2050], F32) for i in range(2)]
        yc = [sb('yc%d' % i, [128, 2048], F32) for i in range(2)]
        ytok = [sb('ytok%d' % i, [128, 16, 128], F32) for i in range(2)]
        qst = [sb('qst%d' % i, [128, 2560], BF16) for i in range(2)]
        vst = [sb('vst%d' % i, [128, 512], BF16) for i in range(2)]
        kb.dma('sp', lambda e: e.dma_start(out=cw[:], in_=t.conv_w.ap().rearrange("k (j p) -> p k j", p=128)), w=['cw'])
        kb.dma('sp', lambda e: e.dma_start(out=cb[:], in_=t.conv_b.ap().rearrange("o (j p) -> p (o j)", p=128)), w=['cb'])
        kb.dma('sp', lambda e: e.dma_start(out=ee[:], in_=bass.AP(t.e01, 0, [[0, 128], [1, 2]])), w=['ee'])
        for i, (ei, tap) in enumerate([(0, 0), (0, 2), (1, 0), (1, 2)]):
            kb.op('dve', lambda e: e.tensor_scalar(out=cwe[:, i, :], in0=cw[:, tap, :], scalar1=ee[:, ei:ei + 1], scalar2=-1.0,
                                                   op0=ALU.mult, op1=ALU.mult), r=['cw', 'ee'], w=['cwe'])
        wv = t.w_in.ap().rearrange("(k p) n -> p k n", p=128)
        hzv = t.hz_tok.ap().rearrange("(tt p) c -> p tt c", p=128)
        state = {'ev': 0}

        def load_blk(blk):
            kb.dma('pool', lambda e: e.dma_start(out=wblk[blk % 2][:], in_=wv[:, :, blk * 512:(blk + 1) * 512]), w=[('wblk', blk % 2)])

        def emit_P(j):
            blk, cc = j // 4, j % 4
            if cc == 0:
                load_blk(blk)
            wb, wk = wblk[blk % 2], ('wblk', blk % 2)
            z, zk = zs[j % 2], ('zs', j % 2)
            for tb in range(4):
                ps = t.ps[tb]
                for k in range(16):
                    kb.op('pe', lambda e: e.matmul(ps[:], lhsT=wb[:, k, cc * 128:(cc + 1) * 128],
                                                   rhs=uT[:, k, 256 + tb * 512:256 + (tb + 1) * 512],
                                                   start=(k == 0), stop=(k == 15)),
                          r=[wk, ('uT', k)], w=[('ps', tb)])
                kb.op('act', lambda e: e.activation(out=z[:, 1 + tb * 512:1 + (tb + 1) * 512], in_=ps[:], func=AF.Identity),
                      r=[('ps', tb)], w=[zk])
            kb.op('act', lambda e: e.activation(out=z[:, 0:1], in_=z[:, 2048:2049], func=AF.Identity), r=[zk], w=[zk])
            kb.op('act', lambda e: e.activation(out=z[:, 2049:2050], in_=z[:, 1:2], func=AF.Identity), r=[zk], w=[zk])

        def emit_C(j):
            z, zk = zs[j % 2], ('zs', j % 2)
            y, yk_ = yc[j % 2], ('yc', j % 2)
            kb.op('dve', lambda e: e.tensor_scalar(out=y[:], in0=z[:, 1:2049], scalar1=cw[:, 1, j:j + 1],
                                                   scalar2=cb[:, j:j + 1], op0=ALU.mult, op1=ALU.add),
                  r=[zk, 'cw', 'cb'], w=[yk_])
            kb.op('dve', lambda e: e.scalar_tensor_tensor(out=y[:], in0=z[:, 0:2048], scalar=cw[:, 0, j:j + 1], in1=y[:],
                                                          op0=ALU.mult, op1=ALU.add), r=[zk, 'cw'], w=[yk_])
            kb.op('dve', lambda e: e.scalar_tensor_tensor(out=y[:], in0=z[:, 2:2050], scalar=cw[:, 2, j:j + 1], in1=y[:],
                                                          op0=ALU.mult, op1=ALU.add), r=[zk, 'cw'], w=[yk_])
            for (ycol, zcol, ci) in [(0, 0, 0), (2047, 2049, 1), (1024, 1024, 2), (1023, 1025, 3)]:
                kb.op('dve', lambda e: e.scalar_tensor_tensor(out=y[:, ycol:ycol + 1], in0=z[:, zcol:zcol + 1],
                                                              scalar=cwe[:, ci, j:j + 1], in1=y[:, ycol:ycol + 1],
                                                              op0=ALU.mult, op1=ALU.add), r=[zk, 'cwe'], w=[yk_])

        def emit_T(j):
            y, yk_ = yc[j % 2], ('yc', j % 2)
            yt = ytok[j % 2]
            yk = ('ytok', j % 2)
            for g in range(4):
                ps = t.ps[4 + g]
                for i in range(4):
                    tt = g * 4 + i
                    kb.op('pe', lambda e: e.transpose(ps[:, i * 128:(i + 1) * 128], y[:, tt * 128:(tt + 1) * 128], t.ident[:]),
                          r=[yk_, 'ident'], w=[('ps', 4 + g)])
                eng = 'act' if (state['ev'] % 2 == 0) else 'dve'
                state['ev'] += 1
                if eng == 'act':
                    kb.op('act', lambda e: e.activation(out=yt[:, g * 4:(g + 1) * 4, :], in_=ps[:].rearrange("p (a b) -> p a b", b=128),
                                                        func=AF.Identity), r=[('ps', 4 + g)], w=[yk])
                else:
                    kb.op('dve', lambda e: e.tensor_copy(out=yt[:, g * 4:(g + 1) * 4, :], in_=ps[:].rearrange("p (a b) -> p a b", b=128)),
                          r=[('ps', 4 + g)], w=[yk])
            kb.dma('sp', lambda e: e.dma_start(out=hzv[:, :, j * 128:(j + 1) * 128], in_=yt[:]), r=[yk], w=[('hz_tok', j)])

        emit_P(0)
        for j in range(24):
            if j + 1 < 24:
                emit_P(j + 1)
            emit_C(j)
            emit_T(j)
        for blk in range(6, 12):
            wb = wblk[blk % 2]
            wk = ('wblk', blk % 2)
            load_blk(blk)
            if False:
                pass
            elif blk < 10:
                isq = blk < 8
                for hp in range(4):
                    h = (blk % 2) * 8 + hp * 2
                    st = qst[hp % 2]
                    sk = ('qst', hp % 2)
                    nb = 2 if isq else 5
                    for tb in range(nb):
                        p = tb % 4
                        ps = t.ps[p]
                        c0 = 256 + tb * 512 if isq else tb * 512
                        for k in range(16):
                            kb.op('pe', lambda e: e.matmul(ps[:], lhsT=wb[:, k, hp * 128:(hp + 1) * 128], rhs=uT[:, k, c0:c0 + 512],
                                                           start=(k == 0), stop=(k == 15)), r=[wk, ('uT', k)], w=[('ps', p)])
                        kb.op('act', lambda e: e.activation(out=st[:, tb * 512:(tb + 1) * 512], in_=ps[:], func=AF.Identity),
                              r=[('ps', p)], w=[sk])
                    if isq:
                        kb.dma('sp', lambda e: e.dma_start(out=t.QT_scr.ap()[h:h + 2].rearrange("h d n -> (h d) n"), in_=st[:, 0:1024]), r=[sk], w=[('QT_scr', h)])
                    else:
                        kb.dma('sp', lambda e: e.dma_start(out=t.KT_scr.ap()[h:h + 2].rearrange("h d n -> (h d) n"), in_=st[:, 0:2560]), r=[sk], w=[('KT_scr', h)])
            else:
                vb = blk - 10
                for rr in range(20):
                    p = rr % 4
                    ps = t.ps[p]
                    for k in range(16):
                        kb.op('pe', lambda e: e.matmul(ps[:], lhsT=uT[:, k, rr * 128:(rr + 1) * 128], rhs=wb[:, k, :],
                                                       start=(k == 0), stop=(k == 15)), r=[wk, ('uT', k)], w=[('ps', p)])
                    st = vst[rr % 2]
                    sk = ('vst', rr % 2)
                    kb.op('act', lambda e: e.activation(out=st[:], in_=ps[:], func=AF.Identity), r=[('ps', p)], w=[sk])
                    kb.dma('sp', lambda e: e.dma_start(out=t.V_scr.ap()[rr * 128:(rr + 1) * 128, vb * 512:(vb + 1) * 512], in_=st[:]),
                           r=[sk], w=[('V_scr', rr, vb)])


def att_rows():
    rows = []
    for i in range(16):
        if i < 4:
            rows.append((i, 0, 12, 3 - i, i))
        elif i <= 12:
            rows.append((i, i, 8, 3, None))
        else:
            rows.append((i, 12, 12, 15 - i, 4 + i - 13))
    return rows


def stage_bias(kb, t):
    nc = kb.nc
    with ExitStack() as es:
        sb = lambda name, shape, dt: es.enter_context(nc.sbuf_tensor(name, shape, dt))
        rpbT = sb('rpbT', [32, 240], F32)
        dhot = sb('dhot_sb', [32, 4096], F32)
        mst = sb('mst', [120, 4096], F32)
        kb.op('dve', lambda e: e.memset(rpbT[:], 1.0), w=['rpbT'])
        kb.dma('sp', lambda e: e.dma_start(out=rpbT[0:31, :], in_=t.na_rpb.ap().rearrange("h r d -> d (h r)")), w=['rpbT'], r=['rpbT'])
        kb.dma('sp', lambda e: e.dma_start(out=dhot[:], in_=t.dhot_d.ap()), w=['dhot'])
        mv = t.MBT_scr.ap()
        for c in range(2):
            for nb in range(8):
                ps = t.ps[nb % 4]
                kb.op('pe', lambda e: e.matmul(ps[0:120, :], lhsT=rpbT[:, c * 120:(c + 1) * 120], rhs=dhot[:, nb * 512:(nb + 1) * 512],
                                               start=True, stop=True), r=['rpbT', 'dhot'], w=[('ps', nb % 4)])
                kb.op('act', lambda e: e.activation(out=mst[:, nb * 512:(nb + 1) * 512], in_=ps[0:120, :], func=AF.Identity),
                      r=[('ps', nb % 4)], w=['mst'])
            kb.dma('sp', lambda e: e.dma_start(out=mv[c * 120:(c + 1) * 120, :], in_=mst[:]), r=['mst'], w=['MBT_scr'])


def stage_att(kb, t, mergedT):
    nc = kb.nc
    scale = 64 ** -0.5
    with ExitStack() as es:
        sb = lambda name, shape, dt: es.enter_context(nc.sbuf_tensor(name, shape, dt))
        KT = [sb('KTh%d' % i, [64, NTOK], BF16) for i in range(2)]
        QT = [sb('QTh%d' % i, [64, 1024], BF16) for i in range(2)]
        VH = [sb('VHh%d' % i, [128, 40, 65], BF16) for i in range(2)]
        MB = [sb('MBh%d' % i, [128, 15, 64], F32) for i in range(2)]
        rm = sb('rm_sb', [128, 7, 6, 64], F32)
        na_all = sb('na_all', [64, 16, 1024], F32)
        tmpA = [sb('tmpA%d' % i, [128, 384], F32) for i in range(3)]
        PT = [sb('PT%d' % i, [128, 512], BF16) for i in range(3)]
        rz = [sb('rz%d' % i, [64, 1], F32) for i in range(3)]
        gna = sb('gna', [64, 1024], F32)
        ssq = sb('ssq', [64, 16], F32)
        sqj = sb('sqj', [64, 1024], F32)
        nrm = [sb('nrm%d' % i, [64, 1024], F32) for i in range(2)]
        kb.dma('sp', lambda e: e.dma_start(out=rm[:], in_=t.rowmask.ap()), w=['rm'])
        kb.dma('sp', lambda e: e.dma_start(out=gna[:], in_=bass.AP(t.na_norm_g, 0, [[0, 64], [1, 1024]])), w=['gna'])
        for i in range(2):
            kb.op('dve', lambda e: e.memset(VH[i][:], 1.0), w=[('VH', i)])
            kb.op('dve', lambda e: e.memset(MB[i][:], 0.0), w=[('MB', i)])
        vv = t.V_scr.ap().rearrange("(r kc) f -> kc r f", kc=64)
        mbv = t.MBT_scr.ap().rearrange("(h r) (kc q) -> h kc r q", r=15, q=64)
        rows = att_rows()
        items = [(h, r) for h in range(16) for r in rows]

        def emit_loads(h):
            hb = h % 2
            kb.dma('sp', lambda e: e.dma_start(out=KT[hb][:], in_=t.KT_scr.ap()[h]), r=['KT_scr'], w=[('KT', hb)])
            kb.dma('sp', lambda e: e.dma_start(out=QT[hb][:], in_=t.QT_scr.ap()[h]), r=['QT_scr'], w=[('QT', hb)])
            kb.dma('sp', lambda e: e.dma_start(out=VH[hb][0:64, :, 0:64], in_=vv[:, :, h * 64:(h + 1) * 64]), r=['V_scr'], w=[('VH', hb)])
            kb.dma('sp', lambda e: e.dma_start(out=VH[hb][64:128, 0:39, 0:64], in_=vv[:, 1:40, h * 64:(h + 1) * 64]), r=['V_scr', ('VH', hb)], w=[('VH', hb)])
            kb.dma('sp', lambda e: e.dma_start(out=MB[hb][0:64, :, :], in_=mbv[h]), r=['MBT_scr'], w=[('MB', hb)])
            kb.dma('sp', lambda e: e.dma_start(out=MB[hb][64:128, 0:14, :], in_=mbv[h][:, 1:15, :]), r=['MBT_scr', ('MB', hb)], w=[('MB', hb)])

        def emit_st(n):
            h, (i, e0, ns, dr0, ci) = items[n]
            hb = h % 2
            nb = n % 3
            if i == 0:
                emit_loads(h)
            psS = t.ps[nb]
            q_ap = QT[hb][:, i * 64:(i + 1) * 64]
            rk = [('KT', hb), ('QT', hb)]
            for c in range(ns // 2):
                kb.op('pe', lambda e: e.matmul(psS[:, c * 64:(c + 1) * 64], lhsT=KT[hb][:, (e0 + 2 * c) * 64:(e0 + 2 * c + 2) * 64], rhs=q_ap,
                                               start=True, stop=True), r=rk, w=[('ps', nb)])
            for c in range(2):
                kb.op('pe', lambda e: e.matmul(psS[:, 384 + c * 64:384 + (c + 1) * 64], lhsT=KT[hb][:, (36 + 2 * c) * 64:(38 + 2 * c) * 64],
                                               rhs=q_ap, start=True, stop=True), r=rk, w=[('ps', nb)])

        def emit_rest(n):
            h, (i, e0, ns, dr0, ci) = items[n]
            hb = h % 2
            nb = n % 3
            so = n % 2
            ok_ = ('psO', so)
            psS = t.ps[nb]
            psO = t.ps[6 + so][:, 0:128]
            ncnk = ns // 2
            w_ = ncnk * 64
            mb_ap = MB[hb][:, dr0:dr0 + ns, :].rearrange("p (c two) q -> p c two q", two=2)[:, :, 0, :]
            kb.op('dve', lambda e: e.scalar_tensor_tensor(out=tmpA[nb][:, 0:w_].rearrange("p (c q) -> p c q", q=64),
                                                          in0=psS[:, 0:w_].rearrange("p (c q) -> p c q", q=64), scalar=scale, in1=mb_ap,
                                                          op0=ALU.mult, op1=ALU.add), r=[('ps', nb), ('MB', hb)], w=[('tmpA', nb)])
            if ns == 12:
                kb.op('dve', lambda e: e.tensor_tensor(out=tmpA[nb][:, 0:w_], in0=tmpA[nb][:, 0:w_], in1=rm[:, ci, :, :].rearrange("p a b -> p (a b)"),
                                                       op=ALU.add), r=['rm'], w=[('tmpA', nb)])
            kb.op('act', lambda e: e.activation(out=PT[nb][:, 0:w_], in_=tmpA[nb][:, 0:w_], func=AF.Exp), r=[('tmpA', nb)], w=[('PT', nb)])
            kb.op('act', lambda e: e.activation(out=PT[nb][:, 384:512], in_=psS[:, 384:512], func=AF.Exp, scale=scale),
                  r=[('ps', nb)], w=[('PT', nb)])
            tot = ncnk + 2
            for c in range(tot):
                if c < ncnk:
                    lt = PT[nb][:, c * 64:(c + 1) * 64]
                    vr = e0 + 2 * c
                else:
                    lt = PT[nb][:, 384 + (c - ncnk) * 64:384 + (c - ncnk + 1) * 64]
                    vr = 36 + 2 * (c - ncnk)
                kb.op('pe', lambda e: e.matmul(psO[0:64, 0:65], lhsT=lt, rhs=VH[hb][:, vr, :], start=(c == 0), stop=(c == tot - 1)),
                      r=[('PT', nb), ('VH', hb)], w=[ok_])

        def emit_tail(n):
            h, (i, e0, ns, dr0, ci) = items[n]
            nb = n % 3
            so = n % 2
            ok_ = ('psO', so)
            psO = t.ps[6 + so][:, 0:128]
            kb.op('dve', lambda e: e.reciprocal(out=rz[nb][:], in_=psO[0:64, 64:65]), r=[ok_], w=[('rz', nb)])
            kb.op('act', lambda e: e.activation(out=na_all[:, i, h * 64:(h + 1) * 64], in_=psO[0:64, 0:64], func=AF.Identity,
                                                scale=rz[nb][:, 0:1]), r=[ok_, ('rz', nb)], w=[('na', i)])

        emit_st(0)
        emit_st(1)
        for n in range(len(items)):
            if n + 2 < len(items):
                emit_st(n + 2)
            emit_rest(n)
            if n >= 1:
                emit_tail(n - 1)
        emit_tail(len(items) - 1)
        if t.debug:
            kb.dma('sp', lambda e: e.dma_start(out=t.na_dbg.ap().rearrange("(i q) f -> q i f", q=64), in_=na_all[:]),
                   r=keys('na', 16), w=['na_dbg'])
        for i in range(16):
            kb.op('act', lambda e: e.activation(out=sqj[:], in_=na_all[:, i, :], func=AF.Square, accum_out=ssq[:, i:i + 1]),
                  r=[('na', i)], w=['sqj', ('ssq', i)])
        kb.op('dve', lambda e: e.tensor_scalar(out=ssq[:], in0=ssq[:], scalar1=1.0 / 1024, scalar2=LN_EPS, op0=ALU.mult, op1=ALU.add),
              r=keys('ssq', 16), w=['ssq2'])
        kb.op('act', lambda e: e.activation(out=ssq[:], in_=ssq[:], func=AF.Sqrt), r=['ssq2'], w=['ssq2'])
        kb.op('dve', lambda e: e.reciprocal(out=ssq[:], in_=ssq[:]), r=['ssq2'], w=['ssq2'])
        for i in range(16):
            nb = i % 2
            kb.op('dve', lambda e: e.scalar_tensor_tensor(out=nrm[nb][:], in0=na_all[:, i, :], scalar=ssq[:, i:i + 1], in1=gna[:],
                                                          op0=ALU.mult, op1=ALU.mult), r=[('na', i), 'ssq2', 'gna'], w=[('nrm', nb)])
            for g in range(2):
                ps = t.ps[g]
                for fc in range(4):
                    kb.op('pe', lambda e: e.transpose(ps[:, fc * 64:(fc + 1) * 64], nrm[nb][:, (g * 4 + fc) * 128:(g * 4 + fc + 1) * 128],
                                                      t.ident[0:64, 0:64]), r=[('nrm', nb), 'ident'], w=[('ps', g)])
                kb.op('act', lambda e: e.activation(out=mergedT[:, 8 + g * 4:8 + (g + 1) * 4, i * 64:(i + 1) * 64],
                                                    in_=ps[:, 0:256].rearrange("p (a b) -> p a b", b=64), func=AF.Identity),
                      r=[('ps', g)], w=['mergedT'])


def stage_filters(kb, t):
    nc = kb.nc
    PI = float(np.pi)
    with ExitStack() as es:
        sb = lambda name, shape, dt: es.enter_context(nc.sbuf_tensor(name, shape, dt))
        zT = sb('zT_sb', [33, 2048], F32)
        w1 = sb('fw1', [33, 64], F32)
        w23 = sb('fw23', [64, 2, 64], F32)
        w4 = sb('fw4', [64, 4096], F32)
        fr = sb('ffr', [64, 3], F32)
        fb = sb('ffb', [64, 3], F32)
        hA = sb('hA', [64, 2048], F32)
        hB = sb('hB', [64, 2048], F32)
        wrp = sb('wrp', [64, 2048], F32)
        ones_c = sb('ones_c', [128, 1], F32)
        ones_r = sb('ones_r', [1, 128], F32)
        nones_b = sb('nones_b', [1, 128], BF16)
        ee = sb('ee2', [1, 2], F32)
        hs = sb('hs', [128, 16, 1024], BF16)
        hd = sb('hd', [128, 16, 1024], BF16)
        Et = [sb('Et%d' % i, [128, 512], F32) for i in range(2)]
        pf = [sb('pf%d' % i, [128, 512], F32) for i in range(2)]
        pb = [sb('pb%d' % i, [128, 512], F32) for i in range(2)]
        af = [sb('af%d' % i, [128, 512], F32) for i in range(2)]
        ab = [sb('ab%d' % i, [128, 512], F32) for i in range(2)]
        bw0 = sb('bw0', [1, 2, 512], F32)
        nb0 = sb('nb0', [1, 2, 512], BF16)
        rcol = sb('rcol', [1, 512], F32)
        rinvb = sb('rinvb', [128, 2, 512], F32)
        wf = [sb('wfA%d' % i, [128, 16, 128], BF16) for i in range(4)]
        kout = [sb('kout%d' % i, [128, 512], F32) for i in range(4)]
        kb.dma('sp', lambda e: e.dma_start(out=zT[:], in_=t.zT_d.ap()), w=['zT'])
        kb.dma('sp', lambda e: e.dma_start(out=w1[:], in_=t.filt_w1.ap()), w=['fw1'])
        kb.dma('sp', lambda e: e.dma_start(out=w23[:, 0, :], in_=t.filt_w2.ap()), w=['fw23'])
        kb.dma('sp', lambda e: e.dma_start(out=w23[:, 1, :], in_=t.filt_w3.ap()), w=['fw23'], r=['fw23'])
        kb.dma('sp', lambda e: e.dma_start(out=w4[:], in_=t.filt_w4.ap()), w=['fw4'])
        kb.dma('sp', lambda e: e.dma_start(out=fr[:], in_=t.filt_freq.ap().rearrange("k j -> j k")), w=['ffr'])
        kb.dma('sp', lambda e: e.dma_start(out=fb[:], in_=t.filt_b.ap().rearrange("k j -> j k")), w=['ffb'])
        kb.dma('sp', lambda e: e.dma_start(out=ee[:], in_=t.e01.ap()), w=['ee2'])
        kb.op('dve', lambda e: e.tensor_tensor(out=fb[:], in0=fb[:], in1=fr[:], op=ALU.mult), r=['ffr', 'ffb'], w=['ffb'])
        kb.op('dve', lambda e: e.memset(ones_c[:], 1.0), w=['ones_c'])
        kb.op('dve', lambda e: e.memset(ones_r[:], 1.0), w=['ones_r'])
        kb.op('dve', lambda e: e.memset(nones_b[:], -1.0), w=['nones_b'])
        src, srck = zT, 'zT'
        for layer in range(3):
            dst, dstk = (hA, 'hA') if layer % 2 == 0 else (hB, 'hB')
            for blk in range(4):
                ps = t.ps[blk]
                lhs = w1[:, :] if layer == 0 else w23[:, layer - 1, :]
                kb.op('pe', lambda e: e.matmul(ps[0:64, :], lhsT=lhs, rhs=src[:, blk * 512:(blk + 1) * 512], start=True, stop=True),
                      r=['fw1', 'fw23', srck], w=[('ps', blk)])
                kb.op('act', lambda e: e.activation(out=dst[:, blk * 512:(blk + 1) * 512], in_=ps[0:64, :], func=AF.Identity,
                                                    scale=fr[:, layer:layer + 1], bias=fb[:, layer:layer + 1]),
                      r=[('ps', blk), 'ffr', 'ffb'], w=[dstk])
            for _ in range(2):
                for (cmp, thr, add) in ((ALU.is_gt, PI, -2 * PI), (ALU.is_lt, -PI, 2 * PI)):
                    kb.op('dve', lambda e: e.tensor_scalar(out=wrp[:], in0=dst[:], scalar1=thr, scalar2=add, op0=cmp, op1=ALU.mult),
                          r=[dstk], w=['wrp'])
                    kb.op('dve', lambda e: e.tensor_tensor(out=dst[:], in0=dst[:], in1=wrp[:], op=ALU.add), r=[dstk, 'wrp'], w=[dstk])
            kb.op('act', lambda e: e.activation(out=dst[:], in_=dst[:], func=AF.Sin), r=[dstk], w=[dstk])
            src, srck = dst, dstk
        h3, h3k = src, srck
        Ev = t.E_d.ap()
        n = 0
        wi = 0
        for o in range(2):
            for hf in range(2):
                hcs = slice(hf * 512, (hf + 1) * 512)
                cs = t.ps[7]
                for tt in range(16):
                    nb = n % 2
                    n += 1
                    psf, psb = t.ps[nb], t.ps[2 + nb]
                    for (ps_, dr, pk) in ((psf, 0, nb), (psb, 1, 2 + nb)):
                        c0 = o * 2048 + dr * 1024 + hf * 512
                        kb.op('pe', lambda e: e.matmul(ps_[:], lhsT=h3[:, tt * 128:(tt + 1) * 128], rhs=w4[:, c0:c0 + 512], start=True, stop=True),
                              r=[h3k, 'fw4'], w=[('ps', pk)])
                    kb.dma('sp', lambda e: e.dma_start(out=Et[nb][:], in_=Ev[tt * 128:(tt + 1) * 128, hcs]), w=[('Et', nb)])
                    kb.op('dve', lambda e: e.tensor_tensor(out=pf[nb][:], in0=psf[:], in1=Et[nb][:], op=ALU.mult), r=[('ps', nb), ('Et', nb)], w=[('pf', nb)])
                    kb.op('dve', lambda e: e.tensor_tensor(out=pb[nb][:], in0=psb[:], in1=Et[nb][:], op=ALU.mult), r=[('ps', 2 + nb), ('Et', nb)], w=[('pb', nb)])
                    kb.op('pool', lambda e: e.tensor_tensor(out=hs[:, tt, hcs], in0=pf[nb][:], in1=pb[nb][:], op=ALU.add), r=[('pf', nb), ('pb', nb)], w=[('hs', hf)])
                    kb.op('pool', lambda e: e.tensor_tensor(out=hd[:, tt, hcs], in0=pf[nb][:], in1=pb[nb][:], op=ALU.subtract), r=[('pf', nb), ('pb', nb)], w=[('hd', hf)])
                    kb.op('act', lambda e: e.activation(out=af[nb][:], in_=pf[nb][:], func=AF.Abs), r=[('pf', nb)], w=[('af', nb)])
                    kb.op('act', lambda e: e.activation(out=ab[nb][:], in_=pb[nb][:], func=AF.Abs), r=[('pb', nb)], w=[('ab', nb)])
                    kb.op('pe', lambda e: e.matmul(cs[0:1, :], lhsT=ones_c[:, :], rhs=af[nb][:], start=(tt == 0), stop=False),
                          r=[('af', nb), 'ones_c'], w=[('ps', 7)])
                    kb.op('pe', lambda e: e.matmul(cs[0:1, :], lhsT=ones_c[:, :], rhs=ab[nb][:], start=False, stop=(tt == 15)),
                          r=[('ab', nb), 'ones_c'], w=[('ps', 7)])
                    if tt in (0, 8):
                        kb.op('dve', lambda e: e.tensor_copy(out=bw0[:, tt // 8, :], in_=pb[nb][0:1, :]), r=[('pb', nb)], w=['bw0'])
                kb.op('dve', lambda e: e.reciprocal(out=rcol[:], in_=cs[0:1, :]), r=[('ps', 7)], w=['rcol'])
                kb.op('dve', lambda e: e.tensor_scalar(out=bw0[:, 0, :], in0=bw0[:, 0, :], scalar1=ee[:, 0:1], scalar2=None, op0=ALU.mult),
                      r=['bw0', 'ee2'], w=['bw0'])
                kb.op('dve', lambda e: e.scalar_tensor_tensor(out=nb0[:, hf, :], in0=bw0[:, 1, :], scalar=ee[:, 1:2], in1=bw0[:, 0, :],
                                                              op0=ALU.mult, op1=ALU.add), r=['bw0', 'ee2'], w=[('nb0', hf)])
                kb.op('pe', lambda e: e.matmul(t.ps[6][:], lhsT=ones_r[:, :], rhs=rcol[:], start=True, stop=True), r=['ones_r', 'rcol'], w=[('ps', 6)])
                kb.op('act', lambda e: e.activation(out=rinvb[:, hf, :], in_=t.ps[6][:], func=AF.Identity), r=[('ps', 6)], w=[('rinvb', hf)])
            kv = t.Kf_scr.ap()[o]
            kn = 0
            for fc in list(range(32)) + [32]:
                real_fc = 16 if fc == 32 else fc
                use_hs = (fc < 16) or (fc == 32)
                wb_ = wf[wi % 4]
                wk = ('wfA', wi % 4)
                wi += 1
                kb.dma('sp' if wi % 2 == 0 else 'pool', lambda e: e.dma_start(out=wb_[:], in_=t.Wf_d.ap()[real_fc]), w=[wk])
                for hf in range(2):
                    hcs = slice(hf * 512, (hf + 1) * 512)
                    p = 4 + (kn % 2)
                    ps = t.ps[p]
                    srcT, srcK = (hs, ('hs', hf)) if use_hs else (hd, ('hd', hf))
                    for k in range(16):
                        kb.op('pe', lambda e: e.matmul(ps[:], lhsT=wb_[:, k, :], rhs=srcT[:, k, hcs], start=(k == 0), stop=(k == 15 and not use_hs)),
                              r=[wk, srcK], w=[('ps', p)])
                    if use_hs:
                        kb.op('pe', lambda e: e.matmul(ps[:], lhsT=nones_b[:, :], rhs=nb0[:, hf, :], start=False, stop=True),
                              r=['nones_b', ('nb0', hf)], w=[('ps', p)])
                    ko, kk = kout[kn % 4], ('kout', kn % 4)
                    kn += 1
                    if fc == 32:
                        kb.op('dve', lambda e: e.tensor_tensor(out=ko[0:1, :], in0=ps[0:1, :], in1=rinvb[0:1, hf, :], op=ALU.mult),
                              r=[('ps', p), ('rinvb', hf)], w=[kk])
                        kb.dma('sp', lambda e: e.dma_start(out=kv[2048:2049, hcs], in_=ko[0:1, :]), r=[kk, ('Kf_scr', 16, hf)], w=[('Kf_scr', 16, hf)])
                    else:
                        kb.op('dve', lambda e: e.tensor_tensor(out=ko[:], in0=ps[:], in1=rinvb[:, hf, :], op=ALU.mult), r=[('ps', p), ('rinvb', hf)], w=[kk])
                        kb.dma('sp', lambda e: e.dma_start(out=kv[fc * 128:(fc + 1) * 128, hcs], in_=ko[:]), r=[kk], w=[('Kf_scr', fc, hf)])
        kb.lastw['Kf_scr'] = None


def stage_conv(kb, t):
    nc = kb.nc
    with ExitStack() as es:
        sb = lambda name, shape, dt: es.enter_context(nc.sbuf_tensor(name, shape, dt))
        vtok = sb('vtok', [128, 16, 1024], BF16)
        Yall = sb('Yall', [128, 32, 1024], BF16)
        skipb = sb('skipb', [128, 2, 1024], F32)
        wf = [sb('wfB%d' % i, [128, 16, 128], BF16) for i in range(4)]
        wi_ = [sb('wiB%d' % i, [128, 32, 128], BF16) for i in range(2)]
        kre = [sb('kre%d' % i, [128, 512], F32) for i in range(2)]
        kim = [sb('kim%d' % i, [128, 512], F32) for i in range(2)]
        vre = [sb('vre%d' % i, [128, 512], F32) for i in range(2)]
        vim = [sb('vim%d' % i, [128, 512], F32) for i in range(2)]
        t1 = [sb('t1_0', [128, 512], F32)] * 2
        t2 = [sb('t2_0', [128, 512], F32)] * 2
        xg = [sb('xgc%d' % i, [128, 512], F32) for i in range(2)]
        aa = [sb('aa%d' % i, [128, 512], F32) for i in range(2)]
        a0 = sb('a0', [1, 512], F32)
        bb = [sb('bbq%d' % i, [128, 512], F32) for i in range(2)]
        hyt = [sb('hyt%d' % i, [128, 512], F32) for i in range(2)]
        hzv = t.hz_tok.ap().rearrange("(tt p) c -> p tt c", p=128)
        for tt in range(16):
            kb.dma('pool', lambda e: e.dma_start(out=vtok[:, tt, :], in_=hzv[:, tt, 0:1024]), r=['hz_tok'], w=[('vtok', tt)])
        kb.dma('sp', lambda e: e.dma_start(out=skipb[:], in_=bass.AP(t.hy_skip, 0, [[0, 128], [1, 2048]])), w=['skipb'])
        wn = 0
        kn = 0
        xn = 0
        for conv in range(2):
            kv = t.Kf_scr.ap()[conv]
            for fc in range(16):
                wbs = []
                for part in range(2):
                    wb_ = wf[wn % 4]
                    wk = ('wfB', wn % 4)
                    kb.dma('sp' if part == 0 else 'pool', lambda e: e.dma_start(out=wb_[:], in_=t.Wf_d.ap()[fc + 16 * part]), w=[wk])
                    wn += 1
                    wbs.append((wb_, wk))
                for cb in range(2):
                    cs = slice(cb * 512, (cb + 1) * 512)
                    q = kn % 2
                    kn += 1
                    pss = []
                    for part in range(2):
                        wb_, wk = wbs[part]
                        p = part * 2 + q
                        ps = t.ps[p]
                        for k in range(16):
                            kb.op('pe', lambda e: e.matmul(ps[:], lhsT=wb_[:, k, :], rhs=vtok[:, k, cs],
                                                           start=(k == 0), stop=(k == 15)), r=[wk, ('vtok', k)], w=[('ps', p)])
                        pss.append((ps, p))
                    kb.dma('sp', lambda e: e.dma_start(out=kre[q][:], in_=kv[fc * 128:(fc + 1) * 128, cs]), r=['Kf_scr'], w=[('kre', q)])
                    kb.dma('sp', lambda e: e.dma_start(out=kim[q][:], in_=kv[2048 + fc * 128:2048 + (fc + 1) * 128, cs]), r=['Kf_scr'], w=[('kim', q)])
                    kb.op('act', lambda e: e.activation(out=vre[q][:], in_=pss[0][0][:], func=AF.Identity), r=[('ps', pss[0][1])], w=[('vre', q)])
                    kb.op('act', lambda e: e.activation(out=vim[q][:], in_=pss[1][0][:], func=AF.Identity), r=[('ps', pss[1][1])], w=[('vim', q)])
                    kb.op('dve', lambda e: e.tensor_tensor(out=t1[q][:], in0=vre[q][:], in1=kre[q][:], op=ALU.mult), r=[('vre', q), ('kre', q)], w=['t1'])
                    kb.op('pool', lambda e: e.tensor_tensor(out=t2[q][:], in0=vim[q][:], in1=kim[q][:], op=ALU.mult), r=[('vim', q), ('kim', q)], w=['t2'])
                    kb.op('dve', lambda e: e.tensor_tensor(out=Yall[:, fc, cs], in0=t1[q][:], in1=t2[q][:], op=ALU.subtract),
                          r=['t1', 't2'], w=[('Y', cb)])
                    if fc == 0:
                        kb.op('dve', lambda e: e.tensor_copy(out=Yall[0:1, 0, cs], in_=t1[q][0:1, :]), r=['t1'], w=[('Y', cb)])
                        kb.op('dve', lambda e: e.tensor_copy(out=a0[0:1, :], in_=t2[q][0:1, :]), r=['t2'], w=['a0'])
                    kb.op('dve', lambda e: e.tensor_tensor(out=t1[q][:], in0=vre[q][:], in1=kim[q][:], op=ALU.mult), r=[('vre', q), ('kim', q)], w=['t1'])
                    kb.op('pool', lambda e: e.tensor_tensor(out=t2[q][:], in0=vim[q][:], in1=kre[q][:], op=ALU.mult), r=[('vim', q), ('kre', q)], w=['t2'])
                    kb.op('dve', lambda e: e.tensor_tensor(out=Yall[:, 16 + fc, cs], in0=t1[q][:], in1=t2[q][:], op=ALU.add),
                          r=['t1', 't2'], w=[('Y', cb)])
                    if fc == 0:
                        kb.op('dve', lambda e: e.tensor_copy(out=Yall[0:1, 16, cs], in_=a0[0:1, :]), r=['a0'], w=[('Y', cb)])
            ntt = 16 if conv == 0 else 8
            for tt in range(ntt):
                wb_ = wi_[tt % 2]
                wk = ('wiB', tt % 2)
                kb.dma('sp' if tt % 2 == 0 else 'pool', lambda e: e.dma_start(out=wb_[:], in_=t.Wi_d.ap()[tt]), w=[wk])
                for cb in range(2):
                    cs = slice(cb * 512, (cb + 1) * 512)
                    q = xn % 2
                    xn += 1
                    xb, xk = xg[q], ('xgc', q)
                    hb_, hk = hyt[q], ('hyt', q)
                    c0 = (1 + conv) * 1024 + cb * 512
                    kb.dma('sp', lambda e: e.dma_start(out=xb[:], in_=t.hz_tok.ap()[tt * 128:(tt + 1) * 128, c0:c0 + 512]),
                           r=['hz_tok'], w=[xk])
                    p = 4 + q
                    ps = t.ps[p]
                    for rc in range(32):
                        kb.op('pe', lambda e: e.matmul(ps[:], lhsT=wb_[:, rc, :], rhs=Yall[:, rc, cs],
                                                       start=(rc == 0), stop=(rc == 31)), r=[wk, ('Y', cb)], w=[('ps', p)])
                    kb.op('pool', lambda e: e.tensor_tensor(out=aa[q][:], in0=vtok[:, tt, cs], in1=skipb[:, conv, cs],
                                                            op=ALU.mult), r=[('vtok', tt), 'skipb'], w=[('aa', q)])
                    kb.op('dve', lambda e: e.tensor_tensor(out=bb[q][:], in0=ps[:], in1=aa[q][:], op=ALU.add), r=[('ps', p), ('aa', q)], w=[('bbq', q)])
                    if conv == 0:
                        kb.op('dve', lambda e: e.tensor_tensor(out=vtok[:, tt, cs], in0=bb[q][:], in1=xb[:],
                                                               op=ALU.mult), r=[('bbq', q), xk], w=[('vtok', tt)])
                    else:
                        kb.op('dve', lambda e: e.tensor_tensor(out=hb_[:], in0=bb[q][:], in1=xb[:], op=ALU.mult), r=[('bbq', q), xk], w=[hk])
                        kb.dma('sp', lambda e: e.dma_start(out=t.hy_scr.ap()[tt * 128:(tt + 1) * 128, cs], in_=hb_[:]), r=[hk], w=[('hy_scr', tt, cb)])


def bcast_row(dt_handle, off, n, parts=128):
    return bass.AP(dt_handle, off, [[0, parts], [1, n]])


def layer_norm_tile(kb, nc, r, rk, st, mv, rs, g, gk, b, bk, tag):
    for k in range(4):
        kb.op('dve', lambda e: e.bn_stats(out=st[:, k, :], in_=r[:, k * 512:(k + 1) * 512]), r=[rk], w=[tag + 'st'])
    kb.op('dve', lambda e: e.bn_aggr(out=mv[:], in_=st[:].rearrange("p a b -> p (a b)")), r=[tag + 'st'], w=[tag + 'mv'])
    kb.op('dve', lambda e: e.tensor_scalar(out=rs[:], in0=mv[:, 1:2], scalar1=LN_EPS, scalar2=None, op0=ALU.add), r=[tag + 'mv'], w=[tag + 'rs'])
    kb.op('act', lambda e: e.activation(out=rs[:], in_=rs[:], func=AF.Sqrt), r=[tag + 'rs'], w=[tag + 'rs'])
    kb.op('dve', lambda e: e.reciprocal(out=rs[:], in_=rs[:]), r=[tag + 'rs'], w=[tag + 'rs'])
    kb.op('dve', lambda e: e.tensor_scalar(out=r[:], in0=r[:], scalar1=mv[:, 0:1], scalar2=rs[:, 0:1], op0=ALU.subtract, op1=ALU.mult),
          r=[rk, tag + 'mv', tag + 'rs'], w=[rk])
    kb.op('pool', lambda e: e.tensor_tensor(out=r[:], in0=r[:], in1=g[:], op=ALU.mult), r=[rk, gk], w=[rk])
    kb.op('dve', lambda e: e.tensor_tensor(out=r[:], in0=r[:], in1=b[:], op=ALU.add), r=[rk, bk], w=[rk])


def stage_merge_a(kb, t, mergedT):
    nc = kb.nc
    with ExitStack() as es:
        sb = lambda name, shape, dt: es.enter_context(nc.sbuf_tensor(name, shape, dt))
        ghy = sb('ghy', [128, 1024], F32)
        hyt = [sb('hym%d' % i, [128, 1024], F32) for i in range(2)]
        sqj = sb('sqj3', [128, 1024], F32)
        nrm = sb('nrm3', [128, 1024], F32)
        ssq = sb('ssq3', [128, 8], F32)
        kb.dma('sp', lambda e: e.dma_start(out=ghy[:], in_=bcast_row(t.hy_norm_g, 0, 1024)), w=['ghy'])
        for tt in range(8):
            hb_, hk = hyt[tt % 2], ('hym', tt % 2)
            kb.dma('sp', lambda e: e.dma_start(out=hb_[:], in_=t.hy_scr.ap()[tt * 128:(tt + 1) * 128, :]), r=['hy_scr'], w=[hk])
            kb.op('act', lambda e: e.activation(out=sqj[:], in_=hb_[:], func=AF.Square, accum_out=ssq[:, tt:tt + 1]), r=[hk], w=['sqj3', ('ssq3', tt)])
            kb.op('dve', lambda e: e.tensor_scalar(out=ssq[:, tt:tt + 1], in0=ssq[:, tt:tt + 1], scalar1=1.0 / 1024, scalar2=LN_EPS, op0=ALU.mult, op1=ALU.add),
                  r=[('ssq3', tt)], w=[('ssq3', tt)])
            kb.op('act', lambda e: e.activation(out=ssq[:, tt:tt + 1], in_=ssq[:, tt:tt + 1], func=AF.Sqrt), r=[('ssq3', tt)], w=[('ssq3', tt)])
            kb.op('dve', lambda e: e.reciprocal(out=ssq[:, tt:tt + 1], in_=ssq[:, tt:tt + 1]), r=[('ssq3', tt)], w=[('ssq3', tt)])
            kb.op('dve', lambda e: e.scalar_tensor_tensor(out=nrm[:], in0=hb_[:], scalar=ssq[:, tt:tt + 1], in1=ghy[:], op0=ALU.mult, op1=ALU.mult),
                  r=[hk, ('ssq3', tt), 'ghy'], w=['nrm3'])
            for g in range(2):
                ps = t.ps[6 + g]
                for fc in range(4):
                    kb.op('pe', lambda e: e.transpose(ps[:, fc * 128:(fc + 1) * 128], nrm[:, (g * 4 + fc) * 128:(g * 4 + fc + 1) * 128], t.ident[:]),
                          r=['nrm3', 'ident'], w=[('ps', 6 + g)])
                kb.op('act', lambda e: e.activation(out=mergedT[:, g * 4:(g + 1) * 4, tt * 128:(tt + 1) * 128],
                                                    in_=ps[:].rearrange("p (a b) -> p a b", b=128), func=AF.Identity),
                      r=[('ps', 6 + g)], w=['mergedT'])


def stage_merge_b(kb, t, mergedT):
    nc = kb.nc
    with ExitStack() as es:
        sb = lambda name, shape, dt: es.enter_context(nc.sbuf_tensor(name, shape, dt))
        wo = sb('wo', [128, 16, 2048], BF16)
        g1b = sb('g1b', [128, 2048], F32)
        l1g = sb('l1g', [128, 2048], F32)
        l1b = sb('l1b', [128, 2048], F32)
        s2p = sb('s2p', [128, 2048], F32)
        h2b = sb('h2b', [128, 2048], F32)
        xt = [sb('xtm%d' % i, [128, 2048], F32) for i in range(2)]
        rr = [sb('rrm%d' % i, [128, 2048], F32) for i in range(2)]
        st = sb('st1', [128, 4, 6], F32)
        mv = sb('mv1', [128, 2], F32)
        rs = sb('rs1', [128, 1], F32)
        wv = t.w_out.ap().rearrange("(k p) n -> p k n", p=128)
        for cb in range(4):
            kb.dma('pool', lambda e: e.dma_start(out=wo[:, :, cb * 512:(cb + 1) * 512], in_=wv[:, :, cb * 512:(cb + 1) * 512]), w=[('wo', cb)])
        kb.dma('sp', lambda e: e.dma_start(out=g1b[:], in_=bcast_row(t.m_scr, 2 * D, D)), r=['m_scr'], w=['g1b'])
        kb.dma('sp', lambda e: e.dma_start(out=s2p[:], in_=bcast_row(t.m_scr, 4 * D, D)), r=['m_scr'], w=['s2p'])
        kb.dma('sp', lambda e: e.dma_start(out=h2b[:], in_=bcast_row(t.m_scr, 3 * D, D)), r=['m_scr'], w=['h2b'])
        kb.dma('sp', lambda e: e.dma_start(out=l1g[:], in_=bcast_row(t.ln1_g, 0, D)), w=['l1g'])
        kb.dma('sp', lambda e: e.dma_start(out=l1b[:], in_=bcast_row(t.ln1_b, 0, D)), w=['l1b'])
        kb.op('dve', lambda e: e.tensor_scalar(out=s2p[:], in0=s2p[:], scalar1=1.0, scalar2=None, op0=ALU.add), r=['s2p'], w=['s2p'])
        for tt in range(8):
            xb, xk = xt[tt % 2], ('xtm', tt % 2)
            r, rk = rr[tt % 2], ('rrm', tt % 2)
            kb.dma('sp', lambda e: e.dma_start(out=xb[:], in_=t.xr.ap()[tt * 128:(tt + 1) * 128, :]), w=[xk])
            for cb in range(4):
                ps = t.ps[cb]
                for k in range(16):
                    kb.op('pe', lambda e: e.matmul(ps[:], lhsT=mergedT[:, k, tt * 128:(tt + 1) * 128], rhs=wo[:, k, cb * 512:(cb + 1) * 512],
                                                   start=(k == 0), stop=(k == 15)), r=['mergedT', ('wo', cb)], w=[('ps', cb)])
                kb.op('dve', lambda e: e.tensor_tensor(out=r[:, cb * 512:(cb + 1) * 512], in0=ps[:], in1=g1b[:, cb * 512:(cb + 1) * 512], op=ALU.mult),
                      r=[('ps', cb), 'g1b'], w=[rk])
            kb.op('dve', lambda e: e.scalar_tensor_tensor(out=r[:], in0=xb[:], scalar=ALPHA, in1=r[:], op0=ALU.mult, op1=ALU.add), r=[xk, rk], w=[rk])
            layer_norm_tile(kb, nc, r, rk, st, mv, rs, l1g, 'l1g', l1b, 'l1b', 'ln1')
            kb.dma('sp', lambda e: e.dma_start(out=t.x1_scr.ap()[tt * 128:(tt + 1) * 128, :], in_=r[:]), r=[rk], w=['x1_scr'])
            kb.op('pool', lambda e: e.tensor_tensor(out=r[:], in0=r[:], in1=s2p[:], op=ALU.mult), r=[rk, 's2p'], w=[rk])
            kb.op('dve', lambda e: e.tensor_tensor(out=r[:], in0=r[:], in1=h2b[:], op=ALU.add), r=[rk, 'h2b'], w=[rk])
            kb.dma('sp', lambda e: e.dma_start(out=t.u2_scr.ap()[tt * 128:(tt + 1) * 128, :], in_=r[:]), r=[rk], w=['u2_scr'])


def stage_peer_q(kb, t, u2T):
    nc = kb.nc
    with ExitStack() as es:
        sb = lambda name, shape, dt: es.enter_context(nc.sbuf_tensor(name, shape, dt))
        ut = [sb('utq%d' % i, [128, 2048], F32) for i in range(2)]
        wq = [sb('wq%d' % i, [128, 16, 512], BF16) for i in range(2)]
        qs = [sb('qs%d' % i, [128, LH], F32) for i in range(2)]
        pi = 0
        for tt in range(8):
            ub, uk = ut[tt % 2], ('utq', tt % 2)
            kb.dma('sp', lambda e: e.dma_start(out=ub[:], in_=t.u2_scr.ap()[tt * 128:(tt + 1) * 128, :]), r=['u2_scr'], w=[uk])
            for g in range(4):
                p = pi % 4
                pi += 1
                ps = t.ps[p]
                for i in range(4):
                    k = g * 4 + i
                    kb.op('pe', lambda e: e.transpose(ps[:, i * 128:(i + 1) * 128], ub[:, k * 128:(k + 1) * 128], t.ident[:]),
                          r=[uk, 'ident'], w=[('ps', p)])
                kb.op('act', lambda e: e.activation(out=u2T[:, g * 4:(g + 1) * 4, tt * 128:(tt + 1) * 128],
                                                    in_=ps[:].rearrange("p (a b) -> p a b", b=128), func=AF.Identity),
                      r=[('ps', p)], w=['u2T'])
        wv = t.peer_wq.ap().rearrange("(k p) n -> p k n", p=128)
        for blk in range(4):
            wb_, wk = wq[blk % 2], ('wq', blk % 2)
            kb.dma('pool', lambda e: e.dma_start(out=wb_[:], in_=wv[:, :, blk * 512:(blk + 1) * 512]), w=[wk])
            for cc in range(4):
                c = blk * 4 + cc
                qb, qk = qs[c % 2], ('qs', c % 2)
                for tb in range(2):
                    p = 4 + tb
                    ps = t.ps[p]
                    for k in range(16):
                        kb.op('pe', lambda e: e.matmul(ps[:], lhsT=wb_[:, k, cc * 128:(cc + 1) * 128], rhs=u2T[:, k, tb * 512:(tb + 1) * 512],
                                                       start=(k == 0), stop=(k == 15)), r=[wk, 'u2T'], w=[('ps', p)])
                    kb.op('act', lambda e: e.activation(out=qb[:, tb * 512:(tb + 1) * 512], in_=ps[:], func=AF.Identity), r=[('ps', p)], w=[qk])
                kb.dma('sp', lambda e: e.dma_start(out=t.qT_scr.ap()[c], in_=qb[:]), r=[qk], w=['qT_scr'])


def stage_peer(kb, t, u2T):
    nc = kb.nc
    pst = lambda a: list(a.ap[0])
    with ExitStack() as es0:
        with ExitStack() as es:
            sb = lambda name, shape, dt: es.enter_context(nc.sbuf_tensor(name, shape, dt))
            keysT = sb('keysT', [128, 16, 128], F32)
            kld = sb('kld', [128, 16, 128], F32)
            iota16 = sb('iota16_sb', [128, 16], F32)
            iota128 = sb('iota128_sb', [128, 128], F32)
            qTt = sb('qTt', [128, 16, 128], F32)
            S = sb('S', [128, 16, 128], F32)
            S2 = sb('S2', [128, 16, 128], F32)
            V16 = sb('V16', [128, 16, 16], F32)
            I16 = sb('I16', [128, 16, 16], U32)
            I16f = sb('I16f', [128, 16, 16], F32)
            cand = sb('cand', [128, 8, 256], F32)
            cand2 = sb('cand2', [128, 8, 256], F32)
            TS = sb('TS', [128, 8, 16], F32)
            P16 = sb('P16', [128, 8, 16], U32)
            Pi = sb('Pi', [128, 8, 16], U32)
            Pj = sb('Pj', [128, 8, 16], U32)
            Pif = sb('Pif', [128, 8, 16], F32)
            Pjf = sb('Pjf', [128, 8, 16], F32)
            eq = sb('eq', [128, 8, 16, 16], F32)
            ia = sb('ia', [128, 8, 16], F32)
            ib = sb('ib', [128, 8, 16], F32)
            nmax = sb('nmax', [128, 8], F32)
            Gt = sb('Gt', [128, 8, 16], F32)
            Z = sb('Z', [128, 8], F32)
            tr3 = sb('tr3', [128, 3, 128], F32)
            At = [sb('At%d' % i, [128, 128], F32) for i in range(4)]
            Bt = [sb('Bt%d' % i, [128, 128], F32) for i in range(8)]
            nb4 = sb('nb4', [128, 128], F32)
            Gst = [sb('Gst%d' % i, [128, 128, 128], BF16) for i in range(2)]
            kb.dma('sp', lambda e: e.dma_start(out=iota16[:], in_=t.iota16_d.ap()), w=['iota16'])
            kb.dma('sp', lambda e: e.dma_start(out=iota128[:], in_=t.iota128_d.ap()), w=['iota128'])
            kb.dma('sp', lambda e: e.dma_start(out=kld[:], in_=t.peer_keys.ap().rearrange("c n d -> n c d")), w=['kld'])
            for g in range(4):
                ps = t.ps[g]
                for i in range(4):
                    c = g * 4 + i
                    kb.op('pe', lambda e: e.transpose(ps[:, i * 128:(i + 1) * 128], kld[:, c, :], t.ident[:]), r=['kld', 'ident'], w=[('ps', g)])
                kb.op('act', lambda e: e.activation(out=keysT[:, g * 4:(g + 1) * 4, :], in_=ps[:].rearrange("p (a b) -> p a b", b=128), func=AF.Identity),
                      r=[('ps', g)], w=['keysT'])
            gsv = t.G_scr.ap().rearrange("a b n -> b a n")
            gq = 0
            for tt in range(8):
                kb.dma('sp', lambda e: e.dma_start(out=qTt[:], in_=t.qT_scr.ap()[:, :, tt * 128:(tt + 1) * 128].rearrange("c d n -> d c n")),
                       r=['qT_scr'], w=['qTt'])
                for g in range(4):
                    ps = t.ps[4 + g]
                    for i in range(4):
                        c = g * 4 + i
                        kb.op('pe', lambda e: e.matmul(ps[:, i * 128:(i + 1) * 128], lhsT=qTt[:, c, :], rhs=keysT[:, c, :], start=True, stop=True),
                              r=['qTt', 'keysT'], w=[('ps', 4 + g)])
                    kb.op('act', lambda e: e.activation(out=S[:, g * 4:(g + 1) * 4, :], in_=ps[:].rearrange("p (a b) -> p a b", b=128), func=AF.Identity),
                          r=[('ps', 4 + g)], w=['S'])
                for c in range(16):
                    kb.op('dve', lambda e: e.max(out=V16[:, c, 0:8], in_=S[:, c, :]), r=['S'], w=['V16'])
                    kb.op('dve', lambda e: e.max_index(out=I16[:, c, 0:8], in_max=V16[:, c, 0:8], in_values=S[:, c, :]), r=['S', 'V16'], w=['I16'])
                    kb.op('dve', lambda e: e.match_replace(out=S2[:, c, :], in_to_replace=V16[:, c, 0:8], in_values=S[:, c, :], imm_value=-1e30),
                          r=['S', 'V16'], w=['S2'])
                    kb.op('dve', lambda e: e.max(out=V16[:, c, 8:16], in_=S2[:, c, :]), r=['S2'], w=['V16'])
                    kb.op('dve', lambda e: e.max_index(out=I16[:, c, 8:16], in_max=V16[:, c, 8:16], in_values=S2[:, c, :]), r=['S2', 'V16'], w=['I16'])
                kb.op('dve', lambda e: e.tensor_copy(out=I16f[:], in_=I16[:]), r=['I16'], w=['I16f'])
                va = bass.AP(V16[:].tensor, V16[:].offset, [pst(V16[:]), [32, 8], [1, 16], [0, 16]])
                vb = bass.AP(V16[:].tensor, V16[:].offset + 16, [pst(V16[:]), [32, 8], [0, 16], [1, 16]])
                kb.op('dve', lambda e: e.tensor_tensor(out=cand[:].rearrange("p h (i j) -> p h i j", j=16), in0=va, in1=vb, op=ALU.add),
                      r=['V16'], w=['cand'])
                for h in range(8):
                    kb.op('dve', lambda e: e.max(out=TS[:, h, 0:8], in_=cand[:, h, :]), r=['cand'], w=['TS'])
                    kb.op('dve', lambda e: e.max_index(out=P16[:, h, 0:8], in_max=TS[:, h, 0:8], in_values=cand[:, h, :]), r=['cand', 'TS'], w=['P16'])
                    kb.op('dve', lambda e: e.match_replace(out=cand2[:, h, :], in_to_replace=TS[:, h, 0:8], in_values=cand[:, h, :], imm_value=-1e30),
                          r=['cand', 'TS'], w=['cand2'])
                    kb.op('dve', lambda e: e.max(out=TS[:, h, 8:16], in_=cand2[:, h, :]), r=['cand2'], w=['TS'])
                    kb.op('dve', lambda e: e.max_index(out=P16[:, h, 8:16], in_max=TS[:, h, 8:16], in_values=cand2[:, h, :]), r=['cand2', 'TS'], w=['P16'])
                kb.op('dve', lambda e: e.tensor_single_scalar(out=Pi[:], in_=P16[:], scalar=4, op=ALU.logical_shift_right), r=['P16'], w=['Pi'])
                kb.op('dve', lambda e: e.tensor_single_scalar(out=Pj[:], in_=P16[:], scalar=15, op=ALU.bitwise_and), r=['P16'], w=['Pj'])
                kb.op('dve', lambda e: e.tensor_copy(out=Pif[:], in_=Pi[:]), r=['Pi'], w=['Pif'])
                kb.op('dve', lambda e: e.tensor_copy(out=Pjf[:], in_=Pj[:]), r=['Pj'], w=['Pjf'])
                io = bass.AP(iota16[:].tensor, iota16[:].offset, [pst(iota16[:]), [0, 8], [0, 16], [1, 16]])
                for (Pf, pk, off, dst, dk) in ((Pif, 'Pif', 0, ia, 'ia'), (Pjf, 'Pjf', 16, ib, 'ib')):
                    pfb = bass.AP(Pf[:].tensor, Pf[:].offset, [pst(Pf[:]), [16, 8], [1, 16], [0, 16]])
                    ifb = bass.AP(I16f[:].tensor, I16f[:].offset + off, [pst(I16f[:]), [32, 8], [0, 16], [1, 16]])
                    kb.op('dve', lambda e: e.tensor_tensor(out=eq[:], in0=io, in1=pfb, op=ALU.is_equal), r=['iota16', pk], w=['eq'])
                    kb.op('dve', lambda e: e.tensor_tensor(out=eq[:], in0=eq[:], in1=ifb, op=ALU.mult), r=['eq', 'I16f'], w=['eq'])
                    kb.op('dve', lambda e: e.tensor_reduce(out=dst[:], in_=eq[:], axis=AX.X, op=ALU.add), r=['eq'], w=[dk])
                kb.op('dve', lambda e: e.tensor_scalar(out=nmax[:], in0=TS[:, :, 0], scalar1=-1.0, scalar2=None, op0=ALU.mult), r=['TS'], w=['nmax'])
                for h in range(8):
                    kb.op('act', lambda e: e.activation(out=Gt[:, h, :], in_=TS[:, h, :], func=AF.Exp, bias=nmax[:, h:h + 1], accum_out=Z[:, h:h + 1]),
                          r=['TS', 'nmax'], w=['Gt', 'Z'])
                kb.op('dve', lambda e: e.reciprocal(out=Z[:], in_=Z[:]), r=['Z'], w=['Z'])
                zb = bass.AP(Z[:].tensor, Z[:].offset, [pst(Z[:]), [1, 8], [0, 16]])
                kb.op('dve', lambda e: e.tensor_tensor(out=Gt[:], in0=Gt[:], in1=zb, op=ALU.mult), r=['Gt', 'Z'], w=['Gt'])
                psx = t.ps[0]
                for i, (src, sk) in enumerate(((ia, 'ia'), (ib, 'ib'), (Gt, 'Gt'))):
                    kb.op('pe', lambda e: e.transpose(psx[:, i * 128:(i + 1) * 128], src[:].rearrange("p h k -> p (h k)"), t.ident[:]),
                          r=[sk, 'ident'], w=[('ps', 0)])
                kb.op('act', lambda e: e.activation(out=tr3[:], in_=psx[:, 0:384].rearrange("p (a b) -> p a b", b=128), func=AF.Identity),
                      r=[('ps', 0)], w=['tr3'])
                gs_, gk = Gst[tt % 2], ('Gst', tt % 2)
                kb.op('dve', lambda e: e.tensor_scalar(out=nb4[:], in0=tr3[:, 1, :], scalar1=-4.0, scalar2=None, op0=ALU.mult), r=['tr3'], w=['nb4'])

                def emit_B(tok):
                    bb_ = tok % 8
                    kb.op('act', lambda e: e.activation(out=Bt[bb_][:], in_=iota128[:], func=AF.Derivative_Erf, scale=4.0, bias=nb4[:, tok:tok + 1]),
                          r=['iota128', 'nb4'], w=[('Bt', bb_)])

                for tok in range(4):
                    emit_B(tok)
                for g4 in range(32):
                    pg = 1 + (gq % 2)
                    psg = t.ps[pg]
                    for tok in range(g4 * 4, g4 * 4 + 4):
                        ab = tok % 4
                        kb.op('dve', lambda e: e.tensor_scalar(out=At[ab][:], in0=iota128[:], scalar1=tr3[:, 0, tok:tok + 1], scalar2=tr3[:, 2, tok:tok + 1],
                                                               op0=ALU.is_equal, op1=ALU.mult), r=['iota128', 'tr3'], w=[('At', ab)])
                        oap = bass.AP(psg[:].tensor, psg[:].offset + ab, [pst(psg[:]), [4, 128]])
                        kb.op('pe', lambda e: e.matmul(oap, lhsT=Bt[tok % 8][:], rhs=At[ab][:], start=True, stop=True),
                              r=[('At', ab), ('Bt', tok % 8)], w=[('ps', pg)])
                    if g4 + 1 < 32:
                        for tok in range((g4 + 1) * 4, (g4 + 1) * 4 + 4):
                            emit_B(tok)
                    kb.op('act', lambda e: e.activation(out=gs_[:, :, g4 * 4:(g4 + 1) * 4], in_=psg[:].rearrange("p (a b) -> p a b", b=4),
                                                        func=AF.Identity, scale=0.8862269254527580), r=[('ps', pg)], w=[gk])
                    gq += 1
                kb.dma('sp', lambda e: e.dma_start(out=gsv[:, :, tt * 128:(tt + 1) * 128], in_=gs_[:]), r=[gk], w=['G_scr'])
        kb.barrier()
        acc = es0.enter_context(nc.sbuf_tensor('acc', [128, 8, 2048], F32))
        GRP = 4
        with ExitStack() as es:
            sb = lambda name, shape, dt: es.enter_context(nc.sbuf_tensor(name, shape, dt))
            GA = [sb('GA%d' % i, [128, GRP, 1024], BF16) for i in range(2)]
            Vb = [sb('Vb%d' % i, [128, GRP, 2048], BF16) for i in range(2)]
            Ur = [sb('Ur%d' % i, [128, 2048], F32) for i in range(3)]
            UT = [sb('UT%d' % i, [128, 16, 128], BF16) for i in range(2)]
            gsb = [sb('gsb%d' % i, [128, 1024], BF16) for i in range(2)]
            Gl = [sb('Gl%d' % i, [128, 1024], BF16) for i in range(2)]
            state = {'n': 0, 'ev': 0}

            def emit_scores(grp):
                gb_ = grp % 2
                for ai in range(GRP):
                    a = grp * GRP + ai
                    nb = state['n'] % 3
                    state['n'] += 1
                    kb.dma('sp', lambda e: e.dma_start(out=Ur[nb][:], in_=t.peer_u.ap()[a * 128:(a + 1) * 128, :]), w=[('Ur', nb)])
                    kb.dma('pool', lambda e: e.dma_start(out=Vb[gb_][:, ai, :], in_=t.peer_v.ap()[a * 128:(a + 1) * 128, :]), w=[('Vb', gb_)])
                    kb.dma('sp', lambda e: e.dma_start(out=Gl[nb % 2][:], in_=t.G_scr.ap()[a]), r=['G_scr'], w=[('Gl', nb % 2)])
                    for g in range(4):
                        p = 4 + g
                        ps = t.ps[p]
                        for i in range(4):
                            k = g * 4 + i
                            kb.op('pe', lambda e: e.transpose(ps[:, i * 128:(i + 1) * 128], Ur[nb][:, k * 128:(k + 1) * 128], t.ident[:]),
                                  r=[('Ur', nb), 'ident'], w=[('ps', p)])
                        eng = 'act' if state['ev'] % 2 == 0 else 'dve'
                        state['ev'] += 1
                        if eng == 'act':
                            kb.op('act', lambda e: e.activation(out=UT[nb % 2][:, g * 4:(g + 1) * 4, :], in_=ps[:].rearrange("p (a b) -> p a b", b=128),
                                                                func=AF.Identity), r=[('ps', p)], w=[('UT', nb % 2)])
                        else:
                            kb.op('dve', lambda e: e.tensor_copy(out=UT[nb % 2][:, g * 4:(g + 1) * 4, :], in_=ps[:].rearrange("p (a b) -> p a b", b=128)),
                                  r=[('ps', p)], w=[('UT', nb % 2)])
                    for tb in range(2):
                        p = 2 + tb
                        ps = t.ps[p]
                        for k in range(16):
                            kb.op('pe', lambda e: e.matmul(ps[:], lhsT=UT[nb % 2][:, k, :], rhs=u2T[:, k, tb * 512:(tb + 1) * 512],
                                                           start=(k == 0), stop=(k == 15)), r=[('UT', nb % 2), 'u2T'], w=[('ps', p)])
                        kb.op('act', lambda e: e.activation(out=gsb[nb % 2][:, tb * 512:(tb + 1) * 512], in_=ps[:], func=AF.Gelu),
                              r=[('ps', p)], w=[('gsb', nb % 2)])
                    kb.op('dve', lambda e: e.tensor_tensor(out=GA[gb_][:, ai, :], in0=gsb[nb % 2][:], in1=Gl[nb % 2][:], op=ALU.mult),
                          r=[('gsb', nb % 2), ('Gl', nb % 2)], w=[('GA', gb_)])

            def emit_out(grp):
                gb_ = grp % 2
                for tt in range(8):
                    for db in range(4):
                        p = (tt * 4 + db) % 2
                        ps = t.ps[p]
                        for ai in range(GRP):
                            kb.op('pe', lambda e: e.matmul(ps[:], lhsT=GA[gb_][:, ai, tt * 128:(tt + 1) * 128], rhs=Vb[gb_][:, ai, db * 512:(db + 1) * 512],
                                                           start=(ai == 0), stop=(ai == GRP - 1)), r=[('GA', gb_), ('Vb', gb_)], w=[('ps', p)])
                        ak = ('acc', tt * 4 + db)
                        if grp == 0:
                            kb.op('act', lambda e: e.activation(out=acc[:, tt, db * 512:(db + 1) * 512], in_=ps[:], func=AF.Identity), r=[('ps', p)], w=[ak])
                        else:
                            kb.op('dve', lambda e: e.tensor_tensor(out=acc[:, tt, db * 512:(db + 1) * 512], in0=ps[:], in1=acc[:, tt, db * 512:(db + 1) * 512],
                                                                   op=ALU.add), r=[('ps', p), ak], w=[ak])

            NG = 128 // GRP
            emit_scores(0)
            for grp in range(NG):
                if grp + 1 < NG:
                    emit_scores(grp + 1)
                emit_out(grp)
        kb.barrier()
        with ExitStack() as es:
            sb = lambda name, shape, dt: es.enter_context(nc.sbuf_tensor(name, shape, dt))
            g2b = sb('g2b', [128, 2048], F32)
            l2g = sb('l2g', [128, 2048], F32)
            l2b = sb('l2b', [128, 2048], F32)
            x1t = [sb('x1t%d' % i, [128, 2048], F32) for i in range(2)]
            rr = [sb('rr2_%d' % i, [128, 2048], F32) for i in range(2)]
            st = sb('st2', [128, 4, 6], F32)
            mv = sb('mv2', [128, 2], F32)
            rs = sb('rs2', [128, 1], F32)
            kb.dma('sp', lambda e: e.dma_start(out=g2b[:], in_=bcast_row(t.m_scr, 5 * D, D)), r=['m_scr'], w=['g2b'])
            kb.dma('sp', lambda e: e.dma_start(out=l2g[:], in_=bcast_row(t.ln2_g, 0, D)), w=['l2g'])
            kb.dma('sp', lambda e: e.dma_start(out=l2b[:], in_=bcast_row(t.ln2_b, 0, D)), w=['l2b'])
            AK = keys('acc', 32)
            for tt in range(8):
                xb, xk = x1t[tt % 2], ('x1t', tt % 2)
                r, rk = rr[tt % 2], ('rr2', tt % 2)
                kb.dma('sp', lambda e: e.dma_start(out=xb[:], in_=t.x1_scr.ap()[tt * 128:(tt + 1) * 128, :]), r=['x1_scr'], w=[xk])
                kb.op('pool', lambda e: e.tensor_tensor(out=r[:], in0=acc[:, tt, :], in1=g2b[:], op=ALU.mult), r=AK + ['g2b'], w=[rk])
                kb.op('dve', lambda e: e.scalar_tensor_tensor(out=r[:], in0=xb[:], scalar=ALPHA, in1=r[:], op0=ALU.mult, op1=ALU.add), r=[xk, rk], w=[rk])
                layer_norm_tile(kb, nc, r, rk, st, mv, rs, l2g, 'l2g', l2b, 'l2b', 'ln2')
                kb.dma('sp', lambda e: e.dma_start(out=t.out.ap()[tt * 128:(tt + 1) * 128, :], in_=r[:]), r=[rk], w=['out'])


ALL_STAGES = ('ada', 'uT', 'proj', 'bias', 'att', 'filt', 'conv', 'merge', 'peer')


def build_program(stages=ALL_STAGES, debug=False):
    nc = bass.Bass("TRN2", target_bir_lowering=False)
    t = T()
    t.debug = debug
    dt_in = lambda name, shape, dt=F32: nc.dram_tensor(name, shape, dt, kind="ExternalInput")
    scr_kind = "ExternalOutput" if debug else "Internal"
    dt_scr = lambda name, shape, dt=F32: nc.dram_tensor(name, shape, dt, kind=scr_kind)
    t.xr = dt_in('xr', [L, D])
    t.c2 = dt_in('c2', [2, D])
    t.ctxb = dt_in('ctxb', [CTX, D])
    t.w_ada = dt_in('w_ada', [D, 6 * D])
    t.b_ada = dt_in('b_ada', [1, 6 * D])
    t.w_in = dt_in('w_in', [D, 6144])
    t.conv_w = dt_in('conv_w', [3, 3072])
    t.conv_b = dt_in('conv_b', [1, 3072])
    t.e01 = dt_in('e01', [1, 2])
    t.ident_d = dt_in('ident', [128, 128])
    t.na_rpb = dt_in('na_rpb', [16, 15, 31])
    t.dhot_d = dt_in('dhot', [32, 4096])
    t.rowmask = dt_in('rowmask', [128, 7, 6, 64])
    t.na_norm_g = dt_in('na_norm_g', [1, 1024])
    t.zT_d = dt_in('zT', [33, 2048])
    t.E_d = dt_in('Edec', [L, 1024])
    t.Wf_d = dt_in('Wf', [32, 128, 16, 128], BF16)
    t.Wi_d = dt_in('Wi', [16, 128, 32, 128], BF16)
    t.filt_w1 = dt_in('filt_w1', [33, 64])
    t.filt_w2 = dt_in('filt_w2', [64, 64])
    t.filt_w3 = dt_in('filt_w3', [64, 64])
    t.filt_w4 = dt_in('filt_w4', [64, 4096])
    t.filt_b = dt_in('filt_b', [3, 64])
    t.filt_freq = dt_in('filt_freq', [3, 64])
    t.hy_skip = dt_in('hy_skip', [1, 2048])
    t.hy_norm_g = dt_in('hy_norm_g', [1, 1024])
    t.w_out = dt_in('w_out', [D, D])
    t.ln1_g = dt_in('ln1_g', [1, D])
    t.ln1_b = dt_in('ln1_b', [1, D])
    t.ln2_g = dt_in('ln2_g', [1, D])
    t.ln2_b = dt_in('ln2_b', [1, D])
    t.peer_wq = dt_in('peer_wq', [D, D])
    t.peer_keys = dt_in('peer_keys', [16, 128, 128])
    t.peer_u = dt_in('peer_u', [16384, D])
    t.peer_v = dt_in('peer_v', [16384, D])
    t.iota16_d = dt_in('iota16', [128, 16])
    t.iota128_d = dt_in('iota128', [128, 128])
    t.out = nc.dram_tensor('out', [LH, D], F32, kind="ExternalOutput")
    t.m_scr = dt_scr('m_scr', [2, 6 * D])
    t.hz_tok = dt_scr('hz_tok', [L, 3072])
    t.QT_scr = dt_scr('QT_scr', [16, 64, 1024], BF16)
    t.KT_scr = dt_scr('KT_scr', [16, 64, NTOK], BF16)
    t.V_scr = dt_scr('V_scr', [NTOK, 1024], BF16)
    t.MBT_scr = dt_scr('MBT_scr', [240, 4096])
    t.Kf_scr = dt_scr('Kf_scr', [2, 4096, 1024])
    t.hy_scr = dt_scr('hy_scr', [LH, 1024])
    t.x1_scr = dt_scr('x1_scr', [LH, D])
    t.u2_scr = dt_scr('u2_scr', [LH, D])
    t.qT_scr = dt_scr('qT_scr', [16, 128, LH])
    t.G_scr = dt_scr('G_scr', [128, 128, LH], BF16)
    if debug:
        t.na_dbg = dt_scr('na_dbg', [LH, 1024])
    with ExitStack() as es:
        es.enter_context(nc.allow_non_contiguous_dma(reason="small strided parameter loads"))
        es.enter_context(nc.allow_low_precision(reason="bf16 matmul operands"))
        kb = KB(nc, es)
        t.ps = [es.enter_context(nc.psum_tensor('ps%d' % i, [128, 512], F32)) for i in range(8)]
        t.ident = es.enter_context(nc.sbuf_tensor('ident_sb', [128, 128], F32))
        kb.dma('sp', lambda e: e.dma_start(out=t.ident[:], in_=t.ident_d.ap()), w=['ident'])
        if 'ada' in stages:
            stage_ada(kb, t)
        kb.barrier()
        with ExitStack() as es2:
            uT = es2.enter_context(nc.sbuf_tensor('uT', [128, 16, NTOK], BF16))
            if 'uT' in stages:
                stage_uT(kb, t, uT)
                kb.barrier()
            if 'proj' in stages:
                stage_proj(kb, t, uT)
        kb.barrier()
        if 'bias' in stages:
            stage_bias(kb, t)
        kb.barrier()
        with ExitStack() as es3:
            mergedT = es3.enter_context(nc.sbuf_tensor('mergedT', [128, 16, LH], BF16))
            if 'att' in stages:
                stage_att(kb, t, mergedT)
            kb.barrier()
            if 'filt' in stages:
                stage_filters(kb, t)
            kb.barrier()
            if 'conv' in stages:
                stage_conv(kb, t)
            kb.barrier()
            if 'merge' in stages:
                stage_merge_a(kb, t, mergedT)
                kb.barrier()
                stage_merge_b(kb, t, mergedT)
                kb.barrier()
        u2T = es.enter_context(nc.sbuf_tensor('u2T', [128, 16, LH], BF16))
        if 'merge' in stages:
            stage_peer_q(kb, t, u2T)
            kb.barrier()
        if 'peer' in stages:
            stage_peer(kb, t, u2T)
        kb.finish(['m_scr', 'hz_tok', 'QT_scr', 'KT_scr', 'V_scr', 'MBT_scr', 'na_dbg', 'Kf_scr', 'hy_scr', 'x1_scr', 'u2_scr', 'qT_scr', 'G_scr', 'out'])
        print("instructions:", kb.ninst)
    return nc


def host_consts(half):
    c = {}
    dh = np.zeros((32, 64, 64), np.float32)
    for kc in range(64):
        for q in range(64):
            d = kc - q + 15
            if 0 <= d <= 30:
                dh[d, kc, q] = 1.0
            ws = min(max(q - 8, 0), 48)
            dh[31, kc, q] = 0.0 if ws <= kc < ws + 16 else NEG
    c['dhot'] = dh.reshape(32, 4096)
    rm = np.full((7, 12), NEG, np.float32)
    for ci, i in enumerate([0, 1, 2, 3, 13, 14, 15]):
        for k in range(12):
            if i < 4:
                ok = (4 <= k < 12) if half == 0 else (i <= k < i + 8)
            else:
                ok = (i - 12 <= k < i - 4) if half == 0 else (0 <= k < 8)
            if ok:
                rm[ci, k] = 0.0
    T0 = half * LH
    sg = (np.arange(L) + T0) % L
    tt_ = np.linspace(0.0, 1.0, L, dtype=np.float32)[sg][:, None]
    wv_ = (2.0 * np.pi * sg.astype(np.float32) / L).astype(np.float32)[:, None]
    ff_ = np.linspace(1e-4, 15.0, 16, dtype=np.float32)[None, :]
    zz = np.concatenate([tt_, np.cos(ff_ * wv_), -np.sin(ff_ * wv_)], axis=-1).astype(np.float32)
    c['zT'] = np.ascontiguousarray(zz.T)
    deltas = np.abs(np.linspace(np.log(1e-2) / 1.5, np.log(1e-2) / 0.3, DHY, dtype=np.float32))
    c['Edec'] = np.exp(-tt_ * deltas[None, :]).astype(np.float32)
    rr = np.arange(NFFT)
    fq = np.where(rr <= 2048, rr, rr - 2048).astype(np.int64)
    ang = 2.0 * np.pi * ((sg[:, None].astype(np.int64) * fq[None, :]) % NFFT) / NFFT
    Wfull = np.where(rr[None, :] <= 2048, np.cos(ang), -np.sin(ang))
    c['Wf'] = np.ascontiguousarray(Wfull.reshape(16, 128, 32, 128).transpose(2, 1, 0, 3)).astype(ml_dtypes.bfloat16)
    wgt = np.where((rr == 0) | (rr == 2048), 1.0, 2.0) / NFFT
    Winv = (np.where(rr[None, :] <= 2048, np.cos(ang), -np.sin(ang)) * wgt[None, :]).T
    c['Wi'] = np.ascontiguousarray(Winv.reshape(32, 128, 16, 128).transpose(2, 1, 0, 3)).astype(ml_dtypes.bfloat16)
    rm2 = np.zeros((128, 7, 6, 64), np.float32)
    for j in range(2):
        rm2[j * 64:(j + 1) * 64] = rm[None, :, j::2, None]
    c['rowmask'] = rm2
    return c


def make_core_inputs(inputs, b, half):
    T0 = half * LH
    f = lambda a: np.ascontiguousarray(a, dtype=np.float32)
    m = {}
    m['xr'] = f(np.roll(inputs['x'][b], -T0, axis=0))
    m['c2'] = f(np.stack([inputs['c'][b], inputs['c_ctx']]))
    m['ctxb'] = f(inputs['ctx'][b])
    m['w_ada'] = f(inputs['w_ada'][0])
    m['b_ada'] = f(inputs['b_ada'][0][None])
    m['w_in'] = f(inputs['w_in'][0])
    m['conv_w'] = f(inputs['conv_w'][0])
    m['conv_b'] = f(inputs['conv_b'][0][None])
    m['e01'] = np.array([[1.0, 0.0]] if half == 0 else [[0.0, 1.0]], dtype=np.float32)
    m['ident'] = np.eye(128, dtype=np.float32)
    m['na_rpb'] = f(inputs['na_rpb'][0])
    m['na_norm_g'] = f(inputs['na_norm_g'][0][None])
    for k_ in ('filt_w1', 'filt_w2', 'filt_w3', 'filt_w4', 'filt_freq'):
        m[k_] = f(inputs[k_][0])
    m['filt_b'] = f(np.stack([inputs['filt_b1'][0], inputs['filt_b2'][0], inputs['filt_b3'][0]]))
    m['hy_skip'] = f(inputs['hy_skip'][0].reshape(1, 2048))
    m['hy_norm_g'] = f(inputs['hy_norm_g'][0][None])
    m['w_out'] = f(inputs['w_out'][0])
    for k_ in ('ln1_g', 'ln1_b', 'ln2_g', 'ln2_b'):
        m[k_] = f(inputs[k_][0][None])
    m['peer_wq'] = f(inputs['peer_wq'][0])
    m['peer_keys'] = f(inputs['peer_keys'][0].reshape(16, 128, 128))
    m['peer_u'] = f(inputs['peer_u'][0])
    m['peer_v'] = f(inputs['peer_v'][0])
    m['iota16'] = np.ascontiguousarray(np.broadcast_to(np.arange(16, dtype=np.float32)[None], (128, 16)))
    m['iota128'] = np.ascontiguousarray(np.broadcast_to(np.arange(128, dtype=np.float32)[None], (128, 128)))
    m.update(host_consts(half))
    return m


def kernel(**inputs):
    nc = build_program()
    in_maps = [make_core_inputs(inputs, c // 2, c % 2) for c in range(8)]
    res = run_bass_kernel_spmd(nc, in_maps, core_ids=list(range(8)))
    out = np.zeros((4, L, D), dtype=np.float32)
    for c in range(8):
        b, half = c // 2, c % 2
        out[b, half * LH:(half + 1) * LH] = res.results[c]['out']
    return out
```
